# Optimizing a Trainium2 kernel written in Bass

```python
import math
import jax, jax.numpy as jnp
from jax import lax
import numpy as np

D_MODEL = 1024
BATCH = 16
SEQ = 2048
DEPTH = 4

N_MIXERS = 3
MLA_HEADS = 8
MLA_Q_LORA = 384
MLA_KV_LORA = 256
MLA_NOPE = 128
MLA_ROPE = 64
MLA_V = 128
MLA_QK = MLA_NOPE + MLA_ROPE
ROPE_THETA = 10000.0
Q_BLOCK = 128
HG_HEADS = 8
HG_DK = D_MODEL // HG_HEADS
HG_DV = D_MODEL // HG_HEADS
HG_CHUNK = 64
CONV_WIDTH = 31
D_FF = 4 * D_MODEL
EPS = 1e-6

N_MLA_LAYERS = (DEPTH + 2) // 3
N_HGRN_LAYERS = (DEPTH + 1) // 3
N_CONV_LAYERS = DEPTH // 3

kernel_name = "hybrid_mla_hgrn2_conformer_trunk"


def rms_norm(x, g):
    xf = x.astype(jnp.float32)
    y = xf * lax.rsqrt(jnp.mean(xf * xf, axis=-1, keepdims=True) + EPS)
    return (y * g.astype(jnp.float32)).astype(x.dtype)


def layer_norm(x, g, b):
    xf = x.astype(jnp.float32)
    mu = jnp.mean(xf, axis=-1, keepdims=True)
    xc = xf - mu
    y = xc * lax.rsqrt(jnp.mean(xc * xc, axis=-1, keepdims=True) + EPS)
    return (y * g.astype(jnp.float32) + b.astype(jnp.float32)).astype(x.dtype)


def apply_rope(t, cos, sin):
    t1, t2 = jnp.split(t.astype(jnp.float32), 2, axis=-1)
    out = jnp.concatenate([t1 * cos - t2 * sin, t2 * cos + t1 * sin], axis=-1)
    return out.astype(t.dtype)


def causal_block_attention(q, k, v):
    B, S, H, Dq = q.shape
    nb = S // Q_BLOCK
    scale = Dq ** -0.5
    qb = q.reshape(B, nb, Q_BLOCK, H, Dq).transpose(1, 0, 2, 3, 4)
    key_pos = jnp.arange(S)

    def one_block(args):
        q_blk, blk = args
        q_pos = blk * Q_BLOCK + jnp.arange(Q_BLOCK)
        s = jnp.einsum('bqhd,bkhd->bhqk', q_blk, k, preferred_element_type=jnp.float32) * scale
        s = jnp.where(key_pos[None, :] <= q_pos[:, None], s, -jnp.inf)
        p = jax.nn.softmax(s, axis=-1)
        return jnp.einsum('bhqk,bkhd->bqhd', p.astype(v.dtype), v)

    ob = lax.map(one_block, (qb, jnp.arange(nb)))
    return ob.transpose(1, 0, 2, 3, 4).reshape(B, S, H, v.shape[-1])


def mla_mixer(h, cos, sin, w_down, q_lat_norm, kv_lat_norm, w_uq, w_ukv, q_head_norm, k_head_norm, w_o):
    B, S, _ = h.shape
    lat = h @ w_down
    c_q, c_kv, k_rope = jnp.split(lat, [MLA_Q_LORA, MLA_Q_LORA + MLA_KV_LORA], axis=-1)
    c_q = rms_norm(c_q, q_lat_norm)
    c_kv = rms_norm(c_kv, kv_lat_norm)
    q = (c_q @ w_uq).reshape(B, S, MLA_HEADS, MLA_QK)
    kv = (c_kv @ w_ukv).reshape(B, S, MLA_HEADS, MLA_NOPE + MLA_V)
    q_nope, q_rope = jnp.split(q, [MLA_NOPE], axis=-1)
    k_nope, v = jnp.split(kv, [MLA_NOPE], axis=-1)
    q_nope = rms_norm(q_nope, q_head_norm[:MLA_NOPE])
    q_rope = rms_norm(q_rope, q_head_norm[MLA_NOPE:])
    k_nope = rms_norm(k_nope, k_head_norm[:MLA_NOPE])
    k_rope = rms_norm(k_rope, k_head_norm[MLA_NOPE:])
    q_rope = apply_rope(q_rope, cos[:, :, None, :], sin[:, :, None, :])
    k_rope = apply_rope(k_rope, cos, sin)
    q = jnp.concatenate([q_nope, q_rope], axis=-1)
    k = jnp.concatenate([k_nope, jnp.broadcast_to(k_rope[:, :, None, :], (B, S, MLA_HEADS, MLA_ROPE))], axis=-1)
    o = causal_block_attention(q, k, v)
    return o.reshape(B, S, MLA_HEADS * MLA_V) @ w_o


def hgrn2_chunk_scan(q, k, v, log_f):
    _, B, H, C, dk = q.shape
    dv = v.shape[-1]
    causal = jnp.tril(jnp.ones((C, C), dtype=bool))

    def step(state, inp):
        qc, kc, vc, lfc = inp
        b = jnp.cumsum(lfc, axis=2)
        b_last = b[:, :, -1:, :]
        o_inter = jnp.einsum('bhtk,bhkv->bhtv', qc * jnp.exp(b), state)
        diff = b[:, :, :, None, :] - b[:, :, None, :, :]
        decay = jnp.exp(jnp.where(causal[:, :, None], diff, -jnp.inf))
        attn = jnp.einsum('bhtk,bhsk,bhtsk->bhts', qc, kc, decay)
        o_intra = jnp.einsum('bhts,bhsv->bhtv', attn, vc)
        new_state = jnp.exp(b_last[:, :, 0, :])[..., None] * state + jnp.einsum(
            'bhsk,bhsv->bhkv', kc * jnp.exp(b_last - b), vc)
        return new_state, o_inter + o_intra

    init = jnp.zeros((B, H, dk, dv), jnp.float32)
    _, o = lax.scan(step, init, (q, k, v, log_f))
    return o


def hgrn2_mixer(h, lb, w_in, out_norm, w_o):
    B, S, _ = h.shape
    nC = S // HG_CHUNK
    proj = h @ w_in
    q, fz, i, g = jnp.split(proj, 4, axis=-1)
    fz = fz.astype(jnp.float32)
    f = lb + (1.0 - lb) * jax.nn.sigmoid(fz)
    log_f = jnp.log(f)
    k = (1.0 - lb) * jax.nn.sigmoid(-fz)

    def to_chunks(t, d):
        return t.astype(jnp.float32).reshape(B, nC, HG_CHUNK, HG_HEADS, d).transpose(1, 0, 3, 2, 4)

    o = hgrn2_chunk_scan(to_chunks(q, HG_DK), to_chunks(k, HG_DK), to_chunks(i, HG_DV), to_chunks(log_f, HG_DK))
    o = o.transpose(1, 0, 3, 2, 4).reshape(B, S, HG_HEADS, HG_DV)
    gate = jax.nn.silu(g.astype(jnp.float32)).reshape(B, S, HG_HEADS, HG_DV)
    o = rms_norm(o, out_norm) * gate
    return o.reshape(B, S, HG_HEADS * HG_DV).astype(h.dtype) @ w_o


def conformer_conv_mixer(h, w_pw1, b_pw1, w_dw, b_dw, ln_g, ln_b, w_pw2, b_pw2):
    a, gate = jnp.split(h @ w_pw1 + b_pw1, 2, axis=-1)
    u = a * jax.nn.sigmoid(gate)
    u = lax.conv_general_dilated(
        u, w_dw[:, None, :].astype(u.dtype), window_strides=(1,), padding=[(CONV_WIDTH - 1, 0)],
        dimension_numbers=('NWC', 'WIO', 'NWC'), feature_group_count=D_MODEL) + b_dw
    u = jax.nn.silu(layer_norm(u, ln_g, ln_b))
    return u @ w_pw2 + b_pw2


def squared_relu_mlp(h, w_in, w_out):
    a = jax.nn.relu(h @ w_in)
    return (a * a) @ w_out


def setup_inputs(seed: int = 0) -> dict:
    key = jax.random.key(seed)
    ks = iter(jax.random.split(key, 32))
    f32 = jnp.float32

    def nrm(shape, scale):
        return jax.random.normal(next(ks), shape, f32) * scale

    def gain(shape):
        return 1.0 + 0.1 * jax.random.normal(next(ks), shape, f32)

    def bias(shape):
        return 0.02 * jax.random.normal(next(ks), shape, f32)

    x = nrm((BATCH, SEQ, D_MODEL), 1.0)
    offsets = jax.random.randint(next(ks), (BATCH, 1), 0, 4096, dtype=jnp.int32)
    positions = offsets + jnp.arange(SEQ, dtype=jnp.int32)[None, :]
    nA, nB, nC = N_MLA_LAYERS, N_HGRN_LAYERS, N_CONV_LAYERS
    return {
        "x": x,
        "positions": positions,
        "norm_mix": gain((DEPTH, D_MODEL)),
        "norm_mlp": gain((DEPTH, D_MODEL)),
        "mlp_w_in": nrm((DEPTH, D_MODEL, D_FF), D_MODEL ** -0.5),
        "mlp_w_out": nrm((DEPTH, D_FF, D_MODEL), D_FF ** -0.5),
        "mla_w_down": nrm((nA, D_MODEL, MLA_Q_LORA + MLA_KV_LORA + MLA_ROPE), D_MODEL ** -0.5),
        "mla_q_lat_norm": gain((nA, MLA_Q_LORA)),
        "mla_kv_lat_norm": gain((nA, MLA_KV_LORA)),
        "mla_w_uq": nrm((nA, MLA_Q_LORA, MLA_HEADS * MLA_QK), MLA_Q_LORA ** -0.5),
        "mla_w_ukv": nrm((nA, MLA_KV_LORA, MLA_HEADS * (MLA_NOPE + MLA_V)), MLA_KV_LORA ** -0.5),
        "mla_q_head_norm": gain((nA, MLA_QK)),
        "mla_k_head_norm": gain((nA, MLA_QK)),
        "mla_w_o": nrm((nA, MLA_HEADS * MLA_V, D_MODEL), (MLA_HEADS * MLA_V) ** -0.5),
        "hg_w_in": nrm((nB, D_MODEL, 2 * HG_HEADS * HG_DK + 2 * HG_HEADS * HG_DV), D_MODEL ** -0.5),
        "hg_lb_logits": nrm((DEPTH, HG_HEADS * HG_DK), 0.5),
        "hg_out_norm": gain((nB, HG_DV)),
        "hg_w_o": nrm((nB, HG_HEADS * HG_DV, D_MODEL), (HG_HEADS * HG_DV) ** -0.5),
        "cv_w_pw1": nrm((nC, D_MODEL, 2 * D_MODEL), D_MODEL ** -0.5),
        "cv_b_pw1": bias((nC, 2 * D_MODEL)),
        "cv_w_dw": nrm((nC, CONV_WIDTH, D_MODEL), CONV_WIDTH ** -0.5),
        "cv_b_dw": bias((nC, D_MODEL)),
        "cv_ln_g": gain((nC, D_MODEL)),
        "cv_ln_b": bias((nC, D_MODEL)),
        "cv_w_pw2": nrm((nC, D_MODEL, D_MODEL), D_MODEL ** -0.5),
        "cv_b_pw2": bias((nC, D_MODEL)),
    }


def reference(x, positions, norm_mix, norm_mlp, mlp_w_in, mlp_w_out,
              mla_w_down, mla_q_lat_norm, mla_kv_lat_norm, mla_w_uq, mla_w_ukv,
              mla_q_head_norm, mla_k_head_norm, mla_w_o,
              hg_w_in, hg_lb_logits, hg_out_norm, hg_w_o,
              cv_w_pw1, cv_b_pw1, cv_w_dw, cv_b_dw, cv_ln_g, cv_ln_b, cv_w_pw2, cv_b_pw2):
    f32 = jnp.float32
    inv_freq = jnp.power(ROPE_THETA, -jnp.arange(0, MLA_ROPE, 2, dtype=f32) / MLA_ROPE)
    ang = positions.astype(f32)[..., None] * inv_freq
    cos, sin = jnp.cos(ang), jnp.sin(ang)
    lb_table = jnp.cumsum(jax.nn.softmax(hg_lb_logits.astype(f32), axis=0), axis=0)
    lb_table = lb_table - lb_table[0]

    for layer in range(DEPTH):
        kind, j = layer % N_MIXERS, layer // N_MIXERS
        h = rms_norm(x, norm_mix[layer])
        if kind == 0:
            mix = mla_mixer(h, cos, sin, mla_w_down[j], mla_q_lat_norm[j], mla_kv_lat_norm[j],
                            mla_w_uq[j], mla_w_ukv[j], mla_q_head_norm[j], mla_k_head_norm[j], mla_w_o[j])
        elif kind == 1:
            mix = hgrn2_mixer(h, lb_table[layer], hg_w_in[j], hg_out_norm[j], hg_w_o[j])
        else:
            mix = conformer_conv_mixer(h, cv_w_pw1[j], cv_b_pw1[j], cv_w_dw[j], cv_b_dw[j],
                                       cv_ln_g[j], cv_ln_b[j], cv_w_pw2[j], cv_b_pw2[j])
        x = x + mix.astype(x.dtype)
        h = rms_norm(x, norm_mlp[layer])
        x = x + squared_relu_mlp(h, mlp_w_in[layer], mlp_w_out[layer]).astype(x.dtype)
    return x
```

```python
import math
import numpy as np
from contextlib import ExitStack
from functools import partial

import concourse.bass as bass
import concourse.mybir as mybir
from concourse.bass_utils import run_bass_kernel_spmd

F32 = mybir.dt.float32
BF16 = mybir.dt.bfloat16
I32 = mybir.dt.int32
AF = mybir.ActivationFunctionType
ALU = mybir.AluOpType
AX = mybir.AxisListType

S = 2048
D = 1024
NT = 16
DFF = 4096
EPS = 1e-6
N_CORES = 8
ENGS = ("pe", "act", "dve", "pool", "sp")


class Buf:
    __slots__ = ("name", "last_w", "readers")

    def __init__(self, name):
        self.name = name
        self.last_w = None
        self.readers = []


class Prog:
    def __init__(self, nc):
        self.nc = nc
        self.ins = []
        self.last_on_eng = {e: None for e in ENGS}
        self.last_dma = {}
        self.pending_fence = {e: None for e in ENGS}

    def add(self, eng, fn, reads=(), writes=(), dma=None):
        i = len(self.ins)
        deps = set()
        for b in reads:
            if b.last_w is not None:
                deps.add(b.last_w)
        for b in writes:
            if b.last_w is not None:
                deps.add(b.last_w)
            deps.update(b.readers)
        if self.pending_fence[eng] is not None:
            deps |= self.pending_fence[eng]
            self.pending_fence[eng] = None
        self.ins.append(dict(eng=eng, fn=fn, deps=deps, dma=dma, sig=False))
        for b in reads:
            b.readers.append(i)
        for b in writes:
            b.last_w = i
            b.readers = []
        self.last_on_eng[eng] = i
        if dma is not None:
            self.last_dma[dma] = i
        return i

    def fence(self):
        s = set(v for v in self.last_on_eng.values() if v is not None)
        s |= set(self.last_dma.values())
        for e in ENGS:
            self.pending_fence[e] = set(s) | (self.pending_fence[e] or set())

    def emit(self, es, final_wait_groups=()):
        nc = self.nc
        ins = self.ins
        for r in ins:
            nd = set()
            for d in r["deps"]:
                p = ins[d]
                if p["dma"] is None and r["dma"] is None and p["eng"] == r["eng"] and r["eng"] == "pe":
                    continue
                nd.add(d)
            r["deps"] = nd
            for d in nd:
                ins[d]["sig"] = True
        eng_sem = {e: es.enter_context(nc.semaphore("s_" + e)) for e in ("pe", "act", "dve", "pool")}
        grp_sem = {}
        for r in ins:
            if r["dma"] is not None and r["dma"] not in grp_sem:
                grp_sem[r["dma"]] = es.enter_context(nc.semaphore("g_" + r["dma"]))
        cnt = {e: 0 for e in eng_sem}
        gcnt = {g: 0 for g in grp_sem}
        for r in ins:
            if r["dma"] is not None:
                gcnt[r["dma"]] += 16
                r["tok"] = ("g", r["dma"], gcnt[r["dma"]])
            elif r["sig"]:
                cnt[r["eng"]] += 1
                r["tok"] = ("e", r["eng"], cnt[r["eng"]])
        gtot = {g: 0 for g in grp_sem}
        per_eng = {e: [] for e in ENGS}
        known = {e: {} for e in ENGS}
        for r in ins:
            waits = {}
            for d in r["deps"]:
                kind, key, val = ins[d]["tok"]
                if kind == "g":
                    val = max(val, gtot[key])
                k = (kind, key)
                waits[k] = max(waits.get(k, 0), val)
            if r["dma"] is not None:
                gtot[r["dma"]] += 16
            kn = known[r["eng"]]
            wl = []
            for k, v in waits.items():
                if kn.get(k, 0) >= v:
                    continue
                kn[k] = v
                wl.append((k, v))
            per_eng[r["eng"]].append((r, wl))
        self.stats = dict(n={e: len(per_eng[e]) for e in ENGS}, sem=dict(cnt), nsem=len(grp_sem) + 4)

        def semof(k):
            return eng_sem[k[1]] if k[0] == "e" else grp_sem[k[1]]

        def run(engname, eobj):
            for r, wl in per_eng[engname]:
                for k, v in wl:
                    eobj.wait_ge(semof(k), v)
                bi = r["fn"](eobj)
                if r["dma"] is not None:
                    bi.then_inc(grp_sem[r["dma"]], 16)
                elif r["sig"]:
                    bi.then_inc(eng_sem[r["eng"]], 1)
            if engname == "sp":
                for g in final_wait_groups:
                    eobj.wait_ge(grp_sem[g], gcnt[g])

        with nc.Block() as block:
            @block.tensor
            def _(e):
                run("pe", e)

            @block.scalar
            def _(e):
                run("act", e)

            @block.vector
            def _(e):
                run("dve", e)

            @block.gpsimd
            def _(e):
                run("pool", e)

            @block.sync
            def _(e):
                run("sp", e)


class K:
    def __init__(self, nc, es, n_seq):
        self.nc = nc
        self.es = es
        self.P = Prog(nc)
        self.n_seq = n_seq
        self.uid = 0
        self.debug = False
        self.stop = None
        self.dbg_names = []

    def sb(self, name, shape, dt):
        return self.es.enter_context(self.nc.sbuf_tensor("sb_" + name, shape, dt))

    def mm(self, out, lhsT, rhs, start, stop, reads, writes):
        self.P.add("pe", lambda e: e.matmul(out, lhsT=lhsT, rhs=rhs, start=start, stop=stop), reads, writes)

    def tr(self, out, in_, ident, reads, writes):
        self.P.add("pe", lambda e: e.transpose(out=out, in_=in_, identity=ident), reads, writes)

    def act(self, out, in_, func, reads, writes, **kw):
        self.P.add("act", lambda e: e.activation(out=out, in_=in_, func=func, **kw), reads, writes)

    def tt(self, eng, out, in0, in1, op, reads, writes):
        self.P.add(eng, lambda e: e.tensor_tensor(out=out, in0=in0, in1=in1, op=op), reads, writes)

    def ts(self, eng, out, in0, s1, s2, op0, op1, reads, writes):
        if s2 is None:
            self.P.add(eng, lambda e: e.tensor_scalar(out=out, in0=in0, scalar1=s1, scalar2=None, op0=op0), reads, writes)
        else:
            self.P.add(eng, lambda e: e.tensor_scalar(out=out, in0=in0, scalar1=s1, scalar2=s2, op0=op0, op1=op1), reads, writes)

    def stt(self, eng, out, in0, scalar, in1, op0, op1, reads, writes):
        self.P.add(eng, lambda e: e.scalar_tensor_tensor(out=out, in0=in0, scalar=scalar, in1=in1, op0=op0, op1=op1), reads, writes)

    def cp(self, eng, out, in_, reads, writes):
        self.P.add(eng, lambda e: e.tensor_copy(out=out, in_=in_), reads, writes)

    def recip(self, out, in_, reads, writes):
        self.P.add("dve", lambda e: e.reciprocal(out=out, in_=in_), reads, writes)

    def memset(self, eng, ap, val, writes):
        self.P.add(eng, lambda e: e.memset(ap, val), (), writes)

    def dma(self, eng, out, in_, grp, reads=(), writes=()):
        self.P.add(eng, lambda e: e.dma_start(out=out, in_=in_), reads, writes, dma=grp)

    def arena_reset(self):
        self.P.fence()
        self.aoff = 0

    def carve(self, free_shape, dt):
        n = int(np.prod(free_shape))
        nbytes = n * (4 if dt in (F32, I32) else 2)
        nbytes = (nbytes + 63) // 64 * 64
        assert self.aoff + nbytes <= self.arena_bytes, (self.aoff, nbytes, self.arena_bytes)
        ap = self.arena[:, self.aoff // 2:(self.aoff + nbytes) // 2]
        self.aoff += nbytes
        if dt != BF16:
            ap = ap.bitcast(dt)
        ap = ap[:, 0:n]
        if len(free_shape) == 2:
            ap = ap.rearrange("p (a b) -> p a b", b=free_shape[1])
        elif len(free_shape) == 3:
            ap = ap.rearrange("p (a b c) -> p a b c", b=free_shape[1], c=free_shape[2])
        return ap

    def buf(self, name):
        self.uid += 1
        return Buf(f"{name}_{self.uid}")

    def arena_mark_reset(self, mark):
        self.P.fence()
        self.aoff = mark

    def red(self, out, in_, reads, writes):
        self.P.add("dve", lambda e: e.tensor_reduce(out=out, in_=in_, axis=AX.X, op=ALU.add), reads, writes)

    def dump(self, name, ap, shape, dt, reads):
        if not getattr(self, "debug", False) or ("dbg_" + name) in self.dbg_names:
            return
        d = self.nc.dram_tensor("dbg_" + name, list(shape), dt, kind="ExternalOutput").ap()
        self.dma("sp", d, ap, "dbg", reads=reads)
        self.dbg_names.append("dbg_" + name)


def build_program(n_seq, layers, lay, debug=False, stop=None):
    nc = bass.Bass("TRN2", target_bir_lowering=False)
    es = ExitStack()
    k = K(nc, es, n_seq)
    k.debug = debug
    k.stop = stop
    P = k.P
    dt = lambda name, shape, d, kind="ExternalInput": nc.dram_tensor(name, shape, d, kind=kind).ap()
    x_d = dt("x", [n_seq, S, D], F32)
    out_d = dt("out", [n_seq, S, D], F32, "ExternalOutput")
    pos_d = dt("pos", [n_seq, 128, NT], I32)
    pf_d = dt("pf", [128, lay["npf"]], F32)
    pt_d = dt("pt", [128, lay["npt"]], F32)
    mlp_wi_d = dt("mlp_wi", [4, D, DFF], F32)
    mlp_wo_d = dt("mlp_wo", [4, DFF, D], F32)
    k.dram = dict(x=x_d, out=out_d, pos=pos_d, pf=pf_d, pt=pt_d, mlp_wi=mlp_wi_d, mlp_wo=mlp_wo_d)
    k.dram["mla_wd"] = dt("mla_wd", [2, D, 704], F32)
    k.dram["mla_wuq"] = dt("mla_wuq", [2, 2, 384, 768], F32)
    k.dram["mla_wukv"] = dt("mla_wukv", [2, 2, 256, 1024], F32)
    k.dram["mla_wo"] = dt("mla_wo", [2, D, D], F32)
    k.dram["cv_w1"] = dt("cv_w1", [D, 2 * D], F32)
    k.dram["cv_w2"] = dt("cv_w2", [D, D], F32)
    k.dram["hg_wi"] = dt("hg_wi", [D, 4 * D], F32)
    k.dram["hg_wo"] = dt("hg_wo", [D, D], F32)
    k.lay = lay

    k.x = k.sb("x", [128, NT, D], F32)
    k.xb = [Buf(f"x{i}") for i in range(NT)]
    k.pf = k.sb("pf", [128, lay["npf"]], F32)
    k.pfb = Buf("pf")
    k.ident = k.sb("ident", [128, 128], BF16)
    k.identf = k.sb("identf", [128, 128], F32)
    k.cb = Buf("consts")
    k.ss = k.sb("ss", [128, 64], F32)
    k.ps = [es.enter_context(nc.psum_tensor(f"ps{i}", [128, 512], F32)) for i in range(8)]
    k.psb = [Buf(f"ps{i}") for i in range(8)]
    k.pt = k.sb("pt", [128, lay["npt"]], F32)
    k.ptb = Buf("pt")
    k.ones_bf = k.sb("ones_bf", [128, 128], BF16)
    k.invn12 = k.sb("invn12", [128, 12], F32)
    k.ones_f = k.sb("ones_f", [128, 128], F32)
    k.hgc = k.sb("hgc", [128, 72], F32)
    k.scanmask = k.sb("scanmask", [128, 512], F32)
    k.mask2 = k.sb("mask2", [128, 128], F32)
    k.negpi = k.sb("negpi", [128, 1], F32)
    k.rope_invf = k.sb("rope_invf", [128, 32], F32)
    k.rope_posi = k.sb("rope_posi", [128, NT], I32)
    k.rope_posf = k.sb("rope_posf", [128, NT], F32)
    k.cos2 = k.sb("cos2", [128, NT, 64], F32)
    k.sinA = k.sb("sinA", [128, NT, 64], F32)
    k.ropeb = Buf("rope")
    k.arena_bytes = 122 * 1024
    k.arena = k.sb("arena", [128, k.arena_bytes // 2], BF16)
    k.aoff = 0

    k.dma("sp", k.pf[:], pf_d, "pf", writes=[k.pfb])
    k.memset("pool", k.identf[:], 0.0, [k.cb])
    P.add("pool", lambda e: e.affine_select(out=k.identf[:], in_=k.identf[:], pattern=[[-1, 128]],
                                            compare_op=ALU.not_equal, fill=1.0, base=0, channel_multiplier=1),
          [k.cb], [k.cb])
    k.cp("dve", k.ident[:], k.identf[:], [k.cb], [k.cb])
    k.dma("sp", k.pt[:], pt_d, "pt", writes=[k.ptb])
    k.memset("dve", k.ones_bf[:], 1.0, [k.cb])
    k.memset("dve", k.ones_f[:], 1.0, [k.cb])
    k.memset("dve", k.invn12[:, 0:4], 1.0 / 128, [k.cb])
    k.memset("dve", k.invn12[:, 4:8], 1.0 / 64, [k.cb])
    k.memset("dve", k.invn12[:, 8:12], 1.0 / 128, [k.cb])
    k.memset("dve", k.negpi[:], -math.pi * (1.0 - 1e-6), [k.cb])
    for f in range(32):
        k.memset("pool", k.rope_invf[:, f:f + 1], float(np.float32(10000.0) ** np.float32(-2.0 * f / 64)), [k.cb])

    for (kind, l, j) in layers:
        if kind == "hgrn":
            emit_hgrn_consts(k, l)
    for s in range(n_seq):
        for i in range(NT):
            k.dma("sp", k.x[:, i, :], x_d[s, i * 128:(i + 1) * 128, :], "xio", writes=[k.xb[i]])
        if any(kd == "mla" for kd, _, _ in layers):
            emit_rope_tables(k, s)
        for (kind, l, j) in layers:
            if kind == "mlp":
                emit_mlp(k, l)
            elif kind == "mla":
                emit_mla(k, l, j)
            elif kind == "conv":
                emit_conv(k, l)
            elif kind == "hgrn":
                emit_hgrn(k, l)
            else:
                raise ValueError(kind)
        for i in range(NT):
            k.dma("sp", out_d[s, i * 128:(i + 1) * 128, :], k.x[:, i, :], "xio", reads=[k.xb[i]])
    P.emit(es, final_wait_groups=["xio"] + (["dbg"] if k.dbg_names else []))
    es.close()
    return nc, P.stats


def emit_norm_T(k, tiles, gcol, dst, tmp, after=None):
    for n, i in enumerate(tiles):
        slot = n % 2
        junk, jb = tmp["junk"][slot], tmp["junkb"][slot]
        xn, xnb = tmp["xn"][slot], tmp["xnb"][slot]
        st, stb = tmp["st"][slot], tmp["stb"][slot]
        pb = tmp["psum"][slot]
        k.act(junk, k.x[:, i, :], AF.Square, [k.xb[i]], [jb, stb], accum_out=st[:, 0:1])
        k.act(st[:, 1:2], st[:, 0:1], AF.Sqrt, [stb], [stb], bias=EPS, scale=1.0 / D)
        k.recip(st[:, 2:3], st[:, 1:2], [stb], [stb])
        k.ts("pool", xn, k.x[:, i, :], st[:, 2:3], None, ALU.mult, None, [k.xb[i], stb], [xnb])
        pbf = k.ps[pb][:].bitcast(BF16)
        for c in range(8):
            k.tr(pbf[:, c * 128:(c + 1) * 128], xn[:, c * 128:(c + 1) * 128], k.ident[:], [xnb, k.cb], [k.psb[pb]])
        d_ap, d_buf = dst(i)
        g = k.pf[:, gcol:gcol + 8]
        k.tt("dve", d_ap, pbf.rearrange("p (c t) -> p c t", t=128),
             g.unsqueeze(2).to_broadcast([128, 8, 128]), ALU.mult, [k.psb[pb], k.pfb], [d_buf])
        if after is not None:
            after(i)


def norm_tmp(k, psum_banks):
    t = dict(junk=[], junkb=[], xn=[], xnb=[], st=[], stb=[], psum=psum_banks)
    for s in range(2):
        t["junk"].append(k.carve([D], BF16))
        t["junkb"].append(k.buf("junk"))
        t["xn"].append(k.carve([D], BF16))
        t["xnb"].append(k.buf("xn"))
        t["st"].append(k.carve([8], F32))
        t["stb"].append(k.buf("st"))
    return t


def emit_mlp(k, l):
    P = k.P
    k.arena_reset()
    hT = k.carve([8, S], BF16)
    hTb = [k.buf("hT") for _ in range(4)]
    tmp = norm_tmp(k, [6, 7])
    wi = [k.carve([8, 512], BF16) for _ in range(2)]
    wo = [k.carve([4, D], BF16) for _ in range(2)]
    wib = [k.buf("wi") for _ in range(2)]
    wob = [k.buf("wo") for _ in range(2)]
    aT = [k.carve([4, 512], BF16) for _ in range(2)]
    aTb = [k.buf("aT") for _ in range(2)]
    r = [k.carve([512], F32) for _ in range(2)]
    rb = [k.buf("r") for _ in range(2)]

    emit_norm_T(k, range(NT), k.lay["norm_mlp"] + 8 * l, lambda i: (hT[:, :, i * 128:(i + 1) * 128], hTb[i // 4]), tmp)

    wi_d = k.dram["mlp_wi"]
    wo_d = k.dram["mlp_wo"]
    k.dump("hT", hT, [128, 8, S], BF16, hTb)

    def load(g):
        sl = g % 2
        k.dma("pool", wi[sl], wi_d[l, :, g * 512:(g + 1) * 512].rearrange("(kc p) f -> p kc f", p=128),
              f"mwi{sl}", writes=[wib[sl]])
        k.dma("pool", wo[sl], wo_d[l, g * 512:(g + 1) * 512, :].rearrange("(fc p) d -> p fc d", p=128),
              f"mwo{sl}", writes=[wob[sl]])

    steps = [(g, tb) for g in range(8) for tb in range(4)]
    abank = [0, 1, 2, 3]
    ybank = [4, 5]
    state = dict(na=0, nr=0)

    def stage1(si):
        g, tb = steps[si]
        sl = g % 2
        a = si % 2
        for m in range(4):
            b = abank[state["na"] % 4]
            state["na"] += 1
            for kc in range(8):
                k.mm(k.ps[b][:], wi[sl][:, kc, m * 128:(m + 1) * 128], hT[:, kc, tb * 512:(tb + 1) * 512],
                     kc == 0, kc == 7, [wib[sl], hTb[tb]], [k.psb[b]])
            rr = state["nr"] % 2
            state["nr"] += 1
            k.act(r[rr], k.ps[b][:], AF.Relu, [k.psb[b]], [rb[rr]])
            k.act(aT[a][:, m, :], r[rr], AF.Square, [rb[rr]], [aTb[a]])

    def stage2(si):
        g, tb = steps[si]
        sl = g % 2
        a = si % 2
        for tt in range(4):
            i = tb * 4 + tt
            for half in range(2):
                b = ybank[half]
                for m in range(4):
                    k.mm(k.ps[b][:], aT[a][:, m, tt * 128:(tt + 1) * 128], wo[sl][:, m, half * 512:(half + 1) * 512],
                         m == 0, m == 3, [aTb[a], wob[sl]], [k.psb[b]])
                xs = k.x[:, i, half * 512:(half + 1) * 512]
                k.tt("dve", xs, xs, k.ps[b][:], ALU.add, [k.xb[i], k.psb[b]], [k.xb[i]])

    load(0)
    k.dump("wi0", wi[0], [128, 8, 512], BF16, [wib[0]])
    k.dump("wo0", wo[0], [128, 4, D], BF16, [wob[0]])
    for si in range(len(steps) + 1):
        if si < len(steps):
            stage1(si)
        if si >= 1:
            stage2(si - 1)
        if si < len(steps):
            g, tb = steps[si]
            if tb == 0 and g + 1 < 8:
                load(g + 1)


def emit_rope_tables(k, s):
    posi = k.rope_posi[:]
    k.dma("sp", posi, k.dram["pos"][s], "pos", writes=[k.ropeb])
    k.cp("dve", k.rope_posf[:], posi, [k.ropeb], [k.ropeb])
    k.arena_reset()
    ang = k.carve([NT, 32], F32)
    k.tt("dve", ang, k.rope_posf[:].unsqueeze(2).to_broadcast([128, NT, 32]),
         k.rope_invf[:].unsqueeze(1).to_broadcast([128, NT, 32]), ALU.mult, [k.ropeb, k.cb], [k.ropeb])
    r1 = k.carve([NT, 32], F32)
    ki = k.carve([NT, 32], I32)
    kf = k.carve([NT, 32], F32)
    sc = 1.0 - 1e-6
    rb = [k.ropeb]
    two_pi = 2 * math.pi

    def reduce_to_pi(shift):
        k.ts("dve", kf, ang, shift, 1.0 / two_pi, ALU.add, ALU.mult, rb, rb)
        k.cp("dve", ki, kf, rb, rb)
        k.cp("dve", kf, ki, rb, rb)
        k.stt("dve", r1, kf, -two_pi, ang, ALU.mult, ALU.add, rb, rb)
        if shift != 0.0:
            k.ts("dve", r1, r1, shift, None, ALU.add, None, rb, rb)
        k.ts("dve", kf, r1, math.pi, two_pi, ALU.is_gt, ALU.mult, rb, rb)
        k.tt("dve", r1, r1, kf, ALU.subtract, rb, rb)
        k.ts("dve", kf, r1, -math.pi, two_pi, ALU.is_lt, ALU.mult, rb, rb)
        k.tt("dve", r1, r1, kf, ALU.add, rb, rb)

    reduce_to_pi(0.0)
    k.act(k.sinA[:, :, 32:64], r1, AF.Sin, rb, rb, scale=sc)
    k.ts("dve", k.sinA[:, :, 0:32], k.sinA[:, :, 32:64], -1.0, None, ALU.mult, None, rb, rb)
    reduce_to_pi(math.pi / 2)
    k.act(k.cos2[:, :, 0:32], r1, AF.Sin, rb, rb, scale=sc)
    k.cp("dve", k.cos2[:, :, 32:64], k.cos2[:, :, 0:32], rb, rb)


def emit_rope(k, out_bf, t, tmp, o, i, nh, reads, writes, tb):
    v3 = lambda ap: ap.rearrange("p (h d) -> p h d", d=64)
    cosb = k.cos2[:, i, :].unsqueeze(1).to_broadcast([128, nh, 64])
    sa = k.sinA[:, i, :]
    s_lo = sa[:, 0:32].unsqueeze(1).to_broadcast([128, nh, 32])
    s_hi = sa[:, 32:64].unsqueeze(1).to_broadcast([128, nh, 32])
    k.tt("dve", v3(tmp)[:, :, 0:32], v3(t)[:, :, 32:64], s_lo, ALU.mult, reads + [k.ropeb], [tb])
    k.tt("dve", v3(tmp)[:, :, 32:64], v3(t)[:, :, 0:32], s_hi, ALU.mult, reads + [k.ropeb], [tb])
    k.tt("dve", v3(o), v3(t), cosb, ALU.mult, reads + [k.ropeb], [tb])
    k.tt("dve", out_bf, o, tmp, ALU.add, [tb], writes)


def emit_mla(k, l, j):
    P = k.P
    lay = k.lay
    if k.stop == "rope":
        return
    k.arena_reset()
    ps, psb = k.ps, k.psb
    cT = k.carve([5, S], BF16)
    cTb = [k.buf("cT") for _ in range(NT)]
    krT = k.carve([S], BF16)
    krTb = [k.buf("krT") for _ in range(NT)]
    mark = k.aoff
    wd = k.carve([8, 704], BF16)
    wdb = k.buf("wd")
    k.dma("pool", wd, k.dram["mla_wd"][j].rearrange("(kc p) f -> p kc f", p=128), "mla_wd", writes=[wdb])
    tmp = norm_tmp(k, [6, 7])
    hTt = [k.carve([8, 128], BF16) for _ in range(2)]
    hTtb = [k.buf("hTt") for _ in range(2)]
    cqn = k.carve([640], BF16); cqnb = k.buf("cqn")
    junkA = k.carve([384], BF16); junkAb = k.buf("junkA")
    st2 = k.carve([16], F32); st2b = k.buf("st2")
    krf = k.carve([64], F32); krfb = k.buf("krf")
    rt = k.carve([64], F32); ro = k.carve([64], F32); rtb = k.buf("rt")
    krb = k.carve([128], BF16); krbb = k.buf("krb")
    gl = lay["mla_lat"] + 5 * j
    gt = lay["mla_head"] + 384 * j
    cnt = [0]

    def passA(i):
        sl = cnt[0] % 2
        cnt[0] += 1
        h, hb = hTt[sl], hTtb[sl]
        for kc in range(8):
            k.mm(ps[0][:, 0:384], h[:, kc, :], wd[:, kc, 0:384], kc == 0, kc == 7, [hb, wdb], [psb[0]])
        for kc in range(8):
            k.mm(ps[1][:, 0:320], h[:, kc, :], wd[:, kc, 384:704], kc == 0, kc == 7, [hb, wdb], [psb[1]])
        k.act(junkA[:, 0:384], ps[0][:, 0:384], AF.Square, [psb[0]], [junkAb, st2b], accum_out=st2[:, 0:1])
        k.act(junkA[:, 0:256], ps[1][:, 0:256], AF.Square, [psb[1]], [junkAb, st2b], accum_out=st2[:, 1:2])
        k.act(junkA[:, 0:64], ps[1][:, 256:320], AF.Square, [psb[1]], [junkAb, st2b], accum_out=st2[:, 2:3])
        for c, n in enumerate((384, 256, 64)):
            k.act(st2[:, 3 + c:4 + c], st2[:, c:c + 1], AF.Sqrt, [st2b], [st2b], bias=EPS, scale=1.0 / n)
        k.recip(st2[:, 6:9], st2[:, 3:6], [st2b], [st2b])
        if k.stop == "A1":
            return
        k.act(cqn[:, 0:384], ps[0][:, 0:384], AF.Copy, [psb[0], st2b], [cqnb], scale=st2[:, 6:7])
        k.act(cqn[:, 384:640], ps[1][:, 0:256], AF.Copy, [psb[1], st2b], [cqnb], scale=st2[:, 7:8])
        if k.stop == "A2":
            return
        k.stt("dve", krf, ps[1][:, 256:320], st2[:, 8:9], k.pt[:, gt + 320:gt + 384], ALU.mult, ALU.mult,
              [psb[1], st2b, k.ptb], [krfb])
        emit_rope(k, krb[:, 0:64], krf, rt, ro, i, 1, [krfb], [krbb], rtb)
        k.cp("dve", krb[:, 64:128], krb[:, 0:64], [krbb], [krbb])
        if k.stop == "A3":
            return
        pbf = ps[2][:].bitcast(BF16)
        for c in range(5):
            k.tr(pbf[:, c * 128:(c + 1) * 128], cqn[:, c * 128:(c + 1) * 128], k.ident[:], [cqnb, k.cb], [psb[2]])
        k.tr(pbf[:, 640:768], krb, k.ident[:], [krbb, k.cb], [psb[2]])
        g = k.pf[:, gl:gl + 5]
        k.tt("dve", cT[:, :, i * 128:(i + 1) * 128], pbf[:, 0:640].rearrange("p (c t) -> p c t", t=128),
             g.unsqueeze(2).to_broadcast([128, 5, 128]), ALU.mult, [psb[2], k.pfb], [cTb[i]])
        if k.stop == "A4":
            return
        k.cp("dve", krT[:, i * 128:(i + 1) * 128], pbf[:, 640:768], [psb[2]], [krTb[i]])

    emit_norm_T(k, range(NT), lay["norm_mix"] + 8 * l, lambda i: (hTt[cnt[0] % 2], hTtb[cnt[0] % 2]), tmp, after=passA)
    k.dump("cT", cT, [128, 5, S], BF16, cTb)
    k.dump("krT", krT, [128, S], BF16, krTb)

    if k.stop in ("passA", "A1", "A2", "A3", "A4"):
        return
    k.arena_mark_reset(mark)
    wuq = [k.carve([3, 768], BF16) for _ in range(2)]
    wukv = [k.carve([2, 1024], BF16) for _ in range(2)]
    wo = k.carve([4, D], BF16)
    wuqb = [k.buf("wuq") for _ in range(2)]
    wukvb = [k.buf("wukv") for _ in range(2)]
    wob = k.buf("wo")
    kT = k.carve([4, S], BF16); kTb = [k.buf("kT") for _ in range(NT)]
    v = k.carve([NT, 512], BF16); vb = [k.buf("v") for _ in range(NT)]
    qTn = [k.carve([4, 512], BF16) for _ in range(2)]; qTnb = [k.buf("qTn") for _ in range(2)]
    qTr = [k.carve([2, 512], BF16) for _ in range(2)]; qTrb = [k.buf("qTr") for _ in range(2)]
    oT = k.carve([4, 512], BF16); oTb = k.buf("oT")
    pT = [k.carve([512], BF16) for _ in range(3)]; pTb = [k.buf("pT") for _ in range(3)]
    rden = k.carve([512], F32); rdenb = k.buf("rden")
    sq1 = k.carve([512], F32); sq1b = k.buf("sq1")
    sq2 = k.carve([256], F32); sq2b = k.buf("sq2")
    sq3 = k.carve([512], F32); sq3b = k.buf("sq3")
    ssq = k.carve([48], F32); ssqb = k.buf("ssq")
    tq = k.carve([512], F32); tqb = k.buf("tq")
    tr_ = k.carve([256], F32); trb = k.buf("tr")
    trt = k.carve([256], F32); tro = k.carve([256], F32); trtb = k.buf("trt")
    t3 = k.carve([512], F32); t3b = k.buf("t3")
    qnb_ = k.carve([512], BF16); qnbb = k.buf("qnb")
    qrb_ = k.carve([256], BF16); qrbb = k.buf("qrb")
    knb_ = k.carve([512], BF16); knbb = k.buf("knb")
    h4 = lambda ap, d: ap.rearrange("p (h d) -> p h d", d=d)
    scale = 192.0 ** -0.5

    def load_group(G):
        sl = G % 2
        k.dma("pool", wuq[sl], k.dram["mla_wuq"][j, G].rearrange("(kc p) f -> p kc f", p=128), f"wuq{sl}", writes=[wuqb[sl]])
        k.dma("pool", wukv[sl], k.dram["mla_wukv"][j, G].rearrange("(kc p) f -> p kc f", p=128), f"wukv{sl}", writes=[wukvb[sl]])

    def load_wo(G):
        k.dma("pool", wo, k.dram["mla_wo"][j, G * 512:(G + 1) * 512, :].rearrange("(h p) d -> p h d", p=128), "mla_wo", writes=[wob])

    def tile_proj(G, qb, i):
        sl = G % 2
        tt = i - 4 * qb
        qs = qb % 2
        csl = slice(i * 128, (i + 1) * 128)
        for kc in range(3):
            k.mm(ps[3][:], cT[:, kc, csl], wuq[sl][:, kc, 0:512], kc == 0, kc == 2, [cTb[i], wuqb[sl]], [psb[3]])
        for kc in range(3):
            k.mm(ps[4][:, 0:256], cT[:, kc, csl], wuq[sl][:, kc, 512:768], kc == 0, kc == 2, [cTb[i], wuqb[sl]], [psb[4]])
        for kc in range(2):
            k.mm(ps[5][:], cT[:, 3 + kc, csl], wukv[sl][:, kc, 0:512], kc == 0, kc == 1, [cTb[i], wukvb[sl]], [psb[5]])
        for kc in range(2):
            k.mm(ps[0][:], cT[:, 3 + kc, csl], wukv[sl][:, kc, 512:1024], kc == 0, kc == 1, [cTb[i], wukvb[sl]], [psb[0]])
        k.act(v[:, i, :], ps[0][:], AF.Copy, [psb[0]], [vb[i]])
        k.act(sq1, ps[3][:], AF.Square, [psb[3]], [sq1b])
        k.act(sq2, ps[4][:, 0:256], AF.Square, [psb[4]], [sq2b])
        k.act(sq3, ps[5][:], AF.Square, [psb[5]], [sq3b])
        k.red(ssq[:, 0:4], h4(sq1, 128), [sq1b], [ssqb])
        k.red(ssq[:, 4:8], h4(sq2, 64), [sq2b], [ssqb])
        k.red(ssq[:, 8:12], h4(sq3, 128), [sq3b], [ssqb])
        k.tt("dve", ssq[:, 12:24], ssq[:, 0:12], k.invn12[:], ALU.mult, [ssqb, k.cb], [ssqb])
        k.act(ssq[:, 24:36], ssq[:, 12:24], AF.Sqrt, [ssqb], [ssqb], bias=EPS, scale=1.0)
        k.recip(ssq[:, 36:48], ssq[:, 24:36], [ssqb], [ssqb])
        rs = ssq[:, 36:48]
        k.tt("dve", h4(tq, 128), h4(ps[3][:], 128), rs[:, 0:4].unsqueeze(2).to_broadcast([128, 4, 128]), ALU.mult,
             [psb[3], ssqb], [tqb])
        k.tt("pool", h4(qnb_, 128), h4(tq, 128), k.pt[:, gt:gt + 128].unsqueeze(1).to_broadcast([128, 4, 128]), ALU.mult,
             [tqb, k.ptb], [qnbb])
        k.tt("dve", h4(tr_, 64), h4(ps[4][:, 0:256], 64), rs[:, 4:8].unsqueeze(2).to_broadcast([128, 4, 64]), ALU.mult,
             [psb[4], ssqb], [trb])
        k.tt("pool", h4(tr_, 64), h4(tr_, 64), k.pt[:, gt + 128:gt + 192].unsqueeze(1).to_broadcast([128, 4, 64]), ALU.mult,
             [trb, k.ptb], [trb])
        emit_rope(k, qrb_, tr_, trt, tro, i, 4, [trb], [qrbb], trtb)
        k.tt("dve", h4(t3, 128), h4(ps[5][:], 128), rs[:, 8:12].unsqueeze(2).to_broadcast([128, 4, 128]), ALU.mult,
             [psb[5], ssqb], [t3b])
        k.tt("pool", h4(knb_, 128), h4(t3, 128), k.pt[:, gt + 192:gt + 320].unsqueeze(1).to_broadcast([128, 4, 128]), ALU.mult,
             [t3b, k.ptb], [knbb])
        p6 = ps[6][:].bitcast(BF16)
        for c in range(4):
            k.tr(p6[:, c * 128:(c + 1) * 128], qnb_[:, c * 128:(c + 1) * 128], k.ident[:], [qnbb, k.cb], [psb[6]])
        for c in range(2):
            k.tr(p6[:, 512 + c * 128:512 + (c + 1) * 128], qrb_[:, c * 128:(c + 1) * 128], k.ident[:], [qrbb, k.cb], [psb[6]])
        k.cp("dve", qTn[qs][:, :, tt * 128:(tt + 1) * 128], h4(p6[:, 0:512], 128), [psb[6]], [qTnb[qs]])
        k.cp("dve", qTr[qs][:, :, tt * 128:(tt + 1) * 128], h4(p6[:, 512:768], 128), [psb[6]], [qTrb[qs]])
        p7 = ps[7][:].bitcast(BF16)
        for c in range(4):
            k.tr(p7[:, c * 128:(c + 1) * 128], knb_[:, c * 128:(c + 1) * 128], k.ident[:], [knbb, k.cb], [psb[7]])
        k.cp("dve", kT[:, :, csl], h4(p7[:, 0:512], 128), [psb[7]], [kTb[i]])

    def attention(G, qb):
        qs = qb % 2
        nkt = 4 * qb + 4
        steps = [(hh, kt) for hh in range(4) for kt in range(nkt)]
        sbank = [2, 3, 4]
        acc = [(0, 1), (5, 6)]

        def qk(si):
            hh, kt = steps[si]
            blk, half = hh % 2, hh // 2
            jd = kt - 4 * qb
            c0 = max(0, jd) * 128
            b = sbank[si % 3]
            ks = slice(kt * 128, (kt + 1) * 128)
            k.mm(ps[b][:, 0:512 - c0], kT[:, hh, ks], qTn[qs][:, hh, c0:512], True, False, [kTb[kt], qTnb[qs]], [psb[b]])
            k.mm(ps[b][:, 0:512 - c0], krT[half * 64:(half + 1) * 64, ks], qTr[qs][half * 64:(half + 1) * 64, blk, c0:512],
                 False, True, [krTb[kt], qTrb[qs]], [psb[b]])
            pb_ = si % 3
            k.act(pT[pb_][:, 0:512 - c0], ps[b][:, 0:512 - c0], AF.Exp, [psb[b]], [pTb[pb_]], scale=scale)
            if jd >= 0:
                blkap = pT[pb_][:, 0:128]
                P.add("pool", lambda e: e.affine_select(out=blkap, in_=blkap, pattern=[[1, 128]], compare_op=ALU.is_ge,
                                                        fill=0.0, base=0, channel_multiplier=-1), [pTb[pb_]], [pTb[pb_]])

        def pv(si):
            hh, kt = steps[si]
            jd = kt - 4 * qb
            c0 = max(0, jd) * 128
            bo, bd = acc[hh % 2]
            pb_ = si % 3
            k.mm(ps[bo][:, c0:512], v[:, kt, hh * 128:(hh + 1) * 128], pT[pb_][:, 0:512 - c0], kt == 0, kt == nkt - 1,
                 [vb[kt], pTb[pb_]], [psb[bo]])
            k.mm(ps[bd][:, c0:512], k.ones_bf[:], pT[pb_][:, 0:512 - c0], kt == 0, kt == nkt - 1,
                 [k.cb, pTb[pb_]], [psb[bd]])
            if kt == nkt - 1:
                k.recip(rden, ps[bd][:], [psb[bd]], [rdenb])
                k.tt("dve", oT[:, hh, :], ps[bo][:], rden, ALU.mult, [psb[bo], rdenb], [oTb])

        for si in range(len(steps) + 2):
            if si < len(steps):
                qk(si)
            if si >= 2:
                pv(si - 2)

    def out_proj(G, qb):
        for tt in range(4):
            i = 4 * qb + tt
            for half in range(2):
                b = (7, 2)[half]
                for hh in range(4):
                    k.mm(ps[b][:], oT[:, hh, tt * 128:(tt + 1) * 128], wo[:, hh, half * 512:(half + 1) * 512],
                         hh == 0, hh == 3, [oTb, wob], [psb[b]])
                xs = k.x[:, i, half * 512:(half + 1) * 512]
                k.tt("dve", xs, xs, ps[b][:], ALU.add, [k.xb[i], psb[b]], [k.xb[i]])

    load_group(0)
    for G in range(2):
        load_wo(G)
        if G == 0:
            load_group(1)
        for qb in range(4):
            for i in range(4 * qb, 4 * qb + 4):
                tile_proj(G, qb, i)
            if k.stop == "proj":
                continue
            attention(G, qb)
            if k.stop == "attn":
                continue
            out_proj(G, qb)


def emit_conv(k, l):
    P = k.P
    lay = k.lay
    ps, psb = k.ps, k.psb
    k.arena_reset()
    w2 = k.carve([8, D], BF16); w2b = k.buf("w2")
    k.dma("pool", w2, k.dram["cv_w2"].rearrange("(cc p) d -> p cc d", p=128), "cv_w2", writes=[w2b])
    tmp = norm_tmp(k, [0, 1])
    hTt = [k.carve([8, 512], BF16) for _ in range(2)]; hTtb = [k.buf("hTt") for _ in range(2)]
    w1 = [k.carve([8, 2, 128], BF16) for _ in range(2)]; w1b = [k.buf("w1") for _ in range(2)]
    Dm = [k.carve([31, 128], BF16) for _ in range(2)]; Dmb = [k.buf("Dm") for _ in range(2)]
    uT = k.carve([8, 542], BF16); uTb = [k.buf("uT") for _ in range(8)]
    ysb = k.carve([8, 512], F32); ysbb = [k.buf("ysb") for _ in range(8)]
    ysq = [k.carve([512], F32) for _ in range(2)]; ysqb = [k.buf("ysq") for _ in range(2)]
    sig = [k.carve([512], F32) for _ in range(2)]; sigb = [k.buf("sig") for _ in range(2)]
    zT = k.carve([8, 512], BF16); zTb = [k.buf("zT") for _ in range(8)]
    mean = k.carve([512], F32); msq = k.carve([512], F32); rstd = k.carve([512], F32); stb = k.buf("cvst")
    tn = [k.carve([512], F32) for _ in range(2)]; tnb = [k.buf("tn") for _ in range(2)]
    cb1 = lay["cv_b1"]; cwd = lay["cv_wdw"]; cbd = lay["cv_bdw"]; cg = lay["cv_lng"]; cbn = lay["cv_lnb"]
    w1_d = k.dram["cv_w1"]

    def load_w1(n):
        cc = n % 8
        sl = n % 2
        k.dma("pool", w1[sl][:, :, 0, :], w1_d[:, cc * 128:(cc + 1) * 128].rearrange("(kc p) f -> p kc f", p=128),
              f"cvw1{sl}", writes=[w1b[sl]])
        k.dma("pool", w1[sl][:, :, 1, :], w1_d[:, D + cc * 128:D + (cc + 1) * 128].rearrange("(kc p) f -> p kc f", p=128),
              f"cvw1{sl}", writes=[w1b[sl]])

    load_w1(0)
    n = 0
    for tb in range(4):
        hs = tb % 2
        emit_norm_T(k, range(4 * tb, 4 * tb + 4), lay["norm_mix"] + 8 * l,
                    lambda i: (hTt[hs][:, :, (i % 4) * 128:(i % 4 + 1) * 128], hTtb[hs]), tmp)
        for i in range(4 * tb, 4 * tb + 4):
            k.tt("pool", k.x[:, i, :], k.x[:, i, :], k.pt[:, lay["cv_b2"]:lay["cv_b2"] + D], ALU.add,
                 [k.xb[i], k.ptb], [k.xb[i]])
        for cc in range(8):
            sl = n % 2
            if n + 1 < 32:
                load_w1(n + 1)
            n += 1
            wv = k.pf[:, cwd + cc * 31:cwd + (cc + 1) * 31]
            k.tt("pool", Dm[sl], k.identf[:].unsqueeze(1).to_broadcast([128, 31, 128]),
                 wv.unsqueeze(2).to_broadcast([128, 31, 128]), ALU.mult, [k.cb, k.pfb], [Dmb[sl]])
            ba, bg, bc = cc % 2, 2 + cc % 2, 4 + cc % 2
            for kc in range(8):
                k.mm(ps[ba][:], w1[sl][:, kc, 0, :], hTt[hs][:, kc, :], kc == 0, kc == 7, [w1b[sl], hTtb[hs]], [psb[ba]])
            for kc in range(8):
                k.mm(ps[bg][:], w1[sl][:, kc, 1, :], hTt[hs][:, kc, :], kc == 0, kc == 7, [w1b[sl], hTtb[hs]], [psb[bg]])
            ss_ = cc % 2
            k.act(sig[ss_], ps[bg][:], AF.Sigmoid, [psb[bg], k.pfb], [sigb[ss_]], bias=k.pf[:, cb1 + 8 + cc:cb1 + 9 + cc], scale=1.0)
            if tb == 0:
                k.memset("pool", uT[:, cc, 0:30], 0.0, [uTb[cc]])
            else:
                k.cp("pool", uT[:, cc, 0:30], uT[:, cc, 512:542], [uTb[cc]], [uTb[cc]])
            k.stt("dve", uT[:, cc, 30:542], ps[ba][:], k.pf[:, cb1 + cc:cb1 + cc + 1], sig[ss_], ALU.add, ALU.mult,
                  [psb[ba], sigb[ss_], k.pfb], [uTb[cc]])
            for jj in range(31):
                k.mm(ps[bc][:], Dm[sl][:, jj, :], uT[:, cc, jj:jj + 512], jj == 0, jj == 30, [Dmb[sl], uTb[cc]], [psb[bc]])
            bdw = k.pf[:, cbd + cc:cbd + cc + 1]
            k.act(ysb[:, cc, :], ps[bc][:], AF.Identity, [psb[bc], k.pfb], [ysbb[cc]], bias=bdw, scale=1.0)
            k.act(ysq[ss_], ps[bc][:], AF.Square, [psb[bc], k.pfb], [ysqb[ss_]], bias=bdw, scale=1.0)
            k.mm(ps[6][:], k.ones_f[:], ysb[:, cc, :], cc == 0, cc == 7, [k.cb, ysbb[cc]], [psb[6]])
            k.mm(ps[7][:], k.ones_f[:], ysq[ss_], cc == 0, cc == 7, [k.cb, ysqb[ss_]], [psb[7]])
        k.act(mean, ps[6][:], AF.Copy, [psb[6]], [stb], scale=1.0 / D)
        k.act(msq, ps[6][:], AF.Square, [psb[6]], [stb], scale=1.0 / D)
        k.stt("dve", rstd, ps[7][:], 1.0 / D, msq, ALU.mult, ALU.subtract, [psb[7], stb], [stb])
        k.act(rstd, rstd, AF.Sqrt, [stb], [stb], bias=EPS, scale=1.0)
        k.recip(rstd, rstd, [stb], [stb])
        for cc in range(8):
            ts_ = cc % 2
            k.tt("pool", tn[ts_], ysb[:, cc, :], mean, ALU.subtract, [ysbb[cc], stb], [tnb[ts_]])
            k.tt("dve", tn[ts_], tn[ts_], rstd, ALU.mult, [tnb[ts_], stb], [tnb[ts_]])
            k.act(zT[:, cc, :], tn[ts_], AF.Silu, [tnb[ts_], k.pfb], [zTb[cc]],
                  bias=k.pf[:, cbn + cc:cbn + cc + 1], scale=k.pf[:, cg + cc:cg + cc + 1])
        for tt in range(4):
            i = 4 * tb + tt
            for half in range(2):
                b = (2, 3)[half]
                for cc in range(8):
                    k.mm(ps[b][:], zT[:, cc, tt * 128:(tt + 1) * 128], w2[:, cc, half * 512:(half + 1) * 512],
                         cc == 0, cc == 7, [zTb[cc], w2b], [psb[b]])
                xs = k.x[:, i, half * 512:(half + 1) * 512]
                k.tt("dve", xs, xs, ps[b][:], ALU.add, [k.xb[i], psb[b]], [k.xb[i]])


def emit_hgrn_consts(k, l):
    lay = k.lay
    hgc = k.hgc
    cbs = [k.cb]
    lg = k.pf[:, lay["hg_lb"]:lay["hg_lb"] + 32]
    e = hgc[:, 0:32]
    k.act(e, lg, AF.Exp, [k.pfb], cbs)
    den = hgc[:, 32:40]
    k.tt("dve", den, e[:, 0:8], e[:, 8:16], ALU.add, cbs, cbs)
    k.tt("dve", den, den, e[:, 16:24], ALU.add, cbs, cbs)
    k.tt("dve", den, den, e[:, 24:32], ALU.add, cbs, cbs)
    k.recip(den, den, cbs, cbs)
    num = hgc[:, 40:48]
    k.memset("dve", num, 0.0, cbs)
    for i in range(1, l + 1):
        k.tt("dve", num, num, e[:, 8 * i:8 * i + 8], ALU.add, cbs, cbs)
    k.tt("dve", hgc[:, 48:56], num, den, ALU.mult, cbs, cbs)
    k.ts("dve", hgc[:, 56:64], hgc[:, 48:56], -1.0, 1.0, ALU.mult, ALU.add, cbs, cbs)
    k.ts("dve", hgc[:, 64:72], hgc[:, 56:64], -1.0, None, ALU.mult, None, cbs, cbs)
    k.memset("dve", k.scanmask[:], 1.0, cbs)
    k.memset("dve", k.scanmask[:].rearrange("p (c t) -> p c t", t=64)[:, :, 0:1], 0.0, cbs)
    k.memset("pool", k.mask2[:], 1.0, cbs)
    k.P.add("pool", lambda e_: e_.affine_select(out=k.mask2[:], in_=k.mask2[:], pattern=[[1, 128]], compare_op=ALU.is_ge,
                                                 fill=0.0, base=0, channel_multiplier=-1), cbs, cbs)
    k.memset("pool", k.mask2[0:64, 64:128], 0.0, cbs)


def emit_hgrn(k, l):
    P = k.P
    lay = k.lay
    ps, psb = k.ps, k.psb
    k.arena_reset()
    hT = k.carve([8, S], BF16); hTb = [k.buf("hT") for _ in range(4)]
    tmp = norm_tmp(k, [6, 7])
    Wp = [k.carve([8, 4, 256], BF16) for _ in range(2)]; Wpb = [k.buf("Wp") for _ in range(2)]
    wop = [k.carve([2, D], BF16) for _ in range(2)]; wopb = [k.buf("wop") for _ in range(2)]
    F = lambda: k.carve([512], F32)
    sg, f_, b_, bp, Ep, Em, gate, t1, t2, on = [F() for _ in range(10)]
    sgb, fb, bb, bpb, Epb, Emb, gateb, t1b, t2b, onb = [k.buf("hg") for _ in range(10)]
    kT_ = k.carve([512], BF16); kTb_ = k.buf("kcT")
    qT_ = k.carve([512], BF16); qTb_ = k.buf("qcT")
    vT_ = k.carve([512], BF16); vTb_ = k.buf("vT")
    ktm = k.carve([4, 128], BF16); ktmb = k.buf("ktm")
    vtm = k.carve([4, 128], BF16); vtmb = k.buf("vtm")
    onT = [k.carve([512], BF16) for _ in range(2)]; onTb = [k.buf("onT") for _ in range(2)]
    Am = [k.carve([128], BF16) for _ in range(2)]; Amb = [k.buf("Am") for _ in range(2)]
    Sst = [k.carve([128], F32) for _ in range(2)]; Sb = [k.buf("S") for _ in range(2)]
    Stil = k.carve([128], BF16); Stilb = k.buf("Stil")
    tmpS = k.carve([128], F32); tmpSb = k.buf("tmpS")
    em = k.carve([8], F32); el = k.carve([8], F32); emb = k.buf("em")
    hc = k.hgc
    w_d = k.dram["hg_wi"]
    wo_d = k.dram["hg_wo"]
    c8 = lambda ap: ap.rearrange("p (c t) -> p c t", t=64)

    emit_norm_T(k, range(NT), lay["norm_mix"] + 8 * l, lambda i: (hT[:, :, i * 128:(i + 1) * 128], hTb[i // 4]), tmp)

    def load(hp):
        sl = hp % 2
        for kind in range(4):
            k.dma("pool", Wp[sl][:, :, kind, :],
                  w_d[:, kind * D + hp * 256:kind * D + (hp + 1) * 256].rearrange("(kc p) f -> p kc f", p=128),
                  f"hgw{sl}", writes=[Wpb[sl]])
        k.dma("pool", wop[sl], wo_d[hp * 256:(hp + 1) * 256, :].rearrange("(hh p) d -> p hh d", p=128),
              f"hgwo{sl}", writes=[wopb[sl]])

    rot = [0]

    def proj(sl, kind, hh, tb, bank):
        for kc in range(8):
            k.mm(ps[bank][:], Wp[sl][:, kc, kind, hh * 128:(hh + 1) * 128], hT[:, kc, tb * 512:(tb + 1) * 512],
                 kc == 0, kc == 7, [Wpb[sl], hTb[tb]], [psb[bank]])

    load(0)
    for hp in range(4):
        sl = hp % 2
        if hp + 1 < 4:
            load(hp + 1)
        for hh in range(2):
            k.memset("pool", Sst[hh], 0.0, [Sb[hh]])
        for tb in range(4):
            for hh in range(2):
                hcol = 2 * hp + hh
                lb = hc[:, 48 + hcol:49 + hcol]
                oml = hc[:, 56 + hcol:57 + hcol]
                noml = hc[:, 64 + hcol:65 + hcol]
                bq = hh
                proj(sl, 0, hh, tb, bq)
                proj(sl, 1, hh, tb, 2)
                proj(sl, 2, hh, tb, 3)
                k.act(sg, ps[2][:], AF.Sigmoid, [psb[2]], [sgb])
                proj(sl, 3, hh, tb, 2)
                k.ts("dve", f_, sg, oml, lb, ALU.mult, ALU.add, [sgb, k.cb], [fb])
                k.act(f_, f_, AF.Ln, [fb], [fb])
                P.add("dve", lambda e: e.tensor_tensor_scan(out=b_, data0=k.scanmask[:], data1=f_, initial=0.0,
                                                            op0=ALU.mult, op1=ALU.add), [fb, k.cb], [bb])
                k.tt("dve", c8(bp), c8(b_), c8(b_)[:, :, 31:32].to_broadcast([128, 8, 64]), ALU.subtract, [bb], [bpb])
                k.act(Ep, bp, AF.Exp, [bpb], [Epb])
                k.act(Em, bp, AF.Exp, [bpb], [Emb], scale=-1.0)
                k.act(em, c8(b_)[:, :, 31], AF.Exp, [bb], [emb])
                k.act(el, c8(b_)[:, :, 63], AF.Exp, [bb], [emb])
                k.ts("dve", t1, sg, noml, oml, ALU.mult, ALU.add, [sgb, k.cb], [t1b])
                k.tt("pool", kT_, t1, Em, ALU.mult, [t1b, Emb], [kTb_])
                k.tt("dve", qT_, ps[bq][:], Ep, ALU.mult, [psb[bq], Epb], [qTb_])
                k.act(vT_, ps[3][:], AF.Copy, [psb[3]], [vTb_])
                k.act(gate, ps[2][:], AF.Silu, [psb[2]], [gateb])
                p6 = ps[6][:].bitcast(BF16)
                for tt in range(4):
                    k.tr(p6[:, tt * 128:(tt + 1) * 128], kT_[:, tt * 128:(tt + 1) * 128], k.ident[:], [kTb_, k.cb], [psb[6]])
                for tt in range(4):
                    k.tr(p6[:, 512 + tt * 128:512 + (tt + 1) * 128], vT_[:, tt * 128:(tt + 1) * 128], k.ident[:], [vTb_, k.cb], [psb[6]])
                k.cp("dve", ktm, p6[:, 0:512].rearrange("p (a b) -> p a b", b=128), [psb[6]], [ktmb])
                k.cp("dve", vtm, p6[:, 512:1024].rearrange("p (a b) -> p a b", b=128), [psb[6]], [vtmb])
                e2 = c8(Ep)[:, :, 63]
                for tt in range(4):
                    tsl = slice(tt * 128, (tt + 1) * 128)
                    a = tt % 2
                    k.mm(ps[4][:, 0:128], kT_[:, tsl], qT_[:, tsl], True, True, [kTb_, qTb_], [psb[4]])
                    k.tt("dve", Am[a], ps[4][:, 0:128], k.mask2[:], ALU.mult, [psb[4], k.cb], [Amb[a]])
                    k.mm(ps[7][:, tsl], vtm[:, tt, :], Am[a], True, False, [vtmb, Amb[a]], [psb[7]])
                    for half in range(2):
                        c = 2 * tt + half
                        hs = slice(half * 64, (half + 1) * 64)
                        csl = slice(tt * 128 + half * 64, tt * 128 + (half + 1) * 64)
                        k.act(Stil, Sst[hh], AF.Copy, [Sb[hh], emb], [Stilb], scale=em[:, c:c + 1])
                        k.mm(ps[7][:, csl], Stil, qT_[:, csl], False, half == 1, [Stilb, qTb_], [psb[7]])
                        k.mm(ps[5][:, 0:128], ktm[hs, tt, :], vtm[hs, tt, :], True, True, [ktmb, vtmb], [psb[5]])
                        k.ts("dve", tmpS, ps[5][:, 0:128], e2[:, c:c + 1], None, ALU.mult, None, [psb[5], Epb], [tmpSb])
                        k.stt("dve", Sst[hh], Sst[hh], el[:, c:c + 1], tmpS, ALU.mult, ALU.add, [Sb[hh], emb, tmpSb], [Sb[hh]])
                k.act(t1, ps[7][:], AF.Square, [psb[7]], [t1b])
                k.mm(ps[4][:], k.ones_f[:], t1, True, True, [k.cb, t1b], [psb[4]])
                k.act(t2, ps[4][:], AF.Sqrt, [psb[4]], [t2b], bias=EPS, scale=1.0 / 128)
                k.recip(t2, t2, [t2b], [t2b])
                go = k.pf[:, lay["hg_on"]:lay["hg_on"] + 1]
                k.stt("dve", on, ps[7][:], go, t2, ALU.mult, ALU.mult, [psb[7], t2b, k.pfb], [onb])
                k.tt("pool", onT[hh], on, gate, ALU.mult, [onb, gateb], [onTb[hh]])
            for tt in range(4):
                i = 4 * tb + tt
                for half in range(2):
                    bnk = (2, 3)[half]
                    for hh in range(2):
                        k.mm(ps[bnk][:], onT[hh][:, tt * 128:(tt + 1) * 128], wop[sl][:, hh, half * 512:(half + 1) * 512],
                             hh == 0, hh == 1, [onTb[hh], wopb[sl]], [psb[bnk]])
                    xs = k.x[:, i, half * 512:(half + 1) * 512]
                    k.tt("dve", xs, xs, ps[bnk][:], ALU.add, [k.xb[i], psb[bnk]], [k.xb[i]])


def fm(v):
    v = np.asarray(v, np.float32)
    return np.ascontiguousarray(v.reshape(-1, 128).T)


def pack_inputs(inp):
    cols = []
    lay = {}

    def put(name, arr):
        lay[name] = sum(c.shape[1] for c in cols)
        cols.append(np.asarray(arr, np.float32))

    put("norm_mix", np.concatenate([fm(inp["norm_mix"][l]) for l in range(4)], axis=1))
    put("norm_mlp", np.concatenate([fm(inp["norm_mlp"][l]) for l in range(4)], axis=1))
    put("mla_lat", np.concatenate([np.concatenate([fm(inp["mla_q_lat_norm"][j]), fm(inp["mla_kv_lat_norm"][j])], axis=1)
                                   for j in range(2)], axis=1))
    put("hg_lb", np.concatenate([fm(inp["hg_lb_logits"][i]) for i in range(4)], axis=1))
    put("hg_on", fm(inp["hg_out_norm"][0]))
    put("cv_b1", fm(inp["cv_b_pw1"][0]))
    put("cv_wdw", np.asarray(inp["cv_w_dw"][0], np.float32).T.reshape(8, 128, 31).transpose(1, 0, 2).reshape(128, 8 * 31))
    put("cv_bdw", fm(inp["cv_b_dw"][0]))
    put("cv_lng", fm(inp["cv_ln_g"][0]))
    put("cv_lnb", fm(inp["cv_ln_b"][0]))
    pf = np.ascontiguousarray(np.concatenate(cols, axis=1))
    lay["npf"] = pf.shape[1]
    tcols = []

    def putt(name, vec):
        lay[name] = sum(c.shape[0] for c in tcols)
        tcols.append(np.asarray(vec, np.float32).reshape(-1))

    putt("mla_head", np.concatenate([np.concatenate([inp["mla_q_head_norm"][j], inp["mla_k_head_norm"][j]]) for j in range(2)]))
    putt("cv_b2", inp["cv_b_pw2"][0])
    ptv = np.concatenate(tcols)
    pt = np.ascontiguousarray(np.broadcast_to(ptv[None, :], (128, ptv.shape[0])))
    lay["npt"] = pt.shape[1]
    return pf, pt, lay


def pack_weights(inp):
    w = {}
    w["mlp_wi"] = np.asarray(inp["mlp_w_in"], np.float32)
    w["mlp_wo"] = np.asarray(inp["mlp_w_out"], np.float32)
    w["mla_wd"] = np.asarray(inp["mla_w_down"], np.float32)
    wuq = np.asarray(inp["mla_w_uq"], np.float32).reshape(2, 384, 8, 192)
    wukv = np.asarray(inp["mla_w_ukv"], np.float32).reshape(2, 256, 8, 256)
    uq = np.empty((2, 2, 384, 768), np.float32)
    ukv = np.empty((2, 2, 256, 1024), np.float32)
    for G in range(2):
        hs = [4 * G + i for i in range(4)]
        uq[:, G, :, 0:512] = wuq[:, :, hs, 0:128].reshape(2, 384, 512)
        rope_order = [4 * G + 0, 4 * G + 2, 4 * G + 1, 4 * G + 3]
        uq[:, G, :, 512:768] = wuq[:, :, rope_order, 128:192].reshape(2, 384, 256)
        ukv[:, G, :, 0:512] = wukv[:, :, hs, 0:128].reshape(2, 256, 512)
        ukv[:, G, :, 512:1024] = wukv[:, :, hs, 128:256].reshape(2, 256, 512)
    w["mla_wuq"] = uq
    w["mla_wukv"] = ukv
    w["mla_wo"] = np.asarray(inp["mla_w_o"], np.float32)
    w["cv_w1"] = np.asarray(inp["cv_w_pw1"][0], np.float32)
    w["cv_w2"] = np.asarray(inp["cv_w_pw2"][0], np.float32)
    w["hg_wi"] = np.asarray(inp["hg_w_in"][0], np.float32)
    w["hg_wo"] = np.asarray(inp["hg_w_o"][0], np.float32)
    return w


ALL_LAYERS = [("mla", 0, 0), ("mlp", 0, 0), ("hgrn", 1, 0), ("mlp", 1, 0),
              ("conv", 2, 0), ("mlp", 2, 0), ("mla", 3, 1), ("mlp", 3, 0)]


def run(inp, layers, n_cores=N_CORES, n_seq=2, trace=False, debug=False, stop=None):
    pf, pt, lay = pack_inputs(inp)
    nc, stats = build_program(n_seq, layers, lay, debug, stop)
    x = np.asarray(inp["x"], np.float32)
    pos = np.asarray(inp["positions"], np.int32)
    wts = pack_weights(inp)
    in_maps = []
    for c in range(n_cores):
        sl = slice(c * n_seq, (c + 1) * n_seq)
        m = dict(
            x=np.ascontiguousarray(x[sl]),
            pos=np.ascontiguousarray(pos[sl].reshape(n_seq, NT, 128).transpose(0, 2, 1)),
            pf=pf, pt=pt, **wts,
        )
        in_maps.append(m)
    res = run_bass_kernel_spmd(nc, in_maps, core_ids=list(range(n_cores)), **({"trace": True} if trace else {}))
    out = np.concatenate([r["out"] for r in res.results], axis=0)
    return out, res, stats


def kernel(**inputs):
    out, _, _ = run(inputs, ALL_LAYERS)
    return out.astype(np.float32)
```

```python
import math
import numpy as np
from contextlib import ExitStack
from functools import partial

import concourse.bass as bass
import concourse.mybir as mybir
from concourse.bass_utils import run_bass_kernel_spmd

F32 = mybir.dt.float32
BF16 = mybir.dt.bfloat16
I32 = mybir.dt.int32
AF = mybir.ActivationFunctionType
ALU = mybir.AluOpType
AX = mybir.AxisListType

S = 2048
D = 1024
NT = 16
DFF = 4096
EPS = 1e-6
N_CORES = 8
ENGS = ("pe", "act", "dve", "pool", "sp")


class Buf:
    __slots__ = ("name", "last_w", "readers")

    def __init__(self, name):
        self.name = name
        self.last_w = None
        self.readers = []


class Prog:
    def __init__(self, nc):
        self.nc = nc
        self.ins = []
        self.last_on_eng = {e: None for e in ENGS}
        self.last_dma = {}
        self.pending_fence = {e: None for e in ENGS}

    def add(self, eng, fn, reads=(), writes=(), dma=None):
        i = len(self.ins)
        deps = set()
        for b in reads:
            if b.last_w is not None:
                deps.add(b.last_w)
        for b in writes:
            if b.last_w is not None:
                deps.add(b.last_w)
            deps.update(b.readers)
        if self.pending_fence[eng] is not None:
            deps |= self.pending_fence[eng]
            self.pending_fence[eng] = None
        self.ins.append(dict(eng=eng, fn=fn, deps=deps, dma=dma, sig=False))
        for b in reads:
            b.readers.append(i)
        for b in writes:
            b.last_w = i
            b.readers = []
        self.last_on_eng[eng] = i
        if dma is not None:
            self.last_dma[dma] = i
        return i

    def fence(self):
        s = set(v for v in self.last_on_eng.values() if v is not None)
        s |= set(self.last_dma.values())
        for e in ENGS:
            self.pending_fence[e] = set(s) | (self.pending_fence[e] or set())

    def emit(self, es, final_wait_groups=()):
        nc = self.nc
        ins = self.ins
        for r in ins:
            nd = set()
            for d in r["deps"]:
                p = ins[d]
                if p["dma"] is None and r["dma"] is None and p["eng"] == r["eng"] and r["eng"] == "pe":
                    continue
                nd.add(d)
            r["deps"] = nd
            for d in nd:
                ins[d]["sig"] = True
        eng_sem = {e: es.enter_context(nc.semaphore("s_" + e)) for e in ("pe", "act", "dve", "pool")}
        grp_sem = {}
        for r in ins:
            if r["dma"] is not None and r["dma"] not in grp_sem:
                grp_sem[r["dma"]] = es.enter_context(nc.semaphore("g_" + r["dma"]))
        cnt = {e: 0 for e in eng_sem}
        gcnt = {g: 0 for g in grp_sem}
        for r in ins:
            if r["dma"] is not None:
                gcnt[r["dma"]] += 16
                r["tok"] = ("g", r["dma"], gcnt[r["dma"]])
            elif r["sig"]:
                cnt[r["eng"]] += 1
                r["tok"] = ("e", r["eng"], cnt[r["eng"]])
        gtot = {g: 0 for g in grp_sem}
        per_eng = {e: [] for e in ENGS}
        known = {e: {} for e in ENGS}
        for r in ins:
            waits = {}
            for d in r["deps"]:
                kind, key, val = ins[d]["tok"]
                if kind == "g":
                    val = max(val, gtot[key])
                k = (kind, key)
                waits[k] = max(waits.get(k, 0), val)
            if r["dma"] is not None:
                gtot[r["dma"]] += 16
            kn = known[r["eng"]]
            wl = []
            for k, v in waits.items():
                if kn.get(k, 0) >= v:
                    continue
                kn[k] = v
                wl.append((k, v))
            per_eng[r["eng"]].append((r, wl))
        self.stats = dict(n={e: len(per_eng[e]) for e in ENGS}, sem=dict(cnt), nsem=len(grp_sem) + 4)

        def semof(k):
            return eng_sem[k[1]] if k[0] == "e" else grp_sem[k[1]]

        def run(engname, eobj):
            for r, wl in per_eng[engname]:
                for k, v in wl:
                    eobj.wait_ge(semof(k), v)
                bi = r["fn"](eobj)
                if r["dma"] is not None:
                    bi.then_inc(grp_sem[r["dma"]], 16)
                elif r["sig"]:
                    bi.then_inc(eng_sem[r["eng"]], 1)
            if engname == "sp":
                for g in final_wait_groups:
                    eobj.wait_ge(grp_sem[g], gcnt[g])

        with nc.Block() as block:
            @block.tensor
            def _(e):
                run("pe", e)

            @block.scalar
            def _(e):
                run("act", e)

            @block.vector
            def _(e):
                run("dve", e)

            @block.gpsimd
            def _(e):
                run("pool", e)

            @block.sync
            def _(e):
                run("sp", e)


class K:
    def __init__(self, nc, es, n_seq):
        self.nc = nc
        self.es = es
        self.P = Prog(nc)
        self.n_seq = n_seq
        self.uid = 0
        self.debug = False
        self.stop = None
        self.dbg_names = []

    def sb(self, name, shape, dt):
        return self.es.enter_context(self.nc.sbuf_tensor("sb_" + name, shape, dt))

    def mm(self, out, lhsT, rhs, start, stop, reads, writes):
        self.P.add("pe", lambda e: e.matmul(out, lhsT=lhsT, rhs=rhs, start=start, stop=stop), reads, writes)

    def tr(self, out, in_, ident, reads, writes):
        self.P.add("pe", lambda e: e.transpose(out=out, in_=in_, identity=ident), reads, writes)

    def act(self, out, in_, func, reads, writes, **kw):
        self.P.add("act", lambda e: e.activation(out=out, in_=in_, func=func, **kw), reads, writes)

    def tt(self, eng, out, in0, in1, op, reads, writes):
        self.P.add(eng, lambda e: e.tensor_tensor(out=out, in0=in0, in1=in1, op=op), reads, writes)

    def ts(self, eng, out, in0, s1, s2, op0, op1, reads, writes):
        if s2 is None:
            self.P.add(eng, lambda e: e.tensor_scalar(out=out, in0=in0, scalar1=s1, scalar2=None, op0=op0), reads, writes)
        else:
            self.P.add(eng, lambda e: e.tensor_scalar(out=out, in0=in0, scalar1=s1, scalar2=s2, op0=op0, op1=op1), reads, writes)

    def stt(self, eng, out, in0, scalar, in1, op0, op1, reads, writes):
        self.P.add(eng, lambda e: e.scalar_tensor_tensor(out=out, in0=in0, scalar=scalar, in1=in1, op0=op0, op1=op1), reads, writes)

    def cp(self, eng, out, in_, reads, writes):
        self.P.add(eng, lambda e: e.tensor_copy(out=out, in_=in_), reads, writes)

    def recip(self, out, in_, reads, writes):
        self.P.add("dve", lambda e: e.reciprocal(out=out, in_=in_), reads, writes)

    def memset(self, eng, ap, val, writes):
        self.P.add(eng, lambda e: e.memset(ap, val), (), writes)

    def dma(self, eng, out, in_, grp, reads=(), writes=()):
        self.P.add(eng, lambda e: e.dma_start(out=out, in_=in_), reads, writes, dma=grp)

    def arena_reset(self):
        self.P.fence()
        self.aoff = 0

    def carve(self, free_shape, dt):
        n = int(np.prod(free_shape))
        nbytes = n * (4 if dt in (F32, I32) else 2)
        nbytes = (nbytes + 63) // 64 * 64
        assert self.aoff + nbytes <= self.arena_bytes, (self.aoff, nbytes, self.arena_bytes)
        ap = self.arena[:, self.aoff // 2:(self.aoff + nbytes) // 2]
        self.aoff += nbytes
        if dt != BF16:
            ap = ap.bitcast(dt)
        ap = ap[:, 0:n]
        if len(free_shape) == 2:
            ap = ap.rearrange("p (a b) -> p a b", b=free_shape[1])
        elif len(free_shape) == 3:
            ap = ap.rearrange("p (a b c) -> p a b c", b=free_shape[1], c=free_shape[2])
        return ap

    def buf(self, name):
        self.uid += 1
        return Buf(f"{name}_{self.uid}")

    def arena_mark_reset(self, mark):
        self.P.fence()
        self.aoff = mark

    def red(self, out, in_, reads, writes):
        self.P.add("dve", lambda e: e.tensor_reduce(out=out, in_=in_, axis=AX.X, op=ALU.add), reads, writes)

    def dump(self, name, ap, shape, dt, reads):
        if not getattr(self, "debug", False) or ("dbg_" + name) in self.dbg_names:
            return
        d = self.nc.dram_tensor("dbg_" + name, list(shape), dt, kind="ExternalOutput").ap()
        self.dma("sp", d, ap, "dbg", reads=reads)
        self.dbg_names.append("dbg_" + name)


def build_program(n_seq, layers, lay, debug=False, stop=None):
    nc = bass.Bass("TRN2", target_bir_lowering=False)
    es = ExitStack()
    k = K(nc, es, n_seq)
    k.debug = debug
    k.stop = stop
    P = k.P
    dt = lambda name, shape, d, kind="ExternalInput": nc.dram_tensor(name, shape, d, kind=kind).ap()
    x_d = dt("x", [n_seq, S, D], F32)
    out_d = dt("out", [n_seq, S, D], F32, "ExternalOutput")
    pos_d = dt("pos", [n_seq, 128, NT], I32)
    pf_d = dt("pf", [128, lay["npf"]], F32)
    pt_d = dt("pt", [128, lay["npt"]], F32)
    mlp_wi_d = dt("mlp_wi", [4, D, DFF], F32)
    mlp_wo_d = dt("mlp_wo", [4, DFF, D], F32)
    k.dram = dict(x=x_d, out=out_d, pos=pos_d, pf=pf_d, pt=pt_d, mlp_wi=mlp_wi_d, mlp_wo=mlp_wo_d)
    k.dram["mla_wd"] = dt("mla_wd", [2, D, 704], F32)
    k.dram["mla_wuq"] = dt("mla_wuq", [2, 2, 384, 768], F32)
    k.dram["mla_wukv"] = dt("mla_wukv", [2, 2, 256, 1024], F32)
    k.dram["mla_wo"] = dt("mla_wo", [2, D, D], F32)
    k.dram["cv_w1"] = dt("cv_w1", [D, 2 * D], F32)
    k.dram["cv_w2"] = dt("cv_w2", [D, D], F32)
    k.dram["hg_wi"] = dt("hg_wi", [D, 4 * D], F32)
    k.dram["hg_wo"] = dt("hg_wo", [D, D], F32)
    k.lay = lay

    k.x = k.sb("x", [128, NT, D], F32)
    k.xb = [Buf(f"x{i}") for i in range(NT)]
    k.pf = k.sb("pf", [128, lay["npf"]], F32)
    k.pfb = Buf("pf")
    k.ident = k.sb("ident", [128, 128], BF16)
    k.identf = k.sb("identf", [128, 128], F32)
    k.cb = Buf("consts")
    k.ss = k.sb("ss", [128, 64], F32)
    k.ps = [es.enter_context(nc.psum_tensor(f"ps{i}", [128, 512], F32)) for i in range(8)]
    k.psb = [Buf(f"ps{i}") for i in range(8)]
    k.pt = k.sb("pt", [128, lay["npt"]], F32)
    k.ptb = Buf("pt")
    k.ones_bf = k.sb("ones_bf", [128, 128], BF16)
    k.invn12 = k.sb("invn12", [128, 12], F32)
    k.ones_f = k.sb("ones_f", [128, 128], F32)
    k.hgc = k.sb("hgc", [128, 72], F32)
    k.scanmask = k.sb("scanmask", [128, 512], F32)
    k.mask2 = k.sb("mask2", [128, 128], F32)
    k.negpi = k.sb("negpi", [128, 1], F32)
    k.rope_invf = k.sb("rope_invf", [128, 32], F32)
    k.rope_posi = k.sb("rope_posi", [128, NT], I32)
    k.rope_posf = k.sb("rope_posf", [128, NT], F32)
    k.cos2 = k.sb("cos2", [128, NT, 64], F32)
    k.sinA = k.sb("sinA", [128, NT, 64], F32)
    k.ropeb = Buf("rope")
    k.arena_bytes = 122 * 1024
    k.arena = k.sb("arena", [128, k.arena_bytes // 2], BF16)
    k.aoff = 0

    k.dma("sp", k.pf[:], pf_d, "pf", writes=[k.pfb])
    k.memset("pool", k.identf[:], 0.0, [k.cb])
    P.add("pool", lambda e: e.affine_select(out=k.identf[:], in_=k.identf[:], pattern=[[-1, 128]],
                                            compare_op=ALU.not_equal, fill=1.0, base=0, channel_multiplier=1),
          [k.cb], [k.cb])
    k.cp("dve", k.ident[:], k.identf[:], [k.cb], [k.cb])
    k.dma("sp", k.pt[:], pt_d, "pt", writes=[k.ptb])
    k.memset("dve", k.ones_bf[:], 1.0, [k.cb])
    k.memset("dve", k.ones_f[:], 1.0, [k.cb])
    k.memset("dve", k.invn12[:, 0:4], 1.0 / 128, [k.cb])
    k.memset("dve", k.invn12[:, 4:8], 1.0 / 64, [k.cb])
    k.memset("dve", k.invn12[:, 8:12], 1.0 / 128, [k.cb])
    k.memset("dve", k.negpi[:], -math.pi * (1.0 - 1e-6), [k.cb])
    for f in range(32):
        k.memset("pool", k.rope_invf[:, f:f + 1], float(np.float32(10000.0) ** np.float32(-2.0 * f / 64)), [k.cb])

    for (kind, l, j) in layers:
        if kind == "hgrn":
            emit_hgrn_consts(k, l)
    for s in range(n_seq):
        for i in range(NT):
            k.dma("sp", k.x[:, i, :], x_d[s, i * 128:(i + 1) * 128, :], "xio", writes=[k.xb[i]])
        if any(kd == "mla" for kd, _, _ in layers):
            emit_rope_tables(k, s)
        for (kind, l, j) in layers:
            if kind == "mlp":
                emit_mlp(k, l)
            elif kind == "mla":
                emit_mla(k, l, j)
            elif kind == "conv":
                emit_conv(k, l)
            elif kind == "hgrn":
                emit_hgrn(k, l)
            else:
                raise ValueError(kind)
        for i in range(NT):
            k.dma("sp", out_d[s, i * 128:(i + 1) * 128, :], k.x[:, i, :], "xio", reads=[k.xb[i]])
    P.emit(es, final_wait_groups=["xio"] + (["dbg"] if k.dbg_names else []))
    es.close()
    return nc, P.stats


def emit_norm_T(k, tiles, gcol, dst, tmp, after=None):
    for n, i in enumerate(tiles):
        slot = n % 2
        junk, jb = tmp["junk"][slot], tmp["junkb"][slot]
        xn, xnb = tmp["xn"][slot], tmp["xnb"][slot]
        st, stb = tmp["st"][slot], tmp["stb"][slot]
        pb = tmp["psum"][slot]
        k.act(junk, k.x[:, i, :], AF.Square, [k.xb[i]], [jb, stb], accum_out=st[:, 0:1])
        k.act(st[:, 1:2], st[:, 0:1], AF.Sqrt, [stb], [stb], bias=EPS, scale=1.0 / D)
        k.recip(st[:, 2:3], st[:, 1:2], [stb], [stb])
        k.act(xn, k.x[:, i, :], AF.Copy, [k.xb[i], stb], [xnb], scale=st[:, 2:3])
        pbf = k.ps[pb][:].bitcast(BF16)
        for c in range(8):
            k.tr(pbf[:, c * 128:(c + 1) * 128], xn[:, c * 128:(c + 1) * 128], k.ident[:], [xnb, k.cb], [k.psb[pb]])
        d_ap, d_buf = dst(i)
        g = k.pf[:, gcol:gcol + 8]
        k.tt("dve", d_ap, pbf.rearrange("p (c t) -> p c t", t=128),
             g.unsqueeze(2).to_broadcast([128, 8, 128]), ALU.mult, [k.psb[pb], k.pfb], [d_buf])
        if after is not None:
            after(i)


def norm_tmp(k, psum_banks):
    t = dict(junk=[], junkb=[], xn=[], xnb=[], st=[], stb=[], psum=psum_banks)
    for s in range(2):
        t["junk"].append(k.carve([D], BF16))
        t["junkb"].append(k.buf("junk"))
        t["xn"].append(k.carve([D], BF16))
        t["xnb"].append(k.buf("xn"))
        t["st"].append(k.carve([8], F32))
        t["stb"].append(k.buf("st"))
    return t


def emit_mlp(k, l):
    P = k.P
    k.arena_reset()
    hT = k.carve([8, S], BF16)
    hTb = [k.buf("hT") for _ in range(4)]
    tmp = norm_tmp(k, [6, 7])
    wi = [k.carve([8, 512], BF16) for _ in range(2)]
    wo = [k.carve([4, D], BF16) for _ in range(2)]
    wib = [k.buf("wi") for _ in range(2)]
    wob = [k.buf("wo") for _ in range(2)]
    aT = [k.carve([4, 512], BF16) for _ in range(2)]
    aTb = [k.buf("aT") for _ in range(2)]
    r = [k.carve([512], F32) for _ in range(2)]
    rb = [k.buf("r") for _ in range(2)]

    emit_norm_T(k, range(NT), k.lay["norm_mlp"] + 8 * l, lambda i: (hT[:, :, i * 128:(i + 1) * 128], hTb[i // 4]), tmp)

    wi_d = k.dram["mlp_wi"]
    wo_d = k.dram["mlp_wo"]
    k.dump("hT", hT, [128, 8, S], BF16, hTb)

    def load(g):
        sl = g % 2
        k.dma("pool", wi[sl], wi_d[l, :, g * 512:(g + 1) * 512].rearrange("(kc p) f -> p kc f", p=128),
              f"mwi{sl}", writes=[wib[sl]])
        k.dma("pool", wo[sl], wo_d[l, g * 512:(g + 1) * 512, :].rearrange("(fc p) d -> p fc d", p=128),
              f"mwo{sl}", writes=[wob[sl]])

    steps = [(g, tb) for g in range(8) for tb in range(4)]
    abank = [0, 1, 2, 3]
    ybank = [4, 5]
    state = dict(na=0, nr=0)

    def stage1(si):
        g, tb = steps[si]
        sl = g % 2
        a = si % 2
        for m in range(4):
            b = abank[state["na"] % 4]
            state["na"] += 1
            for kc in range(8):
                k.mm(k.ps[b][:], wi[sl][:, kc, m * 128:(m + 1) * 128], hT[:, kc, tb * 512:(tb + 1) * 512],
                     kc == 0, kc == 7, [wib[sl], hTb[tb]], [k.psb[b]])
            rr = state["nr"] % 2
            state["nr"] += 1
            k.act(r[rr], k.ps[b][:], AF.Relu, [k.psb[b]], [rb[rr]])
            k.act(aT[a][:, m, :], r[rr], AF.Square, [rb[rr]], [aTb[a]])

    def stage2(si):
        g, tb = steps[si]
        sl = g % 2
        a = si % 2
        for tt in range(4):
            i = tb * 4 + tt
            for half in range(2):
                b = ybank[half]
                for m in range(4):
                    k.mm(k.ps[b][:], aT[a][:, m, tt * 128:(tt + 1) * 128], wo[sl][:, m, half * 512:(half + 1) * 512],
                         m == 0, m == 3, [aTb[a], wob[sl]], [k.psb[b]])
                xs = k.x[:, i, half * 512:(half + 1) * 512]
                k.tt("dve", xs, xs, k.ps[b][:], ALU.add, [k.xb[i], k.psb[b]], [k.xb[i]])

    load(0)
    k.dump("wi0", wi[0], [128, 8, 512], BF16, [wib[0]])
    k.dump("wo0", wo[0], [128, 4, D], BF16, [wob[0]])
    for si in range(len(steps) + 1):
        if si < len(steps):
            stage1(si)
        if si >= 1:
            stage2(si - 1)
        if si < len(steps):
            g, tb = steps[si]
            if tb == 0 and g + 1 < 8:
                load(g + 1)


def emit_rope_tables(k, s):
    posi = k.rope_posi[:]
    k.dma("sp", posi, k.dram["pos"][s], "pos", writes=[k.ropeb])
    k.cp("dve", k.rope_posf[:], posi, [k.ropeb], [k.ropeb])
    k.arena_reset()
    ang = k.carve([NT, 32], F32)
    k.tt("dve", ang, k.rope_posf[:].unsqueeze(2).to_broadcast([128, NT, 32]),
         k.rope_invf[:].unsqueeze(1).to_broadcast([128, NT, 32]), ALU.mult, [k.ropeb, k.cb], [k.ropeb])
    r1 = k.carve([NT, 32], F32)
    ki = k.carve([NT, 32], I32)
    kf = k.carve([NT, 32], F32)
    sc = 1.0 - 1e-6
    rb = [k.ropeb]
    two_pi = 2 * math.pi

    def reduce_to_pi(shift):
        k.ts("dve", kf, ang, shift, 1.0 / two_pi, ALU.add, ALU.mult, rb, rb)
        k.cp("dve", ki, kf, rb, rb)
        k.cp("dve", kf, ki, rb, rb)
        k.stt("dve", r1, kf, -two_pi, ang, ALU.mult, ALU.add, rb, rb)
        if shift != 0.0:
            k.ts("dve", r1, r1, shift, None, ALU.add, None, rb, rb)
        k.ts("dve", kf, r1, math.pi, two_pi, ALU.is_gt, ALU.mult, rb, rb)
        k.tt("dve", r1, r1, kf, ALU.subtract, rb, rb)
        k.ts("dve", kf, r1, -math.pi, two_pi, ALU.is_lt, ALU.mult, rb, rb)
        k.tt("dve", r1, r1, kf, ALU.add, rb, rb)

    reduce_to_pi(0.0)
    k.act(k.sinA[:, :, 32:64], r1, AF.Sin, rb, rb, scale=sc)
    k.ts("dve", k.sinA[:, :, 0:32], k.sinA[:, :, 32:64], -1.0, None, ALU.mult, None, rb, rb)
    reduce_to_pi(math.pi / 2)
    k.act(k.cos2[:, :, 0:32], r1, AF.Sin, rb, rb, scale=sc)
    k.cp("dve", k.cos2[:, :, 32:64], k.cos2[:, :, 0:32], rb, rb)


def emit_rope(k, out_bf, t, tmp, o, i, nh, reads, writes, tb):
    v3 = lambda ap: ap.rearrange("p (h d) -> p h d", d=64)
    cosb = k.cos2[:, i, :].unsqueeze(1).to_broadcast([128, nh, 64])
    sa = k.sinA[:, i, :]
    s_lo = sa[:, 0:32].unsqueeze(1).to_broadcast([128, nh, 32])
    s_hi = sa[:, 32:64].unsqueeze(1).to_broadcast([128, nh, 32])
    k.tt("dve", v3(tmp)[:, :, 0:32], v3(t)[:, :, 32:64], s_lo, ALU.mult, reads + [k.ropeb], [tb])
    k.tt("dve", v3(tmp)[:, :, 32:64], v3(t)[:, :, 0:32], s_hi, ALU.mult, reads + [k.ropeb], [tb])
    k.tt("dve", v3(o), v3(t), cosb, ALU.mult, reads + [k.ropeb], [tb])
    k.tt("dve", out_bf, o, tmp, ALU.add, [tb], writes)


def emit_mla(k, l, j):
    P = k.P
    lay = k.lay
    if k.stop == "rope":
        return
    k.arena_reset()
    ps, psb = k.ps, k.psb
    cT = k.carve([5, S], BF16)
    cTb = [k.buf("cT") for _ in range(NT)]
    krT = k.carve([S], BF16)
    krTb = [k.buf("krT") for _ in range(NT)]
    mark = k.aoff
    wd = k.carve([8, 704], BF16)
    wdb = k.buf("wd")
    k.dma("pool", wd, k.dram["mla_wd"][j].rearrange("(kc p) f -> p kc f", p=128), "mla_wd", writes=[wdb])
    tmp = norm_tmp(k, [6, 7])
    hTt = [k.carve([8, 128], BF16) for _ in range(2)]
    hTtb = [k.buf("hTt") for _ in range(2)]
    cqn = k.carve([640], BF16); cqnb = k.buf("cqn")
    junkA = k.carve([384], BF16); junkAb = k.buf("junkA")
    st2 = k.carve([16], F32); st2b = k.buf("st2")
    krf = k.carve([64], F32); krfb = k.buf("krf")
    rt = k.carve([64], F32); ro = k.carve([64], F32); rtb = k.buf("rt")
    krb = k.carve([128], BF16); krbb = k.buf("krb")
    gl = lay["mla_lat"] + 5 * j
    gt = lay["mla_head"] + 384 * j
    cnt = [0]

    def passA(i):
        sl = cnt[0] % 2
        cnt[0] += 1
        h, hb = hTt[sl], hTtb[sl]
        for kc in range(8):
            k.mm(ps[0][:, 0:384], h[:, kc, :], wd[:, kc, 0:384], kc == 0, kc == 7, [hb, wdb], [psb[0]])
        for kc in range(8):
            k.mm(ps[1][:, 0:320], h[:, kc, :], wd[:, kc, 384:704], kc == 0, kc == 7, [hb, wdb], [psb[1]])
        k.act(junkA[:, 0:384], ps[0][:, 0:384], AF.Square, [psb[0]], [junkAb, st2b], accum_out=st2[:, 0:1])
        k.act(junkA[:, 0:256], ps[1][:, 0:256], AF.Square, [psb[1]], [junkAb, st2b], accum_out=st2[:, 1:2])
        k.act(junkA[:, 0:64], ps[1][:, 256:320], AF.Square, [psb[1]], [junkAb, st2b], accum_out=st2[:, 2:3])
        for c, n in enumerate((384, 256, 64)):
            k.act(st2[:, 3 + c:4 + c], st2[:, c:c + 1], AF.Sqrt, [st2b], [st2b], bias=EPS, scale=1.0 / n)
        k.recip(st2[:, 6:9], st2[:, 3:6], [st2b], [st2b])
        if k.stop == "A1":
            return
        k.act(cqn[:, 0:384], ps[0][:, 0:384], AF.Copy, [psb[0], st2b], [cqnb], scale=st2[:, 6:7])
        k.act(cqn[:, 384:640], ps[1][:, 0:256], AF.Copy, [psb[1], st2b], [cqnb], scale=st2[:, 7:8])
        if k.stop == "A2":
            return
        k.stt("dve", krf, ps[1][:, 256:320], st2[:, 8:9], k.pt[:, gt + 320:gt + 384], ALU.mult, ALU.mult,
              [psb[1], st2b, k.ptb], [krfb])
        emit_rope(k, krb[:, 0:64], krf, rt, ro, i, 1, [krfb], [krbb], rtb)
        k.cp("dve", krb[:, 64:128], krb[:, 0:64], [krbb], [krbb])
        if k.stop == "A3":
            return
        pbf = ps[2][:].bitcast(BF16)
        for c in range(5):
            k.tr(pbf[:, c * 128:(c + 1) * 128], cqn[:, c * 128:(c + 1) * 128], k.ident[:], [cqnb, k.cb], [psb[2]])
        k.tr(pbf[:, 640:768], krb, k.ident[:], [krbb, k.cb], [psb[2]])
        g = k.pf[:, gl:gl + 5]
        k.tt("dve", cT[:, :, i * 128:(i + 1) * 128], pbf[:, 0:640].rearrange("p (c t) -> p c t", t=128),
             g.unsqueeze(2).to_broadcast([128, 5, 128]), ALU.mult, [psb[2], k.pfb], [cTb[i]])
        if k.stop == "A4":
            return
        k.cp("dve", krT[:, i * 128:(i + 1) * 128], pbf[:, 640:768], [psb[2]], [krTb[i]])

    emit_norm_T(k, range(NT), lay["norm_mix"] + 8 * l, lambda i: (hTt[cnt[0] % 2], hTtb[cnt[0] % 2]), tmp, after=passA)
    k.dump("cT", cT, [128, 5, S], BF16, cTb)
    k.dump("krT", krT, [128, S], BF16, krTb)

    if k.stop in ("passA", "A1", "A2", "A3", "A4"):
        return
    k.arena_mark_reset(mark)
    wuq = [k.carve([3, 768], BF16) for _ in range(2)]
    wukv = [k.carve([2, 1024], BF16) for _ in range(2)]
    wo = k.carve([4, D], BF16)
    wuqb = [k.buf("wuq") for _ in range(2)]
    wukvb = [k.buf("wukv") for _ in range(2)]
    wob = k.buf("wo")
    kT = k.carve([4, S], BF16); kTb = [k.buf("kT") for _ in range(NT)]
    v = k.carve([NT, 512], BF16); vb = [k.buf("v") for _ in range(NT)]
    qTn = [k.carve([4, 512], BF16) for _ in range(2)]; qTnb = [k.buf("qTn") for _ in range(2)]
    qTr = [k.carve([2, 512], BF16) for _ in range(2)]; qTrb = [k.buf("qTr") for _ in range(2)]
    oT = k.carve([4, 512], BF16); oTb = k.buf("oT")
    pT = [k.carve([512], BF16) for _ in range(3)]; pTb = [k.buf("pT") for _ in range(3)]
    rden = k.carve([512], F32); rdenb = k.buf("rden")
    sq1 = k.carve([512], F32); sq1b = k.buf("sq1")
    sq2 = k.carve([256], F32); sq2b = k.buf("sq2")
    sq3 = k.carve([512], F32); sq3b = k.buf("sq3")
    ssq = k.carve([48], F32); ssqb = k.buf("ssq")
    tq = k.carve([512], F32); tqb = k.buf("tq")
    tr_ = k.carve([256], F32); trb = k.buf("tr")
    trt = k.carve([256], F32); tro = k.carve([256], F32); trtb = k.buf("trt")
    t3 = k.carve([512], F32); t3b = k.buf("t3")
    qnb_ = k.carve([512], BF16); qnbb = k.buf("qnb")
    qrb_ = k.carve([256], BF16); qrbb = k.buf("qrb")
    knb_ = k.carve([512], BF16); knbb = k.buf("knb")
    h4 = lambda ap, d: ap.rearrange("p (h d) -> p h d", d=d)
    scale = 192.0 ** -0.5

    def load_group(G):
        sl = G % 2
        k.dma("pool", wuq[sl], k.dram["mla_wuq"][j, G].rearrange("(kc p) f -> p kc f", p=128), f"wuq{sl}", writes=[wuqb[sl]])
        k.dma("pool", wukv[sl], k.dram["mla_wukv"][j, G].rearrange("(kc p) f -> p kc f", p=128), f"wukv{sl}", writes=[wukvb[sl]])

    def load_wo(G):
        k.dma("pool", wo, k.dram["mla_wo"][j, G * 512:(G + 1) * 512, :].rearrange("(h p) d -> p h d", p=128), "mla_wo", writes=[wob])

    def tile_proj(G, qb, i):
        sl = G % 2
        tt = i - 4 * qb
        qs = qb % 2
        csl = slice(i * 128, (i + 1) * 128)
        for kc in range(3):
            k.mm(ps[3][:], cT[:, kc, csl], wuq[sl][:, kc, 0:512], kc == 0, kc == 2, [cTb[i], wuqb[sl]], [psb[3]])
        for kc in range(3):
            k.mm(ps[4][:, 0:256], cT[:, kc, csl], wuq[sl][:, kc, 512:768], kc == 0, kc == 2, [cTb[i], wuqb[sl]], [psb[4]])
        for kc in range(2):
            k.mm(ps[5][:], cT[:, 3 + kc, csl], wukv[sl][:, kc, 0:512], kc == 0, kc == 1, [cTb[i], wukvb[sl]], [psb[5]])
        for kc in range(2):
            k.mm(ps[0][:], cT[:, 3 + kc, csl], wukv[sl][:, kc, 512:1024], kc == 0, kc == 1, [cTb[i], wukvb[sl]], [psb[0]])
        k.act(v[:, i, :], ps[0][:], AF.Copy, [psb[0]], [vb[i]])
        k.act(sq1, ps[3][:], AF.Square, [psb[3]], [sq1b])
        k.act(sq2, ps[4][:, 0:256], AF.Square, [psb[4]], [sq2b])
        k.act(sq3, ps[5][:], AF.Square, [psb[5]], [sq3b])
        k.red(ssq[:, 0:4], h4(sq1, 128), [sq1b], [ssqb])
        k.red(ssq[:, 4:8], h4(sq2, 64), [sq2b], [ssqb])
        k.red(ssq[:, 8:12], h4(sq3, 128), [sq3b], [ssqb])
        k.tt("dve", ssq[:, 12:24], ssq[:, 0:12], k.invn12[:], ALU.mult, [ssqb, k.cb], [ssqb])
        k.act(ssq[:, 24:36], ssq[:, 12:24], AF.Sqrt, [ssqb], [ssqb], bias=EPS, scale=1.0)
        k.recip(ssq[:, 36:48], ssq[:, 24:36], [ssqb], [ssqb])
        rs = ssq[:, 36:48]
        k.tt("dve", h4(tq, 128), h4(ps[3][:], 128), rs[:, 0:4].unsqueeze(2).to_broadcast([128, 4, 128]), ALU.mult,
             [psb[3], ssqb], [tqb])
        k.tt("pool", h4(qnb_, 128), h4(tq, 128), k.pt[:, gt:gt + 128].unsqueeze(1).to_broadcast([128, 4, 128]), ALU.mult,
             [tqb, k.ptb], [qnbb])
        k.tt("dve", h4(tr_, 64), h4(ps[4][:, 0:256], 64), rs[:, 4:8].unsqueeze(2).to_broadcast([128, 4, 64]), ALU.mult,
             [psb[4], ssqb], [trb])
        k.tt("pool", h4(tr_, 64), h4(tr_, 64), k.pt[:, gt + 128:gt + 192].unsqueeze(1).to_broadcast([128, 4, 64]), ALU.mult,
             [trb, k.ptb], [trb])
        emit_rope(k, qrb_, tr_, trt, tro, i, 4, [trb], [qrbb], trtb)
        k.tt("dve", h4(t3, 128), h4(ps[5][:], 128), rs[:, 8:12].unsqueeze(2).to_broadcast([128, 4, 128]), ALU.mult,
             [psb[5], ssqb], [t3b])
        k.tt("pool", h4(knb_, 128), h4(t3, 128), k.pt[:, gt + 192:gt + 320].unsqueeze(1).to_broadcast([128, 4, 128]), ALU.mult,
             [t3b, k.ptb], [knbb])
        p6 = ps[6][:].bitcast(BF16)
        for c in range(4):
            k.tr(p6[:, c * 128:(c + 1) * 128], qnb_[:, c * 128:(c + 1) * 128], k.ident[:], [qnbb, k.cb], [psb[6]])
        for c in range(2):
            k.tr(p6[:, 512 + c * 128:512 + (c + 1) * 128], qrb_[:, c * 128:(c + 1) * 128], k.ident[:], [qrbb, k.cb], [psb[6]])
        k.cp("dve", qTn[qs][:, :, tt * 128:(tt + 1) * 128], h4(p6[:, 0:512], 128), [psb[6]], [qTnb[qs]])
        k.cp("dve", qTr[qs][:, :, tt * 128:(tt + 1) * 128], h4(p6[:, 512:768], 128), [psb[6]], [qTrb[qs]])
        p7 = ps[7][:].bitcast(BF16)
        for c in range(4):
            k.tr(p7[:, c * 128:(c + 1) * 128], knb_[:, c * 128:(c + 1) * 128], k.ident[:], [knbb, k.cb], [psb[7]])
        k.cp("dve", kT[:, :, csl], h4(p7[:, 0:512], 128), [psb[7]], [kTb[i]])

    def attention(G, qb):
        qs = qb % 2
        nkt = 4 * qb + 4
        steps = [(hh, kt) for hh in range(4) for kt in range(nkt)]
        sbank = [2, 3, 4]
        acc = [(0, 1), (5, 6)]

        def qk(si):
            hh, kt = steps[si]
            blk, half = hh % 2, hh // 2
            jd = kt - 4 * qb
            c0 = max(0, jd) * 128
            b = sbank[si % 3]
            ks = slice(kt * 128, (kt + 1) * 128)
            k.mm(ps[b][:, 0:512 - c0], kT[:, hh, ks], qTn[qs][:, hh, c0:512], True, False, [kTb[kt], qTnb[qs]], [psb[b]])
            k.mm(ps[b][:, 0:512 - c0], krT[half * 64:(half + 1) * 64, ks], qTr[qs][half * 64:(half + 1) * 64, blk, c0:512],
                 False, True, [krTb[kt], qTrb[qs]], [psb[b]])
            pb_ = si % 3
            k.act(pT[pb_][:, 0:512 - c0], ps[b][:, 0:512 - c0], AF.Exp, [psb[b]], [pTb[pb_]], scale=scale)
            if jd >= 0:
                blkap = pT[pb_][:, 0:128]
                P.add("pool", lambda e: e.affine_select(out=blkap, in_=blkap, pattern=[[1, 128]], compare_op=ALU.is_ge,
                                                        fill=0.0, base=0, channel_multiplier=-1), [pTb[pb_]], [pTb[pb_]])

        def pv(si):
            hh, kt = steps[si]
            jd = kt - 4 * qb
            c0 = max(0, jd) * 128
            bo, bd = acc[hh % 2]
            pb_ = si % 3
            k.mm(ps[bo][:, c0:512], v[:, kt, hh * 128:(hh + 1) * 128], pT[pb_][:, 0:512 - c0], kt == 0, kt == nkt - 1,
                 [vb[kt], pTb[pb_]], [psb[bo]])
            k.mm(ps[bd][:, c0:512], k.ones_bf[:], pT[pb_][:, 0:512 - c0], kt == 0, kt == nkt - 1,
                 [k.cb, pTb[pb_]], [psb[bd]])
            if kt == nkt - 1:
                k.recip(rden, ps[bd][:], [psb[bd]], [rdenb])
                k.tt("dve", oT[:, hh, :], ps[bo][:], rden, ALU.mult, [psb[bo], rdenb], [oTb])

        for si in range(len(steps) + 2):
            if si < len(steps):
                qk(si)
            if si >= 2:
                pv(si - 2)

    def out_proj(G, qb):
        for tt in range(4):
            i = 4 * qb + tt
            for half in range(2):
                b = (7, 2)[half]
                for hh in range(4):
                    k.mm(ps[b][:], oT[:, hh, tt * 128:(tt + 1) * 128], wo[:, hh, half * 512:(half + 1) * 512],
                         hh == 0, hh == 3, [oTb, wob], [psb[b]])
                xs = k.x[:, i, half * 512:(half + 1) * 512]
                k.tt("dve", xs, xs, ps[b][:], ALU.add, [k.xb[i], psb[b]], [k.xb[i]])

    load_group(0)
    for G in range(2):
        load_wo(G)
        if G == 0:
            load_group(1)
        for qb in range(4):
            for i in range(4 * qb, 4 * qb + 4):
                tile_proj(G, qb, i)
            if k.stop == "proj":
                continue
            attention(G, qb)
            if k.stop == "attn":
                continue
            out_proj(G, qb)


def emit_conv(k, l):
    P = k.P
    lay = k.lay
    ps, psb = k.ps, k.psb
    k.arena_reset()
    w2 = k.carve([8, D], BF16); w2b = k.buf("w2")
    k.dma("pool", w2, k.dram["cv_w2"].rearrange("(cc p) d -> p cc d", p=128), "cv_w2", writes=[w2b])
    tmp = norm_tmp(k, [0, 1])
    hTt = [k.carve([8, 512], BF16) for _ in range(2)]; hTtb = [k.buf("hTt") for _ in range(2)]
    w1 = [k.carve([8, 2, 128], BF16) for _ in range(2)]; w1b = [k.buf("w1") for _ in range(2)]
    Dm = [k.carve([31, 128], BF16) for _ in range(2)]; Dmb = [k.buf("Dm") for _ in range(2)]
    uT = k.carve([8, 542], BF16); uTb = [k.buf("uT") for _ in range(8)]
    ysb = k.carve([8, 512], F32); ysbb = [k.buf("ysb") for _ in range(8)]
    ysq = [k.carve([512], F32) for _ in range(2)]; ysqb = [k.buf("ysq") for _ in range(2)]
    sig = [k.carve([512], F32) for _ in range(2)]; sigb = [k.buf("sig") for _ in range(2)]
    zT = k.carve([8, 512], BF16); zTb = [k.buf("zT") for _ in range(8)]
    mean = k.carve([512], F32); msq = k.carve([512], F32); rstd = k.carve([512], F32); stb = k.buf("cvst")
    tn = [k.carve([512], F32) for _ in range(2)]; tnb = [k.buf("tn") for _ in range(2)]
    cb1 = lay["cv_b1"]; cwd = lay["cv_wdw"]; cbd = lay["cv_bdw"]; cg = lay["cv_lng"]; cbn = lay["cv_lnb"]
    w1_d = k.dram["cv_w1"]

    def load_w1(n):
        cc = n % 8
        sl = n % 2
        k.dma("pool", w1[sl][:, :, 0, :], w1_d[:, cc * 128:(cc + 1) * 128].rearrange("(kc p) f -> p kc f", p=128),
              f"cvw1{sl}", writes=[w1b[sl]])
        k.dma("pool", w1[sl][:, :, 1, :], w1_d[:, D + cc * 128:D + (cc + 1) * 128].rearrange("(kc p) f -> p kc f", p=128),
              f"cvw1{sl}", writes=[w1b[sl]])

    load_w1(0)
    n = 0
    for tb in range(4):
        hs = tb % 2
        emit_norm_T(k, range(4 * tb, 4 * tb + 4), lay["norm_mix"] + 8 * l,
                    lambda i: (hTt[hs][:, :, (i % 4) * 128:(i % 4 + 1) * 128], hTtb[hs]), tmp)
        for i in range(4 * tb, 4 * tb + 4):
            k.tt("pool", k.x[:, i, :], k.x[:, i, :], k.pt[:, lay["cv_b2"]:lay["cv_b2"] + D], ALU.add,
                 [k.xb[i], k.ptb], [k.xb[i]])
        for cc in range(8):
            sl = n % 2
            if n + 1 < 32:
                load_w1(n + 1)
            n += 1
            wv = k.pf[:, cwd + cc * 31:cwd + (cc + 1) * 31]
            k.tt("pool", Dm[sl], k.identf[:].unsqueeze(1).to_broadcast([128, 31, 128]),
                 wv.unsqueeze(2).to_broadcast([128, 31, 128]), ALU.mult, [k.cb, k.pfb], [Dmb[sl]])
            ba, bg, bc = cc % 2, 2 + cc % 2, 4 + cc % 2
            for kc in range(8):
                k.mm(ps[ba][:], w1[sl][:, kc, 0, :], hTt[hs][:, kc, :], kc == 0, kc == 7, [w1b[sl], hTtb[hs]], [psb[ba]])
            for kc in range(8):
                k.mm(ps[bg][:], w1[sl][:, kc, 1, :], hTt[hs][:, kc, :], kc == 0, kc == 7, [w1b[sl], hTtb[hs]], [psb[bg]])
            ss_ = cc % 2
            k.act(sig[ss_], ps[bg][:], AF.Sigmoid, [psb[bg], k.pfb], [sigb[ss_]], bias=k.pf[:, cb1 + 8 + cc:cb1 + 9 + cc], scale=1.0)
            if tb == 0:
                k.memset("pool", uT[:, cc, 0:30], 0.0, [uTb[cc]])
            else:
                k.cp("pool", uT[:, cc, 0:30], uT[:, cc, 512:542], [uTb[cc]], [uTb[cc]])
            k.stt("dve", uT[:, cc, 30:542], ps[ba][:], k.pf[:, cb1 + cc:cb1 + cc + 1], sig[ss_], ALU.add, ALU.mult,
                  [psb[ba], sigb[ss_], k.pfb], [uTb[cc]])
            for jj in range(31):
                k.mm(ps[bc][:], Dm[sl][:, jj, :], uT[:, cc, jj:jj + 512], jj == 0, jj == 30, [Dmb[sl], uTb[cc]], [psb[bc]])
            bdw = k.pf[:, cbd + cc:cbd + cc + 1]
            k.act(ysb[:, cc, :], ps[bc][:], AF.Identity, [psb[bc], k.pfb], [ysbb[cc]], bias=bdw, scale=1.0)
            k.act(ysq[ss_], ps[bc][:], AF.Square, [psb[bc], k.pfb], [ysqb[ss_]], bias=bdw, scale=1.0)
            k.mm(ps[6][:], k.ones_f[:], ysb[:, cc, :], cc == 0, cc == 7, [k.cb, ysbb[cc]], [psb[6]])
            k.mm(ps[7][:], k.ones_f[:], ysq[ss_], cc == 0, cc == 7, [k.cb, ysqb[ss_]], [psb[7]])
        k.act(mean, ps[6][:], AF.Copy, [psb[6]], [stb], scale=1.0 / D)
        k.act(msq, ps[6][:], AF.Square, [psb[6]], [stb], scale=1.0 / D)
        k.stt("dve", rstd, ps[7][:], 1.0 / D, msq, ALU.mult, ALU.subtract, [psb[7], stb], [stb])
        k.act(rstd, rstd, AF.Sqrt, [stb], [stb], bias=EPS, scale=1.0)
        k.recip(rstd, rstd, [stb], [stb])
        for cc in range(8):
            ts_ = cc % 2
            k.tt("pool", tn[ts_], ysb[:, cc, :], mean, ALU.subtract, [ysbb[cc], stb], [tnb[ts_]])
            k.tt("dve", tn[ts_], tn[ts_], rstd, ALU.mult, [tnb[ts_], stb], [tnb[ts_]])
            k.act(zT[:, cc, :], tn[ts_], AF.Silu, [tnb[ts_], k.pfb], [zTb[cc]],
                  bias=k.pf[:, cbn + cc:cbn + cc + 1], scale=k.pf[:, cg + cc:cg + cc + 1])
        for tt in range(4):
            i = 4 * tb + tt
            for half in range(2):
                b = (2, 3)[half]
                for cc in range(8):
                    k.mm(ps[b][:], zT[:, cc, tt * 128:(tt + 1) * 128], w2[:, cc, half * 512:(half + 1) * 512],
                         cc == 0, cc == 7, [zTb[cc], w2b], [psb[b]])
                xs = k.x[:, i, half * 512:(half + 1) * 512]
                k.tt("dve", xs, xs, ps[b][:], ALU.add, [k.xb[i], psb[b]], [k.xb[i]])


def emit_hgrn_consts(k, l):
    lay = k.lay
    hgc = k.hgc
    cbs = [k.cb]
    lg = k.pf[:, lay["hg_lb"]:lay["hg_lb"] + 32]
    e = hgc[:, 0:32]
    k.act(e, lg, AF.Exp, [k.pfb], cbs)
    den = hgc[:, 32:40]
    k.tt("dve", den, e[:, 0:8], e[:, 8:16], ALU.add, cbs, cbs)
    k.tt("dve", den, den, e[:, 16:24], ALU.add, cbs, cbs)
    k.tt("dve", den, den, e[:, 24:32], ALU.add, cbs, cbs)
    k.recip(den, den, cbs, cbs)
    num = hgc[:, 40:48]
    k.memset("dve", num, 0.0, cbs)
    for i in range(1, l + 1):
        k.tt("dve", num, num, e[:, 8 * i:8 * i + 8], ALU.add, cbs, cbs)
    k.tt("dve", hgc[:, 48:56], num, den, ALU.mult, cbs, cbs)
    k.ts("dve", hgc[:, 56:64], hgc[:, 48:56], -1.0, 1.0, ALU.mult, ALU.add, cbs, cbs)
    k.ts("dve", hgc[:, 64:72], hgc[:, 56:64], -1.0, None, ALU.mult, None, cbs, cbs)
    k.memset("dve", k.scanmask[:], 1.0, cbs)
    k.memset("dve", k.scanmask[:].rearrange("p (c t) -> p c t", t=64)[:, :, 0:1], 0.0, cbs)
    k.memset("pool", k.mask2[:], 1.0, cbs)
    k.P.add("pool", lambda e_: e_.affine_select(out=k.mask2[:], in_=k.mask2[:], pattern=[[1, 128]], compare_op=ALU.is_ge,
                                                 fill=0.0, base=0, channel_multiplier=-1), cbs, cbs)
    k.memset("pool", k.mask2[0:64, 64:128], 0.0, cbs)


def emit_hgrn(k, l):
    P = k.P
    lay = k.lay
    ps, psb = k.ps, k.psb
    k.arena_reset()
    hT = k.carve([8, S], BF16); hTb = [k.buf("hT") for _ in range(4)]
    tmp = norm_tmp(k, [6, 7])
    Wp = [k.carve([8, 4, 256], BF16) for _ in range(2)]; Wpb = [k.buf("Wp") for _ in range(2)]
    wop = [k.carve([2, D], BF16) for _ in range(2)]; wopb = [k.buf("wop") for _ in range(2)]
    F = lambda: k.carve([512], F32)
    sg, f_, b_, bp, Ep, Em, gate, t1, t2, on = [F() for _ in range(10)]
    sgb, fb, bb, bpb, Epb, Emb, gateb, t1b, t2b, onb = [k.buf("hg") for _ in range(10)]
    kT_ = k.carve([512], BF16); kTb_ = k.buf("kcT")
    qT_ = k.carve([512], BF16); qTb_ = k.buf("qcT")
    vT_ = k.carve([512], BF16); vTb_ = k.buf("vT")
    ktm = k.carve([4, 128], BF16); ktmb = k.buf("ktm")
    vtm = k.carve([4, 128], BF16); vtmb = k.buf("vtm")
    onT = [k.carve([512], BF16) for _ in range(2)]; onTb = [k.buf("onT") for _ in range(2)]
    Am = [k.carve([128], BF16) for _ in range(2)]; Amb = [k.buf("Am") for _ in range(2)]
    Sst = [k.carve([128], F32) for _ in range(2)]; Sb = [k.buf("S") for _ in range(2)]
    Stil = k.carve([128], BF16); Stilb = k.buf("Stil")
    tmpS = k.carve([128], F32); tmpSb = k.buf("tmpS")
    em = k.carve([8], F32); el = k.carve([8], F32); emb = k.buf("em")
    hc = k.hgc
    w_d = k.dram["hg_wi"]
    wo_d = k.dram["hg_wo"]
    c8 = lambda ap: ap.rearrange("p (c t) -> p c t", t=64)

    emit_norm_T(k, range(NT), lay["norm_mix"] + 8 * l, lambda i: (hT[:, :, i * 128:(i + 1) * 128], hTb[i // 4]), tmp)

    def load(hp):
        sl = hp % 2
        for kind in range(4):
            k.dma("pool", Wp[sl][:, :, kind, :],
                  w_d[:, kind * D + hp * 256:kind * D + (hp + 1) * 256].rearrange("(kc p) f -> p kc f", p=128),
                  f"hgw{sl}", writes=[Wpb[sl]])
        k.dma("pool", wop[sl], wo_d[hp * 256:(hp + 1) * 256, :].rearrange("(hh p) d -> p hh d", p=128),
              f"hgwo{sl}", writes=[wopb[sl]])

    rot = [0]

    def proj(sl, kind, hh, tb, bank):
        for kc in range(8):
            k.mm(ps[bank][:], Wp[sl][:, kc, kind, hh * 128:(hh + 1) * 128], hT[:, kc, tb * 512:(tb + 1) * 512],
                 kc == 0, kc == 7, [Wpb[sl], hTb[tb]], [psb[bank]])

    load(0)
    for hp in range(4):
        sl = hp % 2
        if hp + 1 < 4:
            load(hp + 1)
        for hh in range(2):
            k.memset("pool", Sst[hh], 0.0, [Sb[hh]])
        for tb in range(4):
            for hh in range(2):
                hcol = 2 * hp + hh
                lb = hc[:, 48 + hcol:49 + hcol]
                oml = hc[:, 56 + hcol:57 + hcol]
                noml = hc[:, 64 + hcol:65 + hcol]
                bq = hh
                proj(sl, 0, hh, tb, bq)
                proj(sl, 1, hh, tb, 2)
                proj(sl, 2, hh, tb, 3)
                k.act(sg, ps[2][:], AF.Sigmoid, [psb[2]], [sgb])
                proj(sl, 3, hh, tb, 2)
                k.ts("dve", f_, sg, oml, lb, ALU.mult, ALU.add, [sgb, k.cb], [fb])
                k.act(f_, f_, AF.Ln, [fb], [fb])
                P.add("dve", lambda e: e.tensor_tensor_scan(out=b_, data0=k.scanmask[:], data1=f_, initial=0.0,
                                                            op0=ALU.mult, op1=ALU.add), [fb, k.cb], [bb])
                k.tt("dve", c8(bp), c8(b_), c8(b_)[:, :, 31:32].to_broadcast([128, 8, 64]), ALU.subtract, [bb], [bpb])
                k.act(Ep, bp, AF.Exp, [bpb], [Epb])
                k.act(Em, bp, AF.Exp, [bpb], [Emb], scale=-1.0)
                k.act(em, c8(b_)[:, :, 31], AF.Exp, [bb], [emb])
                k.act(el, c8(b_)[:, :, 63], AF.Exp, [bb], [emb])
                k.ts("dve", t1, sg, noml, oml, ALU.mult, ALU.add, [sgb, k.cb], [t1b])
                k.tt("pool", kT_, t1, Em, ALU.mult, [t1b, Emb], [kTb_])
                k.tt("dve", qT_, ps[bq][:], Ep, ALU.mult, [psb[bq], Epb], [qTb_])
                k.act(vT_, ps[3][:], AF.Copy, [psb[3]], [vTb_])
                k.act(gate, ps[2][:], AF.Silu, [psb[2]], [gateb])
                p6 = ps[6][:].bitcast(BF16)
                for tt in range(4):
                    k.tr(p6[:, tt * 128:(tt + 1) * 128], kT_[:, tt * 128:(tt + 1) * 128], k.ident[:], [kTb_, k.cb], [psb[6]])
                for tt in range(4):
                    k.tr(p6[:, 512 + tt * 128:512 + (tt + 1) * 128], vT_[:, tt * 128:(tt + 1) * 128], k.ident[:], [vTb_, k.cb], [psb[6]])
                k.cp("dve", ktm, p6[:, 0:512].rearrange("p (a b) -> p a b", b=128), [psb[6]], [ktmb])
                k.cp("dve", vtm, p6[:, 512:1024].rearrange("p (a b) -> p a b", b=128), [psb[6]], [vtmb])
                e2 = c8(Ep)[:, :, 63]
                for tt in range(4):
                    tsl = slice(tt * 128, (tt + 1) * 128)
                    a = tt % 2
                    k.mm(ps[4][:, 0:128], kT_[:, tsl], qT_[:, tsl], True, True, [kTb_, qTb_], [psb[4]])
                    k.tt("dve", Am[a], ps[4][:, 0:128], k.mask2[:], ALU.mult, [psb[4], k.cb], [Amb[a]])
                    k.mm(ps[7][:, tsl], vtm[:, tt, :], Am[a], True, False, [vtmb, Amb[a]], [psb[7]])
                    for half in range(2):
                        c = 2 * tt + half
                        hs = slice(half * 64, (half + 1) * 64)
                        csl = slice(tt * 128 + half * 64, tt * 128 + (half + 1) * 64)
                        k.act(Stil, Sst[hh], AF.Copy, [Sb[hh], emb], [Stilb], scale=em[:, c:c + 1])
                        k.mm(ps[7][:, csl], Stil, qT_[:, csl], False, half == 1, [Stilb, qTb_], [psb[7]])
                        k.mm(ps[5][:, 0:128], ktm[hs, tt, :], vtm[hs, tt, :], True, True, [ktmb, vtmb], [psb[5]])
                        k.ts("dve", tmpS, ps[5][:, 0:128], e2[:, c:c + 1], None, ALU.mult, None, [psb[5], Epb], [tmpSb])
                        k.stt("dve", Sst[hh], Sst[hh], el[:, c:c + 1], tmpS, ALU.mult, ALU.add, [Sb[hh], emb, tmpSb], [Sb[hh]])
                k.act(t1, ps[7][:], AF.Square, [psb[7]], [t1b])
                k.mm(ps[4][:], k.ones_f[:], t1, True, True, [k.cb, t1b], [psb[4]])
                k.act(t2, ps[4][:], AF.Sqrt, [psb[4]], [t2b], bias=EPS, scale=1.0 / 128)
                k.recip(t2, t2, [t2b], [t2b])
                go = k.pf[:, lay["hg_on"]:lay["hg_on"] + 1]
                k.stt("dve", on, ps[7][:], go, t2, ALU.mult, ALU.mult, [psb[7], t2b, k.pfb], [onb])
                k.tt("pool", onT[hh], on, gate, ALU.mult, [onb, gateb], [onTb[hh]])
            for tt in range(4):
                i = 4 * tb + tt
                for half in range(2):
                    bnk = (2, 3)[half]
                    for hh in range(2):
                        k.mm(ps[bnk][:], onT[hh][:, tt * 128:(tt + 1) * 128], wop[sl][:, hh, half * 512:(half + 1) * 512],
                             hh == 0, hh == 1, [onTb[hh], wopb[sl]], [psb[bnk]])
                    xs = k.x[:, i, half * 512:(half + 1) * 512]
                    k.tt("dve", xs, xs, ps[bnk][:], ALU.add, [k.xb[i], psb[bnk]], [k.xb[i]])


def fm(v):
    v = np.asarray(v, np.float32)
    return np.ascontiguousarray(v.reshape(-1, 128).T)


def pack_inputs(inp):
    cols = []
    lay = {}

    def put(name, arr):
        lay[name] = sum(c.shape[1] for c in cols)
        cols.append(np.asarray(arr, np.float32))

    put("norm_mix", np.concatenate([fm(inp["norm_mix"][l]) for l in range(4)], axis=1))
    put("norm_mlp", np.concatenate([fm(inp["norm_mlp"][l]) for l in range(4)], axis=1))
    put("mla_lat", np.concatenate([np.concatenate([fm(inp["mla_q_lat_norm"][j]), fm(inp["mla_kv_lat_norm"][j])], axis=1)
                                   for j in range(2)], axis=1))
    put("hg_lb", np.concatenate([fm(inp["hg_lb_logits"][i]) for i in range(4)], axis=1))
    put("hg_on", fm(inp["hg_out_norm"][0]))
    put("cv_b1", fm(inp["cv_b_pw1"][0]))
    put("cv_wdw", np.asarray(inp["cv_w_dw"][0], np.float32).T.reshape(8, 128, 31).transpose(1, 0, 2).reshape(128, 8 * 31))
    put("cv_bdw", fm(inp["cv_b_dw"][0]))
    put("cv_lng", fm(inp["cv_ln_g"][0]))
    put("cv_lnb", fm(inp["cv_ln_b"][0]))
    pf = np.ascontiguousarray(np.concatenate(cols, axis=1))
    lay["npf"] = pf.shape[1]
    tcols = []

    def putt(name, vec):
        lay[name] = sum(c.shape[0] for c in tcols)
        tcols.append(np.asarray(vec, np.float32).reshape(-1))

    putt("mla_head", np.concatenate([np.concatenate([inp["mla_q_head_norm"][j], inp["mla_k_head_norm"][j]]) for j in range(2)]))
    putt("cv_b2", inp["cv_b_pw2"][0])
    ptv = np.concatenate(tcols)
    pt = np.ascontiguousarray(np.broadcast_to(ptv[None, :], (128, ptv.shape[0])))
    lay["npt"] = pt.shape[1]
    return pf, pt, lay


def pack_weights(inp):
    w = {}
    w["mlp_wi"] = np.asarray(inp["mlp_w_in"], np.float32)
    w["mlp_wo"] = np.asarray(inp["mlp_w_out"], np.float32)
    w["mla_wd"] = np.asarray(inp["mla_w_down"], np.float32)
    wuq = np.asarray(inp["mla_w_uq"], np.float32).reshape(2, 384, 8, 192)
    wukv = np.asarray(inp["mla_w_ukv"], np.float32).reshape(2, 256, 8, 256)
    uq = np.empty((2, 2, 384, 768), np.float32)
    ukv = np.empty((2, 2, 256, 1024), np.float32)
    for G in range(2):
        hs = [4 * G + i for i in range(4)]
        uq[:, G, :, 0:512] = wuq[:, :, hs, 0:128].reshape(2, 384, 512)
        rope_order = [4 * G + 0, 4 * G + 2, 4 * G + 1, 4 * G + 3]
        uq[:, G, :, 512:768] = wuq[:, :, rope_order, 128:192].reshape(2, 384, 256)
        ukv[:, G, :, 0:512] = wukv[:, :, hs, 0:128].reshape(2, 256, 512)
        ukv[:, G, :, 512:1024] = wukv[:, :, hs, 128:256].reshape(2, 256, 512)
    w["mla_wuq"] = uq
    w["mla_wukv"] = ukv
    w["mla_wo"] = np.asarray(inp["mla_w_o"], np.float32)
    w["cv_w1"] = np.asarray(inp["cv_w_pw1"][0], np.float32)
    w["cv_w2"] = np.asarray(inp["cv_w_pw2"][0], np.float32)
    w["hg_wi"] = np.asarray(inp["hg_w_in"][0], np.float32)
    w["hg_wo"] = np.asarray(inp["hg_w_o"][0], np.float32)
    return w


ALL_LAYERS = [("mla", 0, 0), ("mlp", 0, 0), ("hgrn", 1, 0), ("mlp", 1, 0),
              ("conv", 2, 0), ("mlp", 2, 0), ("mla", 3, 1), ("mlp", 3, 0)]


def run(inp, layers, n_cores=N_CORES, n_seq=2, trace=False, debug=False, stop=None):
    pf, pt, lay = pack_inputs(inp)
    nc, stats = build_program(n_seq, layers, lay, debug, stop)
    x = np.asarray(inp["x"], np.float32)
    pos = np.asarray(inp["positions"], np.int32)
    wts = pack_weights(inp)
    in_maps = []
    for c in range(n_cores):
        sl = slice(c * n_seq, (c + 1) * n_seq)
        m = dict(
            x=np.ascontiguousarray(x[sl]),
            pos=np.ascontiguousarray(pos[sl].reshape(n_seq, NT, 128).transpose(0, 2, 1)),
            pf=pf, pt=pt, **wts,
        )
        in_maps.append(m)
    res = run_bass_kernel_spmd(nc, in_maps, core_ids=list(range(n_cores)), **({"trace": True} if trace else {}))
    out = np.concatenate([r["out"] for r in res.results], axis=0)
    return out, res, stats


def kernel(**inputs):
    out, _, _ = run(inputs, ALL_LAYERS)
    return out.astype(np.float32)
```

```python
import math
import numpy as np
from contextlib import ExitStack
from functools import partial

import concourse.bass as bass
import concourse.mybir as mybir
from concourse.bass_utils import run_bass_kernel_spmd

F32 = mybir.dt.float32
BF16 = mybir.dt.bfloat16
I32 = mybir.dt.int32
AF = mybir.ActivationFunctionType
ALU = mybir.AluOpType
AX = mybir.AxisListType

S = 2048
D = 1024
NT = 16
DFF = 4096
EPS = 1e-6
N_CORES = 8
ENGS = ("pe", "act", "dve", "pool", "sp")


class Buf:
    __slots__ = ("name", "last_w", "readers")

    def __init__(self, name):
        self.name = name
        self.last_w = None
        self.readers = []


class Prog:
    def __init__(self, nc):
        self.nc = nc
        self.ins = []
        self.last_on_eng = {e: None for e in ENGS}
        self.last_dma = {}
        self.pending_fence = {e: None for e in ENGS}

    def add(self, eng, fn, reads=(), writes=(), dma=None):
        i = len(self.ins)
        deps = set()
        for b in reads:
            if b.last_w is not None:
                deps.add(b.last_w)
        for b in writes:
            if b.last_w is not None:
                deps.add(b.last_w)
            deps.update(b.readers)
        if self.pending_fence[eng] is not None:
            deps |= self.pending_fence[eng]
            self.pending_fence[eng] = None
        self.ins.append(dict(eng=eng, fn=fn, deps=deps, dma=dma, sig=False))
        for b in reads:
            b.readers.append(i)
        for b in writes:
            b.last_w = i
            b.readers = []
        self.last_on_eng[eng] = i
        if dma is not None:
            self.last_dma[dma] = i
        return i

    def fence(self):
        s = set(v for v in self.last_on_eng.values() if v is not None)
        s |= set(self.last_dma.values())
        for e in ENGS:
            self.pending_fence[e] = set(s) | (self.pending_fence[e] or set())

    def emit(self, es, final_wait_groups=()):
        nc = self.nc
        ins = self.ins
        for r in ins:
            nd = set()
            for d in r["deps"]:
                p = ins[d]
                if p["dma"] is None and r["dma"] is None and p["eng"] == r["eng"] and r["eng"] == "pe":
                    continue
                nd.add(d)
            r["deps"] = nd
            for d in nd:
                ins[d]["sig"] = True
        eng_sem = {e: es.enter_context(nc.semaphore("s_" + e)) for e in ("pe", "act", "dve", "pool")}
        grp_sem = {}
        for r in ins:
            if r["dma"] is not None and r["dma"] not in grp_sem:
                grp_sem[r["dma"]] = es.enter_context(nc.semaphore("g_" + r["dma"]))
        cnt = {e: 0 for e in eng_sem}
        gcnt = {g: 0 for g in grp_sem}
        for r in ins:
            if r["dma"] is not None:
                gcnt[r["dma"]] += 16
                r["tok"] = ("g", r["dma"], gcnt[r["dma"]])
            elif r["sig"]:
                cnt[r["eng"]] += 1
                r["tok"] = ("e", r["eng"], cnt[r["eng"]])
        gtot = {g: 0 for g in grp_sem}
        per_eng = {e: [] for e in ENGS}
        known = {e: {} for e in ENGS}
        for r in ins:
            waits = {}
            for d in r["deps"]:
                kind, key, val = ins[d]["tok"]
                if kind == "g":
                    val = max(val, gtot[key])
                k = (kind, key)
                waits[k] = max(waits.get(k, 0), val)
            if r["dma"] is not None:
                gtot[r["dma"]] += 16
            kn = known[r["eng"]]
            wl = []
            for k, v in waits.items():
                if kn.get(k, 0) >= v:
                    continue
                kn[k] = v
                wl.append((k, v))
            per_eng[r["eng"]].append((r, wl))
        self.stats = dict(n={e: len(per_eng[e]) for e in ENGS}, sem=dict(cnt), nsem=len(grp_sem) + 4)

        def semof(k):
            return eng_sem[k[1]] if k[0] == "e" else grp_sem[k[1]]

        def run(engname, eobj):
            for r, wl in per_eng[engname]:
                for k, v in wl:
                    eobj.wait_ge(semof(k), v)
                bi = r["fn"](eobj)
                if r["dma"] is not None:
                    bi.then_inc(grp_sem[r["dma"]], 16)
                elif r["sig"]:
                    bi.then_inc(eng_sem[r["eng"]], 1)
            if engname == "sp":
                for g in final_wait_groups:
                    eobj.wait_ge(grp_sem[g], gcnt[g])

        with nc.Block() as block:
            @block.tensor
            def _(e):
                run("pe", e)

            @block.scalar
            def _(e):
                run("act", e)

            @block.vector
            def _(e):
                run("dve", e)

            @block.gpsimd
            def _(e):
                run("pool", e)

            @block.sync
            def _(e):
                run("sp", e)


class K:
    def __init__(self, nc, es, n_seq):
        self.nc = nc
        self.es = es
        self.P = Prog(nc)
        self.n_seq = n_seq
        self.uid = 0
        self.debug = False
        self.stop = None
        self.dbg_names = []

    def sb(self, name, shape, dt):
        return self.es.enter_context(self.nc.sbuf_tensor("sb_" + name, shape, dt))

    def mm(self, out, lhsT, rhs, start, stop, reads, writes):
        self.P.add("pe", lambda e: e.matmul(out, lhsT=lhsT, rhs=rhs, start=start, stop=stop), reads, writes)

    def tr(self, out, in_, ident, reads, writes):
        self.P.add("pe", lambda e: e.transpose(out=out, in_=in_, identity=ident), reads, writes)

    def act(self, out, in_, func, reads, writes, **kw):
        self.P.add("act", lambda e: e.activation(out=out, in_=in_, func=func, **kw), reads, writes)

    def tt(self, eng, out, in0, in1, op, reads, writes):
        self.P.add(eng, lambda e: e.tensor_tensor(out=out, in0=in0, in1=in1, op=op), reads, writes)

    def ts(self, eng, out, in0, s1, s2, op0, op1, reads, writes):
        if s2 is None:
            self.P.add(eng, lambda e: e.tensor_scalar(out=out, in0=in0, scalar1=s1, scalar2=None, op0=op0), reads, writes)
        else:
            self.P.add(eng, lambda e: e.tensor_scalar(out=out, in0=in0, scalar1=s1, scalar2=s2, op0=op0, op1=op1), reads, writes)

    def stt(self, eng, out, in0, scalar, in1, op0, op1, reads, writes):
        self.P.add(eng, lambda e: e.scalar_tensor_tensor(out=out, in0=in0, scalar=scalar, in1=in1, op0=op0, op1=op1), reads, writes)

    def cp(self, eng, out, in_, reads, writes):
        self.P.add(eng, lambda e: e.tensor_copy(out=out, in_=in_), reads, writes)

    def recip(self, out, in_, reads, writes):
        self.P.add("dve", lambda e: e.reciprocal(out=out, in_=in_), reads, writes)

    def memset(self, eng, ap, val, writes):
        self.P.add(eng, lambda e: e.memset(ap, val), (), writes)

    def dma(self, eng, out, in_, grp, reads=(), writes=()):
        self.P.add(eng, lambda e: e.dma_start(out=out, in_=in_), reads, writes, dma=grp)

    def arena_reset(self):
        self.P.fence()
        self.aoff = 0

    def carve(self, free_shape, dt):
        n = int(np.prod(free_shape))
        nbytes = n * (4 if dt in (F32, I32) else 2)
        nbytes = (nbytes + 63) // 64 * 64
        assert self.aoff + nbytes <= self.arena_bytes, (self.aoff, nbytes, self.arena_bytes)
        ap = self.arena[:, self.aoff // 2:(self.aoff + nbytes) // 2]
        self.aoff += nbytes
        if dt != BF16:
            ap = ap.bitcast(dt)
        ap = ap[:, 0:n]
        if len(free_shape) == 2:
            ap = ap.rearrange("p (a b) -> p a b", b=free_shape[1])
        elif len(free_shape) == 3:
            ap = ap.rearrange("p (a b c) -> p a b c", b=free_shape[1], c=free_shape[2])
        return ap

    def buf(self, name):
        self.uid += 1
        return Buf(f"{name}_{self.uid}")

    def arena_mark_reset(self, mark):
        self.P.fence()
        self.aoff = mark

    def red(self, out, in_, reads, writes):
        self.P.add("dve", lambda e: e.tensor_reduce(out=out, in_=in_, axis=AX.X, op=ALU.add), reads, writes)

    def dump(self, name, ap, shape, dt, reads):
        if not getattr(self, "debug", False) or ("dbg_" + name) in self.dbg_names:
            return
        d = self.nc.dram_tensor("dbg_" + name, list(shape), dt, kind="ExternalOutput").ap()
        self.dma("sp", d, ap, "dbg", reads=reads)
        self.dbg_names.append("dbg_" + name)


def build_program(n_seq, layers, lay, debug=False, stop=None):
    nc = bass.Bass("TRN2", target_bir_lowering=False)
    es = ExitStack()
    k = K(nc, es, n_seq)
    k.debug = debug
    k.stop = stop
    P = k.P
    dt = lambda name, shape, d, kind="ExternalInput": nc.dram_tensor(name, shape, d, kind=kind).ap()
    x_d = dt("x", [n_seq, S, D], F32)
    out_d = dt("out", [n_seq, S, D], F32, "ExternalOutput")
    pos_d = dt("pos", [n_seq, 128, NT], I32)
    pf_d = dt("pf", [128, lay["npf"]], F32)
    pt_d = dt("pt", [128, lay["npt"]], F32)
    mlp_wi_d = dt("mlp_wi", [4, D, DFF], F32)
    mlp_wo_d = dt("mlp_wo", [4, DFF, D], F32)
    k.dram = dict(x=x_d, out=out_d, pos=pos_d, pf=pf_d, pt=pt_d, mlp_wi=mlp_wi_d, mlp_wo=mlp_wo_d)
    k.dram["mla_wd"] = dt("mla_wd", [2, D, 704], F32)
    k.dram["mla_wuq"] = dt("mla_wuq", [2, 2, 384, 768], F32)
    k.dram["mla_wukv"] = dt("mla_wukv", [2, 2, 256, 1024], F32)
    k.dram["mla_wo"] = dt("mla_wo", [2, D, D], F32)
    k.dram["cv_w1"] = dt("cv_w1", [D, 2 * D], F32)
    k.dram["cv_w2"] = dt("cv_w2", [D, D], F32)
    k.dram["hg_wi"] = dt("hg_wi", [D, 4 * D], F32)
    k.dram["hg_wo"] = dt("hg_wo", [D, D], F32)
    k.lay = lay

    k.x = k.sb("x", [128, NT, D], F32)
    k.xb = [Buf(f"x{i}") for i in range(NT)]
    k.pf = k.sb("pf", [128, lay["npf"]], F32)
    k.pfb = Buf("pf")
    k.ident = k.sb("ident", [128, 128], BF16)
    k.identf = k.sb("identf", [128, 128], F32)
    k.cb = Buf("consts")
    k.ss = k.sb("ss", [128, 64], F32)
    k.ps = [es.enter_context(nc.psum_tensor(f"ps{i}", [128, 512], F32)) for i in range(8)]
    k.psb = [Buf(f"ps{i}") for i in range(8)]
    k.pt = k.sb("pt", [128, lay["npt_res"]], F32)
    k.ptb = Buf("pt")
    k.ones_bf = k.sb("ones_bf", [128, 128], BF16)
    k.invn12 = k.sb("invn12", [128, 12], F32)
    k.ones_f = k.sb("ones_f", [128, 128], F32)
    k.hgc = k.sb("hgc", [128, 72], F32)
    k.mask2 = k.sb("mask2", [128, 128], F32)
    k.negpi = k.sb("negpi", [128, 1], F32)
    k.rope_invf = k.sb("rope_invf", [128, 32], F32)
    k.rope_posi = k.sb("rope_posi", [128, NT], I32)
    k.rope_posf = k.sb("rope_posf", [128, NT], F32)
    k.cos2 = k.sb("cos2", [128, NT, 64], F32)
    k.sinA = k.sb("sinA", [128, NT, 64], F32)
    k.ropeb = Buf("rope")
    k.arena_bytes = 128 * 1024
    k.arena = k.sb("arena", [128, k.arena_bytes // 2], BF16)
    k.aoff = 0

    k.dma("sp", k.pf[:], pf_d, "pf", writes=[k.pfb])
    k.memset("pool", k.identf[:], 0.0, [k.cb])
    P.add("pool", lambda e: e.affine_select(out=k.identf[:], in_=k.identf[:], pattern=[[-1, 128]],
                                            compare_op=ALU.not_equal, fill=1.0, base=0, channel_multiplier=1),
          [k.cb], [k.cb])
    k.cp("dve", k.ident[:], k.identf[:], [k.cb], [k.cb])
    k.dma("sp", k.pt[:], pt_d[:, 0:lay["npt_res"]], "pt", writes=[k.ptb])
    k.memset("dve", k.ones_bf[:], 1.0, [k.cb])
    k.memset("dve", k.ones_f[:], 1.0, [k.cb])
    k.memset("dve", k.invn12[:, 0:4], 1.0 / 128, [k.cb])
    k.memset("dve", k.invn12[:, 4:8], 1.0 / 64, [k.cb])
    k.memset("dve", k.invn12[:, 8:12], 1.0 / 128, [k.cb])
    k.memset("dve", k.negpi[:], -math.pi * (1.0 - 1e-6), [k.cb])
    for f in range(32):
        k.memset("pool", k.rope_invf[:, f:f + 1], float(np.float32(10000.0) ** np.float32(-2.0 * f / 64)), [k.cb])

    for (kind, l, j) in layers:
        if kind == "hgrn":
            emit_hgrn_consts(k, l)
    for s in range(n_seq):
        for i in range(NT):
            k.dma("sp", k.x[:, i, :], x_d[s, i * 128:(i + 1) * 128, :], "xio", writes=[k.xb[i]])
        if any(kd == "mla" for kd, _, _ in layers):
            emit_rope_tables(k, s)
        for (kind, l, j) in layers:
            if kind == "mlp":
                emit_mlp(k, l)
            elif kind == "mla":
                emit_mla(k, l, j)
            elif kind == "conv":
                emit_conv(k, l)
            elif kind == "hgrn":
                emit_hgrn(k, l)
            else:
                raise ValueError(kind)
        for i in range(NT):
            k.dma("sp", out_d[s, i * 128:(i + 1) * 128, :], k.x[:, i, :], "xio", reads=[k.xb[i]])
    P.emit(es, final_wait_groups=["xio"] + (["dbg"] if k.dbg_names else []))
    es.close()
    return nc, P.stats


def emit_norm_T(k, tiles, gcol, dst, tmp, after=None, lag=0):
    tiles = list(tiles)
    for n, i in enumerate(tiles):
        slot = n % 2
        junk, jb = tmp["junk"][slot], tmp["junkb"][slot]
        xn, xnb = tmp["xn"][slot], tmp["xnb"][slot]
        st, stb = tmp["st"][slot], tmp["stb"][slot]
        pb = tmp["psum"][slot]
        k.act(junk, k.x[:, i, :], AF.Square, [k.xb[i]], [jb, stb], accum_out=st[:, 0:1])
        k.act(st[:, 1:2], st[:, 0:1], AF.Sqrt, [stb], [stb], bias=EPS, scale=1.0 / D)
        k.recip(st[:, 2:3], st[:, 1:2], [stb], [stb])
        k.act(xn, k.x[:, i, :], AF.Copy, [k.xb[i], stb], [xnb], scale=st[:, 2:3])
        pbf = k.ps[pb][:].bitcast(BF16)
        for c in range(8):
            k.tr(pbf[:, c * 128:(c + 1) * 128], xn[:, c * 128:(c + 1) * 128], k.ident[:], [xnb, k.cb], [k.psb[pb]])
        d_ap, d_buf = dst(i)
        g = k.pf[:, gcol:gcol + 8]
        k.tt("dve", d_ap, pbf.rearrange("p (c t) -> p c t", t=128),
             g.unsqueeze(2).to_broadcast([128, 8, 128]), ALU.mult, [k.psb[pb], k.pfb], [d_buf])
        if after is not None:
            if lag == 0:
                after(i)
            elif n >= lag:
                after(tiles[n - lag])
    if after is not None and lag > 0:
        for i in tiles[len(tiles) - lag:]:
            after(i)


def norm_tmp(k, psum_banks):
    t = dict(junk=[], junkb=[], xn=[], xnb=[], st=[], stb=[], psum=psum_banks)
    for s in range(2):
        t["junk"].append(k.carve([D], BF16))
        t["junkb"].append(k.buf("junk"))
        t["xn"].append(k.carve([D], BF16))
        t["xnb"].append(k.buf("xn"))
        t["st"].append(k.carve([8], F32))
        t["stb"].append(k.buf("st"))
    return t


def emit_mlp(k, l):
    P = k.P
    k.arena_reset()
    hT = k.carve([8, S], BF16)
    hTb = [k.buf("hT") for _ in range(4)]
    tmp = norm_tmp(k, [6, 7])
    wi = [k.carve([8, 512], BF16) for _ in range(2)]
    wo = [k.carve([4, D], BF16) for _ in range(2)]
    wib = [k.buf("wi") for _ in range(2)]
    wob = [k.buf("wo") for _ in range(2)]
    aT = [k.carve([4, 512], BF16) for _ in range(2)]
    aTb = [k.buf("aT") for _ in range(2)]
    r = [k.carve([512], F32) for _ in range(2)]
    rb = [k.buf("r") for _ in range(2)]

    def norm_block(tb):
        emit_norm_T(k, range(4 * tb, 4 * tb + 4), k.lay["norm_mlp"] + 8 * l,
                    lambda i: (hT[:, :, i * 128:(i + 1) * 128], hTb[i // 4]), tmp)

    wi_d = k.dram["mlp_wi"]
    wo_d = k.dram["mlp_wo"]

    def load(g):
        sl = g % 2
        k.dma("pool", wi[sl], wi_d[l, :, g * 512:(g + 1) * 512].rearrange("(kc p) f -> p kc f", p=128),
              f"mwi{sl}", writes=[wib[sl]])
        k.dma("pool", wo[sl], wo_d[l, g * 512:(g + 1) * 512, :].rearrange("(fc p) d -> p fc d", p=128),
              f"mwo{sl}", writes=[wob[sl]])

    steps = [(g, tb) for g in range(8) for tb in range(4)]
    abank = [0, 1, 2, 3]
    ybank = [4, 5]
    state = dict(na=0, nr=0)

    def stage1(si):
        g, tb = steps[si]
        sl = g % 2
        a = si % 2
        for m in range(4):
            b = abank[state["na"] % 4]
            state["na"] += 1
            for kc in range(8):
                k.mm(k.ps[b][:], wi[sl][:, kc, m * 128:(m + 1) * 128], hT[:, kc, tb * 512:(tb + 1) * 512],
                     kc == 0, kc == 7, [wib[sl], hTb[tb]], [k.psb[b]])
            rr = state["nr"] % 2
            state["nr"] += 1
            k.act(r[rr], k.ps[b][:], AF.Relu, [k.psb[b]], [rb[rr]])
            k.act(aT[a][:, m, :], r[rr], AF.Square, [rb[rr]], [aTb[a]])

    def stage2(si):
        g, tb = steps[si]
        sl = g % 2
        a = si % 2
        for tt in range(4):
            i = tb * 4 + tt
            for half in range(2):
                b = ybank[half]
                for m in range(4):
                    k.mm(k.ps[b][:], aT[a][:, m, tt * 128:(tt + 1) * 128], wo[sl][:, m, half * 512:(half + 1) * 512],
                         m == 0, m == 3, [aTb[a], wob[sl]], [k.psb[b]])
                xs = k.x[:, i, half * 512:(half + 1) * 512]
                k.tt("dve", xs, xs, k.ps[b][:], ALU.add, [k.xb[i], k.psb[b]], [k.xb[i]])

    load(0)
    norm_block(0)
    for si in range(len(steps) + 1):
        if si < len(steps):
            stage1(si)
            if si + 1 < 4:
                norm_block(si + 1)
        if si >= 1:
            stage2(si - 1)
        if si < len(steps):
            g, tb = steps[si]
            if tb == 0 and g + 1 < 8:
                load(g + 1)


def emit_rope_tables(k, s):
    posi = k.rope_posi[:]
    k.dma("sp", posi, k.dram["pos"][s], "pos", writes=[k.ropeb])
    k.cp("dve", k.rope_posf[:], posi, [k.ropeb], [k.ropeb])
    k.arena_reset()
    ang = k.carve([NT, 32], F32)
    k.tt("dve", ang, k.rope_posf[:].unsqueeze(2).to_broadcast([128, NT, 32]),
         k.rope_invf[:].unsqueeze(1).to_broadcast([128, NT, 32]), ALU.mult, [k.ropeb, k.cb], [k.ropeb])
    r1 = k.carve([NT, 32], F32)
    ki = k.carve([NT, 32], I32)
    kf = k.carve([NT, 32], F32)
    sc = 1.0 - 1e-6
    rb = [k.ropeb]
    two_pi = 2 * math.pi

    def reduce_to_pi(shift):
        k.ts("dve", kf, ang, shift, 1.0 / two_pi, ALU.add, ALU.mult, rb, rb)
        k.cp("dve", ki, kf, rb, rb)
        k.cp("dve", kf, ki, rb, rb)
        k.stt("dve", r1, kf, -two_pi, ang, ALU.mult, ALU.add, rb, rb)
        if shift != 0.0:
            k.ts("dve", r1, r1, shift, None, ALU.add, None, rb, rb)
        k.ts("dve", kf, r1, math.pi, two_pi, ALU.is_gt, ALU.mult, rb, rb)
        k.tt("dve", r1, r1, kf, ALU.subtract, rb, rb)
        k.ts("dve", kf, r1, -math.pi, two_pi, ALU.is_lt, ALU.mult, rb, rb)
        k.tt("dve", r1, r1, kf, ALU.add, rb, rb)

    reduce_to_pi(0.0)
    k.act(k.sinA[:, :, 32:64], r1, AF.Sin, rb, rb, scale=sc)
    k.ts("dve", k.sinA[:, :, 0:32], k.sinA[:, :, 32:64], -1.0, None, ALU.mult, None, rb, rb)
    reduce_to_pi(math.pi / 2)
    k.act(k.cos2[:, :, 0:32], r1, AF.Sin, rb, rb, scale=sc)
    k.cp("dve", k.cos2[:, :, 32:64], k.cos2[:, :, 0:32], rb, rb)


def emit_rope(k, out_bf, t, tmp, o, i, nh, reads, writes, tb):
    v3 = lambda ap: ap.rearrange("p (h d) -> p h d", d=64)
    cosb = k.cos2[:, i, :].unsqueeze(1).to_broadcast([128, nh, 64])
    sa = k.sinA[:, i, :]
    s_lo = sa[:, 0:32].unsqueeze(1).to_broadcast([128, nh, 32])
    s_hi = sa[:, 32:64].unsqueeze(1).to_broadcast([128, nh, 32])
    k.tt("dve", v3(tmp)[:, :, 0:32], v3(t)[:, :, 32:64], s_lo, ALU.mult, reads + [k.ropeb], [tb])
    k.tt("dve", v3(tmp)[:, :, 32:64], v3(t)[:, :, 0:32], s_hi, ALU.mult, reads + [k.ropeb], [tb])
    k.tt("dve", v3(o), v3(t), cosb, ALU.mult, reads + [k.ropeb], [tb])
    k.tt("dve", out_bf, o, tmp, ALU.add, [tb], writes)


def emit_mla(k, l, j):
    P = k.P
    lay = k.lay
    if k.stop == "rope":
        return
    k.arena_reset()
    ps, psb = k.ps, k.psb
    cT = k.carve([5, S], BF16)
    cTb = [k.buf("cT") for _ in range(NT)]
    krT = k.carve([S], BF16)
    krTb = [k.buf("krT") for _ in range(NT)]
    mark = k.aoff
    wd = k.carve([8, 704], BF16)
    wdb = k.buf("wd")
    k.dma("pool", wd, k.dram["mla_wd"][j].rearrange("(kc p) f -> p kc f", p=128), "mla_wd", writes=[wdb])
    tmp = norm_tmp(k, [6, 7])
    hTt = [k.carve([8, 128], BF16) for _ in range(2)]
    hTtb = [k.buf("hTt") for _ in range(2)]
    two = lambda shape, dt_: ([k.carve(shape, dt_) for _ in range(2)], [k.buf("pa") for _ in range(2)])
    cqn_, cqnb_ = two([640], BF16)
    junkA_, junkAb_ = two([384], BF16)
    st2_, st2b_ = two([16], F32)
    krf_, krfb_ = two([64], F32)
    rt_, rtb_ = two([64], F32)
    ro_, _ = two([64], F32)
    krb_, krbb_ = two([128], BF16)
    gl = lay["mla_lat"] + 5 * j
    gt = lay["mla_head"] + 384 * j
    cnt = [0]

    def passA(i):
        sl = i % 2
        h, hb = hTt[sl], hTtb[sl]
        cqn, cqnb, junkA, junkAb, st2, st2b = cqn_[sl], cqnb_[sl], junkA_[sl], junkAb_[sl], st2_[sl], st2b_[sl]
        krf, krfb, rt, rtb, ro, krb, krbb = krf_[sl], krfb_[sl], rt_[sl], rtb_[sl], ro_[sl], krb_[sl], krbb_[sl]
        b0, b1, b2 = (0, 1, 2) if sl == 0 else (3, 4, 5)
        for kc in range(8):
            k.mm(ps[b0][:, 0:384], h[:, kc, :], wd[:, kc, 0:384], kc == 0, kc == 7, [hb, wdb], [psb[b0]])
        for kc in range(8):
            k.mm(ps[b1][:, 0:320], h[:, kc, :], wd[:, kc, 384:704], kc == 0, kc == 7, [hb, wdb], [psb[b1]])
        k.act(junkA[:, 0:384], ps[b0][:, 0:384], AF.Square, [psb[b0]], [junkAb, st2b], accum_out=st2[:, 0:1])
        k.act(junkA[:, 0:256], ps[b1][:, 0:256], AF.Square, [psb[b1]], [junkAb, st2b], accum_out=st2[:, 1:2])
        k.act(junkA[:, 0:64], ps[b1][:, 256:320], AF.Square, [psb[b1]], [junkAb, st2b], accum_out=st2[:, 2:3])
        for c, n in enumerate((384, 256, 64)):
            k.act(st2[:, 3 + c:4 + c], st2[:, c:c + 1], AF.Sqrt, [st2b], [st2b], bias=EPS, scale=1.0 / n)
        k.recip(st2[:, 6:9], st2[:, 3:6], [st2b], [st2b])
        if k.stop == "A1":
            return
        k.act(cqn[:, 0:384], ps[b0][:, 0:384], AF.Copy, [psb[b0], st2b], [cqnb], scale=st2[:, 6:7])
        k.act(cqn[:, 384:640], ps[b1][:, 0:256], AF.Copy, [psb[b1], st2b], [cqnb], scale=st2[:, 7:8])
        if k.stop == "A2":
            return
        k.stt("dve", krf, ps[b1][:, 256:320], st2[:, 8:9], k.pt[:, gt + 320:gt + 384], ALU.mult, ALU.mult,
              [psb[b1], st2b, k.ptb], [krfb])
        emit_rope(k, krb[:, 0:64], krf, rt, ro, i, 1, [krfb], [krbb], rtb)
        k.cp("dve", krb[:, 64:128], krb[:, 0:64], [krbb], [krbb])
        if k.stop == "A3":
            return
        pbf = ps[b2][:].bitcast(BF16)
        for c in range(5):
            k.tr(pbf[:, c * 128:(c + 1) * 128], cqn[:, c * 128:(c + 1) * 128], k.ident[:], [cqnb, k.cb], [psb[b2]])
        k.tr(pbf[:, 640:768], krb, k.ident[:], [krbb, k.cb], [psb[b2]])
        g = k.pf[:, gl:gl + 5]
        k.tt("dve", cT[:, :, i * 128:(i + 1) * 128], pbf[:, 0:640].rearrange("p (c t) -> p c t", t=128),
             g.unsqueeze(2).to_broadcast([128, 5, 128]), ALU.mult, [psb[b2], k.pfb], [cTb[i]])
        if k.stop == "A4":
            return
        k.cp("dve", krT[:, i * 128:(i + 1) * 128], pbf[:, 640:768], [psb[b2]], [krTb[i]])

    emit_norm_T(k, range(NT), lay["norm_mix"] + 8 * l, lambda i: (hTt[i % 2], hTtb[i % 2]), tmp, after=passA, lag=1)
    k.dump("cT", cT, [128, 5, S], BF16, cTb)
    k.dump("krT", krT, [128, S], BF16, krTb)

    if k.stop in ("passA", "A1", "A2", "A3", "A4"):
        return
    k.arena_mark_reset(mark)
    wuq = [k.carve([3, 768], BF16)]
    wukv = [k.carve([2, 1024], BF16)]
    wo = k.carve([4, D], BF16)
    wuqb = [k.buf("wuq")]
    wukvb = [k.buf("wukv")]
    wob = k.buf("wo")
    kT = k.carve([4, S], BF16); kTb = [k.buf("kT") for _ in range(NT)]
    v = k.carve([NT, 512], BF16); vb = [k.buf("v") for _ in range(NT)]
    qTn = [k.carve([4, 512], BF16) for _ in range(2)]; qTnb = [k.buf("qTn") for _ in range(2)]
    qTr = [k.carve([2, 512], BF16) for _ in range(2)]; qTrb = [k.buf("qTr") for _ in range(2)]
    oT = k.carve([4, 512], BF16); oTb = k.buf("oT")
    pT = [k.carve([512], BF16) for _ in range(3)]; pTb = [k.buf("pT") for _ in range(3)]
    rden = k.carve([512], F32); rdenb = k.buf("rden")
    two = lambda shape, dt_: ([k.carve(shape, dt_) for _ in range(2)], [k.buf("tp") for _ in range(2)])
    sq1_, sq1b_ = two([512], F32)
    sq2_, sq2b_ = two([256], F32)
    sq3_, sq3b_ = two([512], F32)
    ssq_, ssqb_ = two([48], F32)
    tq_, tqb_ = two([512], F32)
    tr__, trb_ = two([256], F32)
    trt_, trtb_ = two([256], F32)
    tro_, _ = two([256], F32)
    t3_, t3b_ = two([512], F32)
    qnb__, qnbb_ = two([512], BF16)
    qrb__, qrbb_ = two([256], BF16)
    knb__, knbb_ = two([512], BF16)
    h4 = lambda ap, d: ap.rearrange("p (h d) -> p h d", d=d)
    scale = 192.0 ** -0.5

    def load_group(G):
        sl = 0
        k.dma("pool", wuq[sl], k.dram["mla_wuq"][j, G].rearrange("(kc p) f -> p kc f", p=128), f"wuq{sl}", writes=[wuqb[sl]])
        k.dma("pool", wukv[sl], k.dram["mla_wukv"][j, G].rearrange("(kc p) f -> p kc f", p=128), f"wukv{sl}", writes=[wukvb[sl]])

    def load_wo(G):
        k.dma("pool", wo, k.dram["mla_wo"][j, G * 512:(G + 1) * 512, :].rearrange("(h p) d -> p h d", p=128), "mla_wo", writes=[wob])

    def tile_proj(G, qb, i, part):
        sl = 0
        tt = i - 4 * qb
        qs = qb % 2
        csl = slice(i * 128, (i + 1) * 128)
        pr = i % 2
        sq1, sq1b, sq2, sq2b, sq3, sq3b, ssq, ssqb = sq1_[pr], sq1b_[pr], sq2_[pr], sq2b_[pr], sq3_[pr], sq3b_[pr], ssq_[pr], ssqb_[pr]
        tq, tqb, tr_, trb, trt, trtb, tro, t3, t3b = tq_[pr], tqb_[pr], tr__[pr], trb_[pr], trt_[pr], trtb_[pr], tro_[pr], t3_[pr], t3b_[pr]
        qnb_, qnbb, qrb_, qrbb, knb_, knbb = qnb__[pr], qnbb_[pr], qrb__[pr], qrbb_[pr], knb__[pr], knbb_[pr]
        Bqn, Bqr, Bk, Bv = (0, 1, 2, 3) if pr == 0 else (4, 5, 6, 7)
        if part == 1:
            tile_proj_tail(locals())
            return
        for kc in range(3):
            k.mm(ps[Bqn][:], cT[:, kc, csl], wuq[sl][:, kc, 0:512], kc == 0, kc == 2, [cTb[i], wuqb[sl]], [psb[Bqn]])
        for kc in range(3):
            k.mm(ps[Bqr][:, 0:256], cT[:, kc, csl], wuq[sl][:, kc, 512:768], kc == 0, kc == 2, [cTb[i], wuqb[sl]], [psb[Bqr]])
        for kc in range(2):
            k.mm(ps[Bk][:], cT[:, 3 + kc, csl], wukv[sl][:, kc, 0:512], kc == 0, kc == 1, [cTb[i], wukvb[sl]], [psb[Bk]])
        for kc in range(2):
            k.mm(ps[Bv][:], cT[:, 3 + kc, csl], wukv[sl][:, kc, 512:1024], kc == 0, kc == 1, [cTb[i], wukvb[sl]], [psb[Bv]])
        k.act(v[:, i, :], ps[Bv][:], AF.Copy, [psb[Bv]], [vb[i]])
        k.act(sq1, ps[Bqn][:], AF.Square, [psb[Bqn]], [sq1b])
        k.act(sq2, ps[Bqr][:, 0:256], AF.Square, [psb[Bqr]], [sq2b])
        k.act(sq3, ps[Bk][:], AF.Square, [psb[Bk]], [sq3b])
        k.red(ssq[:, 0:4], h4(sq1, 128), [sq1b], [ssqb])
        k.red(ssq[:, 4:8], h4(sq2, 64), [sq2b], [ssqb])
        k.red(ssq[:, 8:12], h4(sq3, 128), [sq3b], [ssqb])
        k.tt("dve", ssq[:, 12:24], ssq[:, 0:12], k.invn12[:], ALU.mult, [ssqb, k.cb], [ssqb])
        k.act(ssq[:, 24:36], ssq[:, 12:24], AF.Sqrt, [ssqb], [ssqb], bias=EPS, scale=1.0)
        k.recip(ssq[:, 36:48], ssq[:, 24:36], [ssqb], [ssqb])
        rs = ssq[:, 36:48]

    def tile_proj_tail(L):
        i, tt, qs, csl = L["i"], L["tt"], L["qs"], L["csl"]
        sq1, sq1b, sq2, sq2b, sq3, sq3b, ssq, ssqb = (L[n] for n in ("sq1", "sq1b", "sq2", "sq2b", "sq3", "sq3b", "ssq", "ssqb"))
        tq, tqb, tr_, trb, trt, trtb, tro, t3, t3b = (L[n] for n in ("tq", "tqb", "tr_", "trb", "trt", "trtb", "tro", "t3", "t3b"))
        qnb_, qnbb, qrb_, qrbb, knb_, knbb = (L[n] for n in ("qnb_", "qnbb", "qrb_", "qrbb", "knb_", "knbb"))
        Bqn, Bqr, Bk, Bv = L["Bqn"], L["Bqr"], L["Bk"], L["Bv"]
        rs = ssq[:, 36:48]
        k.tt("dve", h4(tq, 128), h4(ps[Bqn][:], 128), rs[:, 0:4].unsqueeze(2).to_broadcast([128, 4, 128]), ALU.mult,
             [psb[Bqn], ssqb], [tqb])
        k.tt("dve", h4(qnb_, 128), h4(tq, 128), k.pt[:, gt:gt + 128].unsqueeze(1).to_broadcast([128, 4, 128]), ALU.mult,
             [tqb, k.ptb], [qnbb])
        k.tt("dve", h4(tr_, 64), h4(ps[Bqr][:, 0:256], 64), rs[:, 4:8].unsqueeze(2).to_broadcast([128, 4, 64]), ALU.mult,
             [psb[Bqr], ssqb], [trb])
        k.tt("dve", h4(tr_, 64), h4(tr_, 64), k.pt[:, gt + 128:gt + 192].unsqueeze(1).to_broadcast([128, 4, 64]), ALU.mult,
             [trb, k.ptb], [trb])
        emit_rope(k, qrb_, tr_, trt, tro, i, 4, [trb], [qrbb], trtb)
        k.tt("dve", h4(t3, 128), h4(ps[Bk][:], 128), rs[:, 8:12].unsqueeze(2).to_broadcast([128, 4, 128]), ALU.mult,
             [psb[Bk], ssqb], [t3b])
        k.tt("dve", h4(knb_, 128), h4(t3, 128), k.pt[:, gt + 192:gt + 320].unsqueeze(1).to_broadcast([128, 4, 128]), ALU.mult,
             [t3b, k.ptb], [knbb])
        p6 = ps[Bqn][:].bitcast(BF16)
        for c in range(4):
            k.tr(p6[:, c * 128:(c + 1) * 128], qnb_[:, c * 128:(c + 1) * 128], k.ident[:], [qnbb, k.cb], [psb[Bqn]])
        for c in range(2):
            k.tr(p6[:, 512 + c * 128:512 + (c + 1) * 128], qrb_[:, c * 128:(c + 1) * 128], k.ident[:], [qrbb, k.cb], [psb[Bqn]])
        k.cp("dve", qTn[qs][:, :, tt * 128:(tt + 1) * 128], h4(p6[:, 0:512], 128), [psb[Bqn]], [qTnb[qs]])
        k.cp("dve", qTr[qs][:, :, tt * 128:(tt + 1) * 128], h4(p6[:, 512:768], 128), [psb[Bqn]], [qTrb[qs]])
        p7 = ps[Bk][:].bitcast(BF16)
        for c in range(4):
            k.tr(p7[:, c * 128:(c + 1) * 128], knb_[:, c * 128:(c + 1) * 128], k.ident[:], [knbb, k.cb], [psb[Bk]])
        k.cp("dve", kT[:, :, csl], h4(p7[:, 0:512], 128), [psb[Bk]], [kTb[i]])

    def attention(G, qb):
        qs = qb % 2
        nkt = 4 * qb + 4
        steps = [(hh, kt) for hh in range(4) for kt in range(nkt)]
        sbank = [2, 3, 4]
        acc = [(0, 1), (5, 6)]

        def qk(si):
            hh, kt = steps[si]
            blk, half = hh % 2, hh // 2
            jd = kt - 4 * qb
            c0 = max(0, jd) * 128
            b = sbank[si % 3]
            ks = slice(kt * 128, (kt + 1) * 128)
            k.mm(ps[b][:, 0:512 - c0], kT[:, hh, ks], qTn[qs][:, hh, c0:512], True, False, [kTb[kt], qTnb[qs]], [psb[b]])
            k.mm(ps[b][:, 0:512 - c0], krT[half * 64:(half + 1) * 64, ks], qTr[qs][half * 64:(half + 1) * 64, blk, c0:512],
                 False, True, [krTb[kt], qTrb[qs]], [psb[b]])
            pb_ = si % 3
            k.act(pT[pb_][:, 0:512 - c0], ps[b][:, 0:512 - c0], AF.Exp, [psb[b]], [pTb[pb_]], scale=scale)
            if jd >= 0:
                blkap = pT[pb_][:, 0:128]
                P.add("pool", lambda e: e.affine_select(out=blkap, in_=blkap, pattern=[[1, 128]], compare_op=ALU.is_ge,
                                                        fill=0.0, base=0, channel_multiplier=-1), [pTb[pb_]], [pTb[pb_]])

        def pv(si):
            hh, kt = steps[si]
            jd = kt - 4 * qb
            c0 = max(0, jd) * 128
            bo, bd = acc[hh % 2]
            pb_ = si % 3
            k.mm(ps[bo][:, c0:512], v[:, kt, hh * 128:(hh + 1) * 128], pT[pb_][:, 0:512 - c0], kt == 0, kt == nkt - 1,
                 [vb[kt], pTb[pb_]], [psb[bo]])
            k.mm(ps[bd][:, c0:512], k.ones_bf[:], pT[pb_][:, 0:512 - c0], kt == 0, kt == nkt - 1,
                 [k.cb, pTb[pb_]], [psb[bd]])
            if kt == nkt - 1:
                k.recip(rden, ps[bd][:], [psb[bd]], [rdenb])
                k.tt("dve", oT[:, hh, :], ps[bo][:], rden, ALU.mult, [psb[bo], rdenb], [oTb])

        for si in range(len(steps) + 2):
            if si < len(steps):
                qk(si)
            if si >= 2:
                pv(si - 2)

    def out_proj(G, qb):
        for tt in range(4):
            i = 4 * qb + tt
            for half in range(2):
                b = (7, 2)[half]
                for hh in range(4):
                    k.mm(ps[b][:], oT[:, hh, tt * 128:(tt + 1) * 128], wo[:, hh, half * 512:(half + 1) * 512],
                         hh == 0, hh == 3, [oTb, wob], [psb[b]])
                xs = k.x[:, i, half * 512:(half + 1) * 512]
                k.tt("dve", xs, xs, ps[b][:], ALU.add, [k.xb[i], psb[b]], [k.xb[i]])

    for G in range(2):
        load_group(G)
        load_wo(G)
        for qb in range(4):
            for i in range(4 * qb, 4 * qb + 4):
                tile_proj(G, qb, i, 0)
                if i > 4 * qb:
                    tile_proj(G, qb, i - 1, 1)
            tile_proj(G, qb, 4 * qb + 3, 1)
            if k.stop == "proj":
                continue
            attention(G, qb)
            if k.stop == "attn":
                continue
            out_proj(G, qb)


def emit_conv(k, l):
    P = k.P
    lay = k.lay
    ps, psb = k.ps, k.psb
    k.arena_reset()
    w2 = k.carve([8, D], BF16); w2b = k.buf("w2")
    k.dma("pool", w2, k.dram["cv_w2"].rearrange("(cc p) d -> p cc d", p=128), "cv_w2", writes=[w2b])
    tmp = norm_tmp(k, [0, 1])
    hTt = [k.carve([8, 512], BF16) for _ in range(2)]; hTtb = [k.buf("hTt") for _ in range(2)]
    w1 = [k.carve([8, 2, 128], BF16) for _ in range(2)]; w1b = [k.buf("w1") for _ in range(2)]
    Dm = [k.carve([31, 128], BF16) for _ in range(2)]; Dmb = [k.buf("Dm") for _ in range(2)]
    uT = k.carve([8, 542], BF16); uTb = [k.buf("uT") for _ in range(8)]
    ysb = k.carve([8, 512], F32); ysbb = [k.buf("ysb") for _ in range(8)]
    ysq = [k.carve([512], F32) for _ in range(2)]; ysqb = [k.buf("ysq") for _ in range(2)]
    sig = [k.carve([512], F32) for _ in range(2)]; sigb = [k.buf("sig") for _ in range(2)]
    zT = k.carve([8, 512], BF16); zTb = [k.buf("zT") for _ in range(8)]
    mean = k.carve([512], F32); msq = k.carve([512], F32); rstd = k.carve([512], F32); stb = k.buf("cvst")
    tn = [k.carve([512], F32) for _ in range(2)]; tnb = [k.buf("tn") for _ in range(2)]
    cb1 = lay["cv_b1"]; cwd = lay["cv_wdw"]; cbd = lay["cv_bdw"]; cg = lay["cv_lng"]; cbn = lay["cv_lnb"]
    w1_d = k.dram["cv_w1"]
    b2t = k.carve([D], F32); b2b = k.buf("b2t")
    k.dma("sp", b2t, k.dram["pt"][:, lay["cv_b2"]:lay["cv_b2"] + D], "cv_b2", writes=[b2b])

    def load_w1(n):
        cc = n % 8
        sl = n % 2
        k.dma("pool", w1[sl][:, :, 0, :], w1_d[:, cc * 128:(cc + 1) * 128].rearrange("(kc p) f -> p kc f", p=128),
              f"cvw1{sl}", writes=[w1b[sl]])
        k.dma("pool", w1[sl][:, :, 1, :], w1_d[:, D + cc * 128:D + (cc + 1) * 128].rearrange("(kc p) f -> p kc f", p=128),
              f"cvw1{sl}", writes=[w1b[sl]])

    load_w1(0)
    n = 0
    for tb in range(4):
        hs = tb % 2
        emit_norm_T(k, range(4 * tb, 4 * tb + 4), lay["norm_mix"] + 8 * l,
                    lambda i: (hTt[hs][:, :, (i % 4) * 128:(i % 4 + 1) * 128], hTtb[hs]), tmp)
        for i in range(4 * tb, 4 * tb + 4):
            k.tt("pool", k.x[:, i, :], k.x[:, i, :], b2t, ALU.add, [k.xb[i], b2b], [k.xb[i]])
        for cc in range(8):
            sl = n % 2
            if n + 1 < 32:
                load_w1(n + 1)
            n += 1
            wv = k.pf[:, cwd + cc * 31:cwd + (cc + 1) * 31]
            k.tt("pool", Dm[sl], k.identf[:].unsqueeze(1).to_broadcast([128, 31, 128]),
                 wv.unsqueeze(2).to_broadcast([128, 31, 128]), ALU.mult, [k.cb, k.pfb], [Dmb[sl]])
            ba, bg, bc = cc % 2, 2 + cc % 2, 4 + cc % 2
            for kc in range(8):
                k.mm(ps[ba][:], w1[sl][:, kc, 0, :], hTt[hs][:, kc, :], kc == 0, kc == 7, [w1b[sl], hTtb[hs]], [psb[ba]])
            for kc in range(8):
                k.mm(ps[bg][:], w1[sl][:, kc, 1, :], hTt[hs][:, kc, :], kc == 0, kc == 7, [w1b[sl], hTtb[hs]], [psb[bg]])
            ss_ = cc % 2
            k.act(sig[ss_], ps[bg][:], AF.Sigmoid, [psb[bg], k.pfb], [sigb[ss_]], bias=k.pf[:, cb1 + 8 + cc:cb1 + 9 + cc], scale=1.0)
            if tb == 0:
                k.memset("pool", uT[:, cc, 0:30], 0.0, [uTb[cc]])
            else:
                k.cp("pool", uT[:, cc, 0:30], uT[:, cc, 512:542], [uTb[cc]], [uTb[cc]])
            k.stt("dve", uT[:, cc, 30:542], ps[ba][:], k.pf[:, cb1 + cc:cb1 + cc + 1], sig[ss_], ALU.add, ALU.mult,
                  [psb[ba], sigb[ss_], k.pfb], [uTb[cc]])
            for jj in range(31):
                k.mm(ps[bc][:], Dm[sl][:, jj, :], uT[:, cc, jj:jj + 512], jj == 0, jj == 30, [Dmb[sl], uTb[cc]], [psb[bc]])
            bdw = k.pf[:, cbd + cc:cbd + cc + 1]
            k.act(ysb[:, cc, :], ps[bc][:], AF.Identity, [psb[bc], k.pfb], [ysbb[cc]], bias=bdw, scale=1.0)
            k.act(ysq[ss_], ps[bc][:], AF.Square, [psb[bc], k.pfb], [ysqb[ss_]], bias=bdw, scale=1.0)
            k.mm(ps[6][:], k.ones_f[:], ysb[:, cc, :], cc == 0, cc == 7, [k.cb, ysbb[cc]], [psb[6]])
            k.mm(ps[7][:], k.ones_f[:], ysq[ss_], cc == 0, cc == 7, [k.cb, ysqb[ss_]], [psb[7]])
        k.act(mean, ps[6][:], AF.Copy, [psb[6]], [stb], scale=1.0 / D)
        k.act(msq, ps[6][:], AF.Square, [psb[6]], [stb], scale=1.0 / D)
        k.stt("dve", rstd, ps[7][:], 1.0 / D, msq, ALU.mult, ALU.subtract, [psb[7], stb], [stb])
        k.act(rstd, rstd, AF.Sqrt, [stb], [stb], bias=EPS, scale=1.0)
        k.recip(rstd, rstd, [stb], [stb])
        for cc in range(8):
            ts_ = cc % 2
            k.tt("pool", tn[ts_], ysb[:, cc, :], mean, ALU.subtract, [ysbb[cc], stb], [tnb[ts_]])
            k.tt("dve", tn[ts_], tn[ts_], rstd, ALU.mult, [tnb[ts_], stb], [tnb[ts_]])
            k.act(zT[:, cc, :], tn[ts_], AF.Silu, [tnb[ts_], k.pfb], [zTb[cc]],
                  bias=k.pf[:, cbn + cc:cbn + cc + 1], scale=k.pf[:, cg + cc:cg + cc + 1])
        for tt in range(4):
            i = 4 * tb + tt
            for half in range(2):
                b = (2, 3)[half]
                for cc in range(8):
                    k.mm(ps[b][:], zT[:, cc, tt * 128:(tt + 1) * 128], w2[:, cc, half * 512:(half + 1) * 512],
                         cc == 0, cc == 7, [zTb[cc], w2b], [psb[b]])
                xs = k.x[:, i, half * 512:(half + 1) * 512]
                k.tt("dve", xs, xs, ps[b][:], ALU.add, [k.xb[i], psb[b]], [k.xb[i]])


def emit_hgrn_consts(k, l):
    lay = k.lay
    hgc = k.hgc
    cbs = [k.cb]
    lg = k.pf[:, lay["hg_lb"]:lay["hg_lb"] + 32]
    e = hgc[:, 0:32]
    k.act(e, lg, AF.Exp, [k.pfb], cbs)
    den = hgc[:, 32:40]
    k.tt("dve", den, e[:, 0:8], e[:, 8:16], ALU.add, cbs, cbs)
    k.tt("dve", den, den, e[:, 16:24], ALU.add, cbs, cbs)
    k.tt("dve", den, den, e[:, 24:32], ALU.add, cbs, cbs)
    k.recip(den, den, cbs, cbs)
    num = hgc[:, 40:48]
    k.memset("dve", num, 0.0, cbs)
    for i in range(1, l + 1):
        k.tt("dve", num, num, e[:, 8 * i:8 * i + 8], ALU.add, cbs, cbs)
    k.tt("dve", hgc[:, 48:56], num, den, ALU.mult, cbs, cbs)
    k.ts("dve", hgc[:, 56:64], hgc[:, 48:56], -1.0, 1.0, ALU.mult, ALU.add, cbs, cbs)
    k.ts("dve", hgc[:, 64:72], hgc[:, 56:64], -1.0, None, ALU.mult, None, cbs, cbs)
    k.memset("pool", k.mask2[:], 1.0, cbs)
    k.P.add("pool", lambda e_: e_.affine_select(out=k.mask2[:], in_=k.mask2[:], pattern=[[1, 128]], compare_op=ALU.is_ge,
                                                 fill=0.0, base=0, channel_multiplier=-1), cbs, cbs)
    k.memset("pool", k.mask2[0:64, 64:128], 0.0, cbs)


def emit_hgrn(k, l):
    P = k.P
    lay = k.lay
    ps, psb = k.ps, k.psb
    k.arena_reset()
    hT = k.carve([8, S], BF16); hTb = [k.buf("hT") for _ in range(4)]
    tmp = norm_tmp(k, [6, 7])
    Wp = [k.carve([8, 4, 256], BF16) for _ in range(2)]; Wpb = [k.buf("Wp") for _ in range(2)]
    wop = [k.carve([2, D], BF16) for _ in range(2)]; wopb = [k.buf("wop") for _ in range(2)]
    F = lambda: k.carve([512], F32)
    sg, f_, b_, bp, Em, t1, t1e, t2, on = [F() for _ in range(9)]
    sgb, fb, bb, bpb, Emb, t1b, t1eb, t2b, onb = [k.buf("hg") for _ in range(9)]
    two = lambda shape, dt_: ([k.carve(shape, dt_) for _ in range(2)], [k.buf("hg2") for _ in range(2)])
    Ep_, Epb_ = two([512], F32)
    gate_, gateb_ = two([512], F32)
    kT__, kTb__ = two([512], BF16)
    qT__, qTb__ = two([512], BF16)
    vT__, vTb__ = two([512], BF16)
    ktm_, ktmb_ = two([4, 128], BF16)
    vtm_, vtmb_ = two([4, 128], BF16)
    em_, emb_ = two([8], F32)
    el_, _ = two([8], F32)
    onT = [k.carve([512], BF16) for _ in range(2)]; onTb = [k.buf("onT") for _ in range(2)]
    Am = [k.carve([128], BF16) for _ in range(2)]; Amb = [k.buf("Am") for _ in range(2)]
    Sst = [k.carve([128], F32) for _ in range(2)]; Sb = [k.buf("S") for _ in range(2)]
    Stil = k.carve([128], BF16); Stilb = k.buf("Stil")
    tmpS = k.carve([128], F32); tmpSb = k.buf("tmpS")
    scanmask = k.carve([512], F32); smb = k.buf("scanmask")
    k.memset("dve", scanmask, 1.0, [smb])
    k.memset("dve", scanmask.rearrange("p (c t) -> p c t", t=64)[:, :, 0:1], 0.0, [smb])
    hc = k.hgc
    w_d = k.dram["hg_wi"]
    wo_d = k.dram["hg_wo"]
    c8 = lambda ap: ap.rearrange("p (c t) -> p c t", t=64)

    emit_norm_T(k, range(NT), lay["norm_mix"] + 8 * l, lambda i: (hT[:, :, i * 128:(i + 1) * 128], hTb[i // 4]), tmp)

    def load(hp):
        sl = hp % 2
        for kind in range(4):
            k.dma("pool", Wp[sl][:, :, kind, :],
                  w_d[:, kind * D + hp * 256:kind * D + (hp + 1) * 256].rearrange("(kc p) f -> p kc f", p=128),
                  f"hgw{sl}", writes=[Wpb[sl]])
        k.dma("pool", wop[sl], wo_d[hp * 256:(hp + 1) * 256, :].rearrange("(hh p) d -> p hh d", p=128),
              f"hgwo{sl}", writes=[wopb[sl]])

    rot = [0]

    def proj(sl, kind, hh, tb, bank):
        for kc in range(8):
            k.mm(ps[bank][:], Wp[sl][:, kc, kind, hh * 128:(hh + 1) * 128], hT[:, kc, tb * 512:(tb + 1) * 512],
                 kc == 0, kc == 7, [Wpb[sl], hTb[tb]], [psb[bank]])

    go = k.pf[:, lay["hg_on"]:lay["hg_on"] + 1]

    def pro(sl, hp, tb, hh):
        hcol = 2 * hp + hh
        lb = hc[:, 48 + hcol:49 + hcol]
        oml = hc[:, 56 + hcol:57 + hcol]
        noml = hc[:, 64 + hcol:65 + hcol]
        Ep, Epb, gate, gateb = Ep_[hh], Epb_[hh], gate_[hh], gateb_[hh]
        kT_, kTb_, qT_, qTb_, vT_, vTb_ = kT__[hh], kTb__[hh], qT__[hh], qTb__[hh], vT__[hh], vTb__[hh]
        ktm, ktmb, vtm, vtmb, em, emb, el = ktm_[hh], ktmb_[hh], vtm_[hh], vtmb_[hh], em_[hh], emb_[hh], el_[hh]
        bq = hh
        proj(sl, 0, hh, tb, bq)
        yield
        proj(sl, 1, hh, tb, 2)
        yield
        proj(sl, 2, hh, tb, 3)
        yield
        k.act(sg, ps[2][:], AF.Sigmoid, [psb[2]], [sgb])
        yield
        proj(sl, 3, hh, tb, 2)
        yield
        k.ts("dve", f_, sg, oml, lb, ALU.mult, ALU.add, [sgb, k.cb], [fb])
        yield
        k.act(f_, f_, AF.Ln, [fb], [fb])
        yield
        P.add("dve", lambda e: e.tensor_tensor_scan(out=b_, data0=scanmask, data1=f_, initial=0.0,
                                                    op0=ALU.mult, op1=ALU.add), [fb, smb], [bb])
        k.tt("dve", c8(bp), c8(b_), c8(b_)[:, :, 31:32].to_broadcast([128, 8, 64]), ALU.subtract, [bb], [bpb])
        yield
        k.act(Ep, bp, AF.Exp, [bpb], [Epb])
        yield
        k.act(Em, bp, AF.Exp, [bpb], [Emb], scale=-1.0)
        yield
        k.act(em, c8(b_)[:, :, 31], AF.Exp, [bb], [emb])
        yield
        k.act(el, c8(b_)[:, :, 63], AF.Exp, [bb], [emb])
        yield
        k.ts("dve", t1, sg, noml, oml, ALU.mult, ALU.add, [sgb, k.cb], [t1b])
        yield
        k.tt("pool", kT_, t1, Em, ALU.mult, [t1b, Emb], [kTb_])
        yield
        k.tt("dve", qT_, ps[bq][:], Ep, ALU.mult, [psb[bq], Epb], [qTb_])
        yield
        k.act(vT_, ps[3][:], AF.Copy, [psb[3]], [vTb_])
        yield
        k.act(gate, ps[2][:], AF.Silu, [psb[2]], [gateb])
        yield
        p6 = ps[6][:].bitcast(BF16)
        for tt in range(4):
            k.tr(p6[:, tt * 128:(tt + 1) * 128], kT_[:, tt * 128:(tt + 1) * 128], k.ident[:], [kTb_, k.cb], [psb[6]])
            yield
        for tt in range(4):
            k.tr(p6[:, 512 + tt * 128:512 + (tt + 1) * 128], vT_[:, tt * 128:(tt + 1) * 128], k.ident[:], [vTb_, k.cb], [psb[6]])
            yield
        k.cp("dve", ktm, p6[:, 0:512].rearrange("p (a b) -> p a b", b=128), [psb[6]], [ktmb])
        yield
        k.cp("dve", vtm, p6[:, 512:1024].rearrange("p (a b) -> p a b", b=128), [psb[6]], [vtmb])
        yield

    def loop_epi(hh):
        Ep, Epb, gate, gateb = Ep_[hh], Epb_[hh], gate_[hh], gateb_[hh]
        kT_, kTb_, qT_, qTb_ = kT__[hh], kTb__[hh], qT__[hh], qTb__[hh]
        ktm, ktmb, vtm, vtmb, em, emb, el = ktm_[hh], ktmb_[hh], vtm_[hh], vtmb_[hh], em_[hh], emb_[hh], el_[hh]
        e2 = c8(Ep)[:, :, 63]
        for tt in range(4):
            tsl = slice(tt * 128, (tt + 1) * 128)
            a = tt % 2
            k.mm(ps[4][:, 0:128], kT_[:, tsl], qT_[:, tsl], True, True, [kTb_, qTb_], [psb[4]])
            k.tt("dve", Am[a], ps[4][:, 0:128], k.mask2[:], ALU.mult, [psb[4], k.cb], [Amb[a]])
            k.mm(ps[7][:, tsl], vtm[:, tt, :], Am[a], True, False, [vtmb, Amb[a]], [psb[7]])
            for half in range(2):
                c = 2 * tt + half
                hs = slice(half * 64, (half + 1) * 64)
                csl = slice(tt * 128 + half * 64, tt * 128 + (half + 1) * 64)
                k.act(Stil, Sst[hh], AF.Copy, [Sb[hh], emb], [Stilb], scale=em[:, c:c + 1])
                k.mm(ps[7][:, csl], Stil, qT_[:, csl], False, half == 1, [Stilb, qTb_], [psb[7]])
                k.mm(ps[5][:, 0:128], ktm[hs, tt, :], vtm[hs, tt, :], True, True, [ktmb, vtmb], [psb[5]])
                k.ts("dve", tmpS, ps[5][:, 0:128], e2[:, c:c + 1], None, ALU.mult, None, [psb[5], Epb], [tmpSb])
                k.stt("dve", Sst[hh], Sst[hh], el[:, c:c + 1], tmpS, ALU.mult, ALU.add, [Sb[hh], emb, tmpSb], [Sb[hh]])
                yield
        k.act(t1e, ps[7][:], AF.Square, [psb[7]], [t1eb])
        yield
        k.mm(ps[4][:], k.ones_f[:], t1e, True, True, [k.cb, t1eb], [psb[4]])
        yield
        k.act(t2, ps[4][:], AF.Sqrt, [psb[4]], [t2b], bias=EPS, scale=1.0 / 128)
        yield
        k.recip(t2, t2, [t2b], [t2b])
        yield
        k.stt("dve", on, ps[7][:], go, t2, ALU.mult, ALU.mult, [psb[7], t2b, k.pfb], [onb])
        yield
        k.tt("pool", onT[hh], on, gate, ALU.mult, [onb, gateb], [onTb[hh]])
        yield

    def outp(sl, tb):
        for tt in range(4):
            i = 4 * tb + tt
            for half in range(2):
                bnk = (2, 3)[half]
                for hh in range(2):
                    k.mm(ps[bnk][:], onT[hh][:, tt * 128:(tt + 1) * 128], wop[sl][:, hh, half * 512:(half + 1) * 512],
                         hh == 0, hh == 1, [onTb[hh], wopb[sl]], [psb[bnk]])
                xs = k.x[:, i, half * 512:(half + 1) * 512]
                k.tt("dve", xs, xs, ps[bnk][:], ALU.add, [k.xb[i], psb[bnk]], [k.xb[i]])

    load(0)
    for hp in range(4):
        sl = hp % 2
        if hp + 1 < 4:
            load(hp + 1)
        for hh in range(2):
            k.memset("pool", Sst[hh], 0.0, [Sb[hh]])
        units = [(tb, hh) for tb in range(4) for hh in range(2)]
        for _ in pro(sl, hp, *units[0]):
            pass
        for n, (tb, hh) in enumerate(units):
            ga = loop_epi(hh)
            gb = pro(sl, hp, *units[n + 1]) if n + 1 < len(units) else None
            alive_a, alive_b = True, gb is not None
            while alive_a or alive_b:
                if alive_a:
                    try:
                        next(ga)
                    except StopIteration:
                        alive_a = False
                if alive_b:
                    for _ in range(2):
                        try:
                            next(gb)
                        except StopIteration:
                            alive_b = False
                            break
            if hh == 1:
                outp(sl, tb)

def fm(v):
    v = np.asarray(v, np.float32)
    return np.ascontiguousarray(v.reshape(-1, 128).T)


def pack_inputs(inp):
    cols = []
    lay = {}

    def put(name, arr):
        lay[name] = sum(c.shape[1] for c in cols)
        cols.append(np.asarray(arr, np.float32))

    put("norm_mix", np.concatenate([fm(inp["norm_mix"][l]) for l in range(4)], axis=1))
    put("norm_mlp", np.concatenate([fm(inp["norm_mlp"][l]) for l in range(4)], axis=1))
    put("mla_lat", np.concatenate([np.concatenate([fm(inp["mla_q_lat_norm"][j]), fm(inp["mla_kv_lat_norm"][j])], axis=1)
                                   for j in range(2)], axis=1))
    put("hg_lb", np.concatenate([fm(inp["hg_lb_logits"][i]) for i in range(4)], axis=1))
    put("hg_on", fm(inp["hg_out_norm"][0]))
    put("cv_b1", fm(inp["cv_b_pw1"][0]))
    put("cv_wdw", np.asarray(inp["cv_w_dw"][0], np.float32).T.reshape(8, 128, 31).transpose(1, 0, 2).reshape(128, 8 * 31))
    put("cv_bdw", fm(inp["cv_b_dw"][0]))
    put("cv_lng", fm(inp["cv_ln_g"][0]))
    put("cv_lnb", fm(inp["cv_ln_b"][0]))
    pf = np.ascontiguousarray(np.concatenate(cols, axis=1))
    lay["npf"] = pf.shape[1]
    tcols = []

    def putt(name, vec):
        lay[name] = sum(c.shape[0] for c in tcols)
        tcols.append(np.asarray(vec, np.float32).reshape(-1))

    putt("mla_head", np.concatenate([np.concatenate([inp["mla_q_head_norm"][j], inp["mla_k_head_norm"][j]]) for j in range(2)]))
    lay["npt_res"] = sum(c.shape[0] for c in tcols)
    putt("cv_b2", inp["cv_b_pw2"][0])
    ptv = np.concatenate(tcols)
    pt = np.ascontiguousarray(np.broadcast_to(ptv[None, :], (128, ptv.shape[0])))
    lay["npt"] = pt.shape[1]
    return pf, pt, lay


def pack_weights(inp):
    w = {}
    w["mlp_wi"] = np.asarray(inp["mlp_w_in"], np.float32)
    w["mlp_wo"] = np.asarray(inp["mlp_w_out"], np.float32)
    w["mla_wd"] = np.asarray(inp["mla_w_down"], np.float32)
    wuq = np.asarray(inp["mla_w_uq"], np.float32).reshape(2, 384, 8, 192)
    wukv = np.asarray(inp["mla_w_ukv"], np.float32).reshape(2, 256, 8, 256)
    uq = np.empty((2, 2, 384, 768), np.float32)
    ukv = np.empty((2, 2, 256, 1024), np.float32)
    for G in range(2):
        hs = [4 * G + i for i in range(4)]
        uq[:, G, :, 0:512] = wuq[:, :, hs, 0:128].reshape(2, 384, 512)
        rope_order = [4 * G + 0, 4 * G + 2, 4 * G + 1, 4 * G + 3]
        uq[:, G, :, 512:768] = wuq[:, :, rope_order, 128:192].reshape(2, 384, 256)
        ukv[:, G, :, 0:512] = wukv[:, :, hs, 0:128].reshape(2, 256, 512)
        ukv[:, G, :, 512:1024] = wukv[:, :, hs, 128:256].reshape(2, 256, 512)
    w["mla_wuq"] = uq
    w["mla_wukv"] = ukv
    w["mla_wo"] = np.asarray(inp["mla_w_o"], np.float32)
    w["cv_w1"] = np.asarray(inp["cv_w_pw1"][0], np.float32)
    w["cv_w2"] = np.asarray(inp["cv_w_pw2"][0], np.float32)
    w["hg_wi"] = np.asarray(inp["hg_w_in"][0], np.float32)
    w["hg_wo"] = np.asarray(inp["hg_w_o"][0], np.float32)
    return w


ALL_LAYERS = [("mla", 0, 0), ("mlp", 0, 0), ("hgrn", 1, 0), ("mlp", 1, 0),
              ("conv", 2, 0), ("mlp", 2, 0), ("mla", 3, 1), ("mlp", 3, 0)]


def run(inp, layers, n_cores=N_CORES, n_seq=2, trace=False, debug=False, stop=None):
    pf, pt, lay = pack_inputs(inp)
    nc, stats = build_program(n_seq, layers, lay, debug, stop)
    x = np.asarray(inp["x"], np.float32)
    pos = np.asarray(inp["positions"], np.int32)
    wts = pack_weights(inp)
    in_maps = []
    for c in range(n_cores):
        sl = slice(c * n_seq, (c + 1) * n_seq)
        m = dict(
            x=np.ascontiguousarray(x[sl]),
            pos=np.ascontiguousarray(pos[sl].reshape(n_seq, NT, 128).transpose(0, 2, 1)),
            pf=pf, pt=pt, **wts,
        )
        in_maps.append(m)
    res = run_bass_kernel_spmd(nc, in_maps, core_ids=list(range(n_cores)), **({"trace": True} if trace else {}))
    out = np.concatenate([r["out"] for r in res.results], axis=0)
    return out, res, stats


def kernel(**inputs):
    out, _, _ = run(inputs, ALL_LAYERS)
    return out.astype(np.float32)
```

```python
import math
import numpy as np
from contextlib import ExitStack
from functools import partial

import concourse.bass as bass
import concourse.mybir as mybir
from concourse.bass_utils import run_bass_kernel_spmd

F32 = mybir.dt.float32
BF16 = mybir.dt.bfloat16
I32 = mybir.dt.int32
AF = mybir.ActivationFunctionType
ALU = mybir.AluOpType
AX = mybir.AxisListType

S = 2048
D = 1024
NT = 16
DFF = 4096
EPS = 1e-6
N_CORES = 8
ENGS = ("pe", "act", "dve", "pool", "sp")


class Buf:
    __slots__ = ("name", "last_w", "readers")

    def __init__(self, name):
        self.name = name
        self.last_w = None
        self.readers = []


class Prog:
    def __init__(self, nc):
        self.nc = nc
        self.ins = []
        self.last_on_eng = {e: None for e in ENGS}
        self.last_dma = {}
        self.pending_fence = {e: None for e in ENGS}

    def add(self, eng, fn, reads=(), writes=(), dma=None):
        i = len(self.ins)
        deps = set()
        for b in reads:
            if b.last_w is not None:
                deps.add(b.last_w)
        for b in writes:
            if b.last_w is not None:
                deps.add(b.last_w)
            deps.update(b.readers)
        if self.pending_fence[eng] is not None:
            deps |= self.pending_fence[eng]
            self.pending_fence[eng] = None
        self.ins.append(dict(eng=eng, fn=fn, deps=deps, dma=dma, sig=False))
        for b in reads:
            b.readers.append(i)
        for b in writes:
            b.last_w = i
            b.readers = []
        self.last_on_eng[eng] = i
        if dma is not None:
            self.last_dma[dma] = i
        return i

    def fence(self):
        s = set(v for v in self.last_on_eng.values() if v is not None)
        s |= set(self.last_dma.values())
        for e in ENGS:
            self.pending_fence[e] = set(s) | (self.pending_fence[e] or set())

    def emit(self, es, final_wait_groups=()):
        nc = self.nc
        ins = self.ins
        for r in ins:
            nd = set()
            for d in r["deps"]:
                p = ins[d]
                if p["dma"] is None and r["dma"] is None and p["eng"] == r["eng"] and r["eng"] == "pe":
                    continue
                nd.add(d)
            r["deps"] = nd
            for d in nd:
                ins[d]["sig"] = True
        eng_sem = {e: es.enter_context(nc.semaphore("s_" + e)) for e in ("pe", "act", "dve", "pool")}
        grp_sem = {}
        for r in ins:
            if r["dma"] is not None and r["dma"] not in grp_sem:
                grp_sem[r["dma"]] = es.enter_context(nc.semaphore("g_" + r["dma"]))
        cnt = {e: 0 for e in eng_sem}
        gcnt = {g: 0 for g in grp_sem}
        for r in ins:
            if r["dma"] is not None:
                gcnt[r["dma"]] += 16
                r["tok"] = ("g", r["dma"], gcnt[r["dma"]])
            elif r["sig"]:
                cnt[r["eng"]] += 1
                r["tok"] = ("e", r["eng"], cnt[r["eng"]])
        gtot = {g: 0 for g in grp_sem}
        per_eng = {e: [] for e in ENGS}
        known = {e: {} for e in ENGS}
        for r in ins:
            waits = {}
            for d in r["deps"]:
                kind, key, val = ins[d]["tok"]
                if kind == "g":
                    val = max(val, gtot[key])
                k = (kind, key)
                waits[k] = max(waits.get(k, 0), val)
            if r["dma"] is not None:
                gtot[r["dma"]] += 16
            kn = known[r["eng"]]
            wl = []
            for k, v in waits.items():
                if kn.get(k, 0) >= v:
                    continue
                kn[k] = v
                wl.append((k, v))
            per_eng[r["eng"]].append((r, wl))
        self.stats = dict(n={e: len(per_eng[e]) for e in ENGS}, sem=dict(cnt), nsem=len(grp_sem) + 4)

        def semof(k):
            return eng_sem[k[1]] if k[0] == "e" else grp_sem[k[1]]

        def run(engname, eobj):
            for r, wl in per_eng[engname]:
                for k, v in wl:
                    eobj.wait_ge(semof(k), v)
                bi = r["fn"](eobj)
                if r["dma"] is not None:
                    bi.then_inc(grp_sem[r["dma"]], 16)
                elif r["sig"]:
                    bi.then_inc(eng_sem[r["eng"]], 1)
            if engname == "sp":
                for g in final_wait_groups:
                    eobj.wait_ge(grp_sem[g], gcnt[g])

        with nc.Block() as block:
            @block.tensor
            def _(e):
                run("pe", e)

            @block.scalar
            def _(e):
                run("act", e)

            @block.vector
            def _(e):
                run("dve", e)

            @block.gpsimd
            def _(e):
                run("pool", e)

            @block.sync
            def _(e):
                run("sp", e)


class K:
    def __init__(self, nc, es, n_seq):
        self.nc = nc
        self.es = es
        self.P = Prog(nc)
        self.n_seq = n_seq
        self.uid = 0
        self.debug = False
        self.stop = None
        self.dbg_names = []

    def sb(self, name, shape, dt):
        return self.es.enter_context(self.nc.sbuf_tensor("sb_" + name, shape, dt))

    def mm(self, out, lhsT, rhs, start, stop, reads, writes):
        self.P.add("pe", lambda e: e.matmul(out, lhsT=lhsT, rhs=rhs, start=start, stop=stop), reads, writes)

    def tr(self, out, in_, ident, reads, writes):
        self.P.add("pe", lambda e: e.transpose(out=out, in_=in_, identity=ident), reads, writes)

    def act(self, out, in_, func, reads, writes, **kw):
        self.P.add("act", lambda e: e.activation(out=out, in_=in_, func=func, **kw), reads, writes)

    def tt(self, eng, out, in0, in1, op, reads, writes):
        self.P.add(eng, lambda e: e.tensor_tensor(out=out, in0=in0, in1=in1, op=op), reads, writes)

    def ts(self, eng, out, in0, s1, s2, op0, op1, reads, writes):
        if s2 is None:
            self.P.add(eng, lambda e: e.tensor_scalar(out=out, in0=in0, scalar1=s1, scalar2=None, op0=op0), reads, writes)
        else:
            self.P.add(eng, lambda e: e.tensor_scalar(out=out, in0=in0, scalar1=s1, scalar2=s2, op0=op0, op1=op1), reads, writes)

    def stt(self, eng, out, in0, scalar, in1, op0, op1, reads, writes):
        self.P.add(eng, lambda e: e.scalar_tensor_tensor(out=out, in0=in0, scalar=scalar, in1=in1, op0=op0, op1=op1), reads, writes)

    def cp(self, eng, out, in_, reads, writes):
        self.P.add(eng, lambda e: e.tensor_copy(out=out, in_=in_), reads, writes)

    def recip(self, out, in_, reads, writes):
        self.P.add("dve", lambda e: e.reciprocal(out=out, in_=in_), reads, writes)

    def memset(self, eng, ap, val, writes):
        self.P.add(eng, lambda e: e.memset(ap, val), (), writes)

    def dma(self, eng, out, in_, grp, reads=(), writes=()):
        self.P.add(eng, lambda e: e.dma_start(out=out, in_=in_), reads, writes, dma=grp)

    def arena_reset(self):
        self.P.fence()
        self.aoff = 0

    def carve(self, free_shape, dt):
        n = int(np.prod(free_shape))
        nbytes = n * (4 if dt in (F32, I32) else 2)
        nbytes = (nbytes + 63) // 64 * 64
        assert self.aoff + nbytes <= self.arena_bytes, (self.aoff, nbytes, self.arena_bytes)
        ap = self.arena[:, self.aoff // 2:(self.aoff + nbytes) // 2]
        self.aoff += nbytes
        if dt != BF16:
            ap = ap.bitcast(dt)
        ap = ap[:, 0:n]
        if len(free_shape) == 2:
            ap = ap.rearrange("p (a b) -> p a b", b=free_shape[1])
        elif len(free_shape) == 3:
            ap = ap.rearrange("p (a b c) -> p a b c", b=free_shape[1], c=free_shape[2])
        return ap

    def buf(self, name):
        self.uid += 1
        return Buf(f"{name}_{self.uid}")

    def arena_mark_reset(self, mark):
        self.P.fence()
        self.aoff = mark

    def red(self, out, in_, reads, writes):
        self.P.add("dve", lambda e: e.tensor_reduce(out=out, in_=in_, axis=AX.X, op=ALU.add), reads, writes)

    def dump(self, name, ap, shape, dt, reads):
        if not getattr(self, "debug", False) or ("dbg_" + name) in self.dbg_names:
            return
        d = self.nc.dram_tensor("dbg_" + name, list(shape), dt, kind="ExternalOutput").ap()
        self.dma("sp", d, ap, "dbg", reads=reads)
        self.dbg_names.append("dbg_" + name)


def build_program(n_seq, layers, lay, debug=False, stop=None):
    nc = bass.Bass("TRN2", target_bir_lowering=False)
    es = ExitStack()
    k = K(nc, es, n_seq)
    k.debug = debug
    k.stop = stop
    P = k.P
    dt = lambda name, shape, d, kind="ExternalInput": nc.dram_tensor(name, shape, d, kind=kind).ap()
    x_d = dt("x", [n_seq, S, D], F32)
    out_d = dt("out", [n_seq, S, D], F32, "ExternalOutput")
    pos_d = dt("pos", [n_seq, 128, NT], I32)
    pf_d = dt("pf", [128, lay["npf"]], F32)
    pt_d = dt("pt", [128, lay["npt"]], F32)
    mlp_wi_d = dt("mlp_wi", [4, D, DFF], F32)
    mlp_wo_d = dt("mlp_wo", [4, DFF, D], F32)
    k.dram = dict(x=x_d, out=out_d, pos=pos_d, pf=pf_d, pt=pt_d, mlp_wi=mlp_wi_d, mlp_wo=mlp_wo_d)
    k.dram["mla_wd"] = dt("mla_wd", [2, D, 704], F32)
    k.dram["mla_wuq"] = dt("mla_wuq", [2, 2, 384, 768], F32)
    k.dram["mla_wukv"] = dt("mla_wukv", [2, 2, 256, 1024], F32)
    k.dram["mla_wo"] = dt("mla_wo", [2, D, D], F32)
    k.dram["cv_w1"] = dt("cv_w1", [D, 2 * D], F32)
    k.dram["cv_w2"] = dt("cv_w2", [D, D], F32)
    k.dram["hg_wi"] = dt("hg_wi", [D, 4 * D], F32)
    k.dram["hg_wo"] = dt("hg_wo", [D, D], F32)
    k.lay = lay

    k.x = k.sb("x", [128, NT, D], F32)
    k.xb = [Buf(f"x{i}") for i in range(NT)]
    k.pf = k.sb("pf", [128, lay["npf"]], F32)
    k.pfb = Buf("pf")
    k.ident = k.sb("ident", [128, 128], BF16)
    k.identf = k.sb("identf", [128, 128], F32)
    k.cb = Buf("consts")
    k.ss = k.sb("ss", [128, 64], F32)
    k.ps = [es.enter_context(nc.psum_tensor(f"ps{i}", [128, 512], F32)) for i in range(8)]
    k.psb = [Buf(f"ps{i}") for i in range(8)]
    k.pt = k.sb("pt", [128, lay["npt_res"]], F32)
    k.ptb = Buf("pt")
    k.ones_bf = k.sb("ones_bf", [128, 128], BF16)
    k.invn12 = k.sb("invn12", [128, 12], F32)
    k.ones_f = k.sb("ones_f", [128, 128], F32)
    k.hgc = k.sb("hgc", [128, 72], F32)
    k.mask2 = k.sb("mask2", [128, 128], F32)
    k.negpi = k.sb("negpi", [128, 1], F32)
    k.rope_invf = k.sb("rope_invf", [128, 32], F32)
    k.rope_posi = k.sb("rope_posi", [128, NT], I32)
    k.rope_posf = k.sb("rope_posf", [128, NT], F32)
    k.cos2 = k.sb("cos2", [128, NT, 64], F32)
    k.sinA = k.sb("sinA", [128, NT, 64], F32)
    k.ropeb = Buf("rope")
    k.arena_bytes = 128 * 1024
    k.arena = k.sb("arena", [128, k.arena_bytes // 2], BF16)
    k.aoff = 0

    k.dma("sp", k.pf[:], pf_d, "pf", writes=[k.pfb])
    k.memset("pool", k.identf[:], 0.0, [k.cb])
    P.add("pool", lambda e: e.affine_select(out=k.identf[:], in_=k.identf[:], pattern=[[-1, 128]],
                                            compare_op=ALU.not_equal, fill=1.0, base=0, channel_multiplier=1),
          [k.cb], [k.cb])
    k.cp("dve", k.ident[:], k.identf[:], [k.cb], [k.cb])
    k.dma("sp", k.pt[:], pt_d[:, 0:lay["npt_res"]], "pt", writes=[k.ptb])
    k.memset("dve", k.ones_bf[:], 1.0, [k.cb])
    k.memset("dve", k.ones_f[:], 1.0, [k.cb])
    k.memset("dve", k.invn12[:, 0:4], 1.0 / 128, [k.cb])
    k.memset("dve", k.invn12[:, 4:8], 1.0 / 64, [k.cb])
    k.memset("dve", k.invn12[:, 8:12], 1.0 / 128, [k.cb])
    k.memset("dve", k.negpi[:], -math.pi * (1.0 - 1e-6), [k.cb])
    for f in range(32):
        k.memset("pool", k.rope_invf[:, f:f + 1], float(np.float32(10000.0) ** np.float32(-2.0 * f / 64)), [k.cb])

    for (kind, l, j) in layers:
        if kind == "hgrn":
            emit_hgrn_consts(k, l)
    for s in range(n_seq):
        for i in range(NT):
            k.dma("sp", k.x[:, i, :], x_d[s, i * 128:(i + 1) * 128, :], "xio", writes=[k.xb[i]])
        if any(kd == "mla" for kd, _, _ in layers):
            emit_rope_tables(k, s)
        for (kind, l, j) in layers:
            if kind == "mlp":
                emit_mlp(k, l)
            elif kind == "mla":
                emit_mla(k, l, j)
            elif kind == "conv":
                emit_conv(k, l)
            elif kind == "hgrn":
                emit_hgrn(k, l)
            else:
                raise ValueError(kind)
        for i in range(NT):
            k.dma("sp", out_d[s, i * 128:(i + 1) * 128, :], k.x[:, i, :], "xio", reads=[k.xb[i]])
    P.emit(es, final_wait_groups=["xio"] + (["dbg"] if k.dbg_names else []))
    es.close()
    return nc, P.stats


def emit_norm_T(k, tiles, gcol, dst, tmp, after=None, lag=0):
    tiles = list(tiles)
    for n, i in enumerate(tiles):
        slot = n % 2
        junk, jb = tmp["junk"][slot], tmp["junkb"][slot]
        xn, xnb = tmp["xn"][slot], tmp["xnb"][slot]
        st, stb = tmp["st"][slot], tmp["stb"][slot]
        pb = tmp["psum"][slot]
        k.act(junk, k.x[:, i, :], AF.Square, [k.xb[i]], [jb, stb], accum_out=st[:, 0:1])
        k.act(st[:, 1:2], st[:, 0:1], AF.Sqrt, [stb], [stb], bias=EPS, scale=1.0 / D)
        k.recip(st[:, 2:3], st[:, 1:2], [stb], [stb])
        k.act(xn, k.x[:, i, :], AF.Copy, [k.xb[i], stb], [xnb], scale=st[:, 2:3])
        pbf = k.ps[pb][:].bitcast(BF16)
        for c in range(8):
            k.tr(pbf[:, c * 128:(c + 1) * 128], xn[:, c * 128:(c + 1) * 128], k.ident[:], [xnb, k.cb], [k.psb[pb]])
        d_ap, d_buf = dst(i)
        g = k.pf[:, gcol:gcol + 8]
        k.tt("dve", d_ap, pbf.rearrange("p (c t) -> p c t", t=128),
             g.unsqueeze(2).to_broadcast([128, 8, 128]), ALU.mult, [k.psb[pb], k.pfb], [d_buf])
        if after is not None:
            if lag == 0:
                after(i)
            elif n >= lag:
                after(tiles[n - lag])
    if after is not None and lag > 0:
        for i in tiles[len(tiles) - lag:]:
            after(i)


def norm_tmp(k, psum_banks):
    t = dict(junk=[], junkb=[], xn=[], xnb=[], st=[], stb=[], psum=psum_banks)
    for s in range(2):
        t["junk"].append(k.carve([D], BF16))
        t["junkb"].append(k.buf("junk"))
        t["xn"].append(k.carve([D], BF16))
        t["xnb"].append(k.buf("xn"))
        t["st"].append(k.carve([8], F32))
        t["stb"].append(k.buf("st"))
    return t


def emit_mlp(k, l):
    P = k.P
    k.arena_reset()
    hT = k.carve([8, S], BF16)
    hTb = [k.buf("hT") for _ in range(4)]
    tmp = norm_tmp(k, [6, 7])
    wi = [k.carve([8, 512], BF16) for _ in range(2)]
    wo = [k.carve([4, D], BF16) for _ in range(2)]
    wib = [k.buf("wi") for _ in range(2)]
    wob = [k.buf("wo") for _ in range(2)]
    aT = [k.carve([4, 512], BF16) for _ in range(2)]
    aTb = [k.buf("aT") for _ in range(2)]
    r = [k.carve([512], F32) for _ in range(2)]
    rb = [k.buf("r") for _ in range(2)]

    def norm_block(tb):
        emit_norm_T(k, range(4 * tb, 4 * tb + 4), k.lay["norm_mlp"] + 8 * l,
                    lambda i: (hT[:, :, i * 128:(i + 1) * 128], hTb[i // 4]), tmp)

    wi_d = k.dram["mlp_wi"]
    wo_d = k.dram["mlp_wo"]

    def load(g):
        sl = g % 2
        k.dma("pool", wi[sl], wi_d[l, :, g * 512:(g + 1) * 512].rearrange("(kc p) f -> p kc f", p=128),
              f"mwi{sl}", writes=[wib[sl]])
        k.dma("pool", wo[sl], wo_d[l, g * 512:(g + 1) * 512, :].rearrange("(fc p) d -> p fc d", p=128),
              f"mwo{sl}", writes=[wob[sl]])

    steps = [(g, tb) for g in range(8) for tb in range(4)]
    abank = [0, 1, 2, 3]
    ybank = [4, 5]
    state = dict(na=0, nr=0)

    def stage1(si):
        g, tb = steps[si]
        sl = g % 2
        a = si % 2
        for m in range(4):
            b = abank[state["na"] % 4]
            state["na"] += 1
            for kc in range(8):
                k.mm(k.ps[b][:], wi[sl][:, kc, m * 128:(m + 1) * 128], hT[:, kc, tb * 512:(tb + 1) * 512],
                     kc == 0, kc == 7, [wib[sl], hTb[tb]], [k.psb[b]])
            rr = state["nr"] % 2
            state["nr"] += 1
            k.act(r[rr], k.ps[b][:], AF.Relu, [k.psb[b]], [rb[rr]])
            k.act(aT[a][:, m, :], r[rr], AF.Square, [rb[rr]], [aTb[a]])

    def stage2(si):
        g, tb = steps[si]
        sl = g % 2
        a = si % 2
        for tt in range(4):
            i = tb * 4 + tt
            for half in range(2):
                b = ybank[half]
                for m in range(4):
                    k.mm(k.ps[b][:], aT[a][:, m, tt * 128:(tt + 1) * 128], wo[sl][:, m, half * 512:(half + 1) * 512],
                         m == 0, m == 3, [aTb[a], wob[sl]], [k.psb[b]])
                xs = k.x[:, i, half * 512:(half + 1) * 512]
                k.tt("dve", xs, xs, k.ps[b][:], ALU.add, [k.xb[i], k.psb[b]], [k.xb[i]])

    load(0)
    norm_block(0)
    for si in range(len(steps) + 1):
        if si < len(steps):
            stage1(si)
            if si + 1 < 4:
                norm_block(si + 1)
        if si >= 1:
            stage2(si - 1)
        if si < len(steps):
            g, tb = steps[si]
            if tb == 0 and g + 1 < 8:
                load(g + 1)


def emit_rope_tables(k, s):
    posi = k.rope_posi[:]
    k.dma("sp", posi, k.dram["pos"][s], "pos", writes=[k.ropeb])
    k.cp("dve", k.rope_posf[:], posi, [k.ropeb], [k.ropeb])
    k.arena_reset()
    ang = k.carve([NT, 32], F32)
    k.tt("dve", ang, k.rope_posf[:].unsqueeze(2).to_broadcast([128, NT, 32]),
         k.rope_invf[:].unsqueeze(1).to_broadcast([128, NT, 32]), ALU.mult, [k.ropeb, k.cb], [k.ropeb])
    r1 = k.carve([NT, 32], F32)
    ki = k.carve([NT, 32], I32)
    kf = k.carve([NT, 32], F32)
    sc = 1.0 - 1e-6
    rb = [k.ropeb]
    two_pi = 2 * math.pi

    def reduce_to_pi(shift):
        k.ts("dve", kf, ang, shift, 1.0 / two_pi, ALU.add, ALU.mult, rb, rb)
        k.cp("dve", ki, kf, rb, rb)
        k.cp("dve", kf, ki, rb, rb)
        k.stt("dve", r1, kf, -two_pi, ang, ALU.mult, ALU.add, rb, rb)
        if shift != 0.0:
            k.ts("dve", r1, r1, shift, None, ALU.add, None, rb, rb)
        k.ts("dve", kf, r1, math.pi, two_pi, ALU.is_gt, ALU.mult, rb, rb)
        k.tt("dve", r1, r1, kf, ALU.subtract, rb, rb)
        k.ts("dve", kf, r1, -math.pi, two_pi, ALU.is_lt, ALU.mult, rb, rb)
        k.tt("dve", r1, r1, kf, ALU.add, rb, rb)

    reduce_to_pi(0.0)
    k.act(k.sinA[:, :, 32:64], r1, AF.Sin, rb, rb, scale=sc)
    k.ts("dve", k.sinA[:, :, 0:32], k.sinA[:, :, 32:64], -1.0, None, ALU.mult, None, rb, rb)
    reduce_to_pi(math.pi / 2)
    k.act(k.cos2[:, :, 0:32], r1, AF.Sin, rb, rb, scale=sc)
    k.cp("dve", k.cos2[:, :, 32:64], k.cos2[:, :, 0:32], rb, rb)


def emit_rope(k, out_bf, t, tmp, o, i, nh, reads, writes, tb):
    v3 = lambda ap: ap.rearrange("p (h d) -> p h d", d=64)
    cosb = k.cos2[:, i, :].unsqueeze(1).to_broadcast([128, nh, 64])
    sa = k.sinA[:, i, :]
    s_lo = sa[:, 0:32].unsqueeze(1).to_broadcast([128, nh, 32])
    s_hi = sa[:, 32:64].unsqueeze(1).to_broadcast([128, nh, 32])
    k.tt("dve", v3(tmp)[:, :, 0:32], v3(t)[:, :, 32:64], s_lo, ALU.mult, reads + [k.ropeb], [tb])
    k.tt("dve", v3(tmp)[:, :, 32:64], v3(t)[:, :, 0:32], s_hi, ALU.mult, reads + [k.ropeb], [tb])
    k.tt("dve", v3(o), v3(t), cosb, ALU.mult, reads + [k.ropeb], [tb])
    k.tt("dve", out_bf, o, tmp, ALU.add, [tb], writes)


def emit_mla(k, l, j):
    P = k.P
    lay = k.lay
    if k.stop == "rope":
        return
    k.arena_reset()
    ps, psb = k.ps, k.psb
    cT = k.carve([5, S], BF16)
    cTb = [k.buf("cT") for _ in range(NT)]
    krT = k.carve([S], BF16)
    krTb = [k.buf("krT") for _ in range(NT)]
    mark = k.aoff
    wd = k.carve([8, 704], BF16)
    wdb = k.buf("wd")
    k.dma("pool", wd, k.dram["mla_wd"][j].rearrange("(kc p) f -> p kc f", p=128), "mla_wd", writes=[wdb])
    tmp = norm_tmp(k, [6, 7])
    hTt = [k.carve([8, 128], BF16) for _ in range(2)]
    hTtb = [k.buf("hTt") for _ in range(2)]
    two = lambda shape, dt_: ([k.carve(shape, dt_) for _ in range(2)], [k.buf("pa") for _ in range(2)])
    cqn_, cqnb_ = two([640], BF16)
    junkA_, junkAb_ = two([384], BF16)
    st2_, st2b_ = two([16], F32)
    krf_, krfb_ = two([64], F32)
    rt_, rtb_ = two([64], F32)
    ro_, _ = two([64], F32)
    krb_, krbb_ = two([128], BF16)
    gl = lay["mla_lat"] + 5 * j
    gt = lay["mla_head"] + 384 * j
    cnt = [0]

    def passA(i):
        sl = i % 2
        h, hb = hTt[sl], hTtb[sl]
        cqn, cqnb, junkA, junkAb, st2, st2b = cqn_[sl], cqnb_[sl], junkA_[sl], junkAb_[sl], st2_[sl], st2b_[sl]
        krf, krfb, rt, rtb, ro, krb, krbb = krf_[sl], krfb_[sl], rt_[sl], rtb_[sl], ro_[sl], krb_[sl], krbb_[sl]
        b0, b1, b2 = (0, 1, 2) if sl == 0 else (3, 4, 5)
        for kc in range(8):
            k.mm(ps[b0][:, 0:384], h[:, kc, :], wd[:, kc, 0:384], kc == 0, kc == 7, [hb, wdb], [psb[b0]])
        for kc in range(8):
            k.mm(ps[b1][:, 0:320], h[:, kc, :], wd[:, kc, 384:704], kc == 0, kc == 7, [hb, wdb], [psb[b1]])
        k.act(junkA[:, 0:384], ps[b0][:, 0:384], AF.Square, [psb[b0]], [junkAb, st2b], accum_out=st2[:, 0:1])
        k.act(junkA[:, 0:256], ps[b1][:, 0:256], AF.Square, [psb[b1]], [junkAb, st2b], accum_out=st2[:, 1:2])
        k.act(junkA[:, 0:64], ps[b1][:, 256:320], AF.Square, [psb[b1]], [junkAb, st2b], accum_out=st2[:, 2:3])
        for c, n in enumerate((384, 256, 64)):
            k.act(st2[:, 3 + c:4 + c], st2[:, c:c + 1], AF.Sqrt, [st2b], [st2b], bias=EPS, scale=1.0 / n)
        k.recip(st2[:, 6:9], st2[:, 3:6], [st2b], [st2b])
        if k.stop == "A1":
            return
        k.act(cqn[:, 0:384], ps[b0][:, 0:384], AF.Copy, [psb[b0], st2b], [cqnb], scale=st2[:, 6:7])
        k.act(cqn[:, 384:640], ps[b1][:, 0:256], AF.Copy, [psb[b1], st2b], [cqnb], scale=st2[:, 7:8])
        if k.stop == "A2":
            return
        k.stt("dve", krf, ps[b1][:, 256:320], st2[:, 8:9], k.pt[:, gt + 320:gt + 384], ALU.mult, ALU.mult,
              [psb[b1], st2b, k.ptb], [krfb])
        emit_rope(k, krb[:, 0:64], krf, rt, ro, i, 1, [krfb], [krbb], rtb)
        k.cp("dve", krb[:, 64:128], krb[:, 0:64], [krbb], [krbb])
        if k.stop == "A3":
            return
        pbf = ps[b2][:].bitcast(BF16)
        for c in range(5):
            k.tr(pbf[:, c * 128:(c + 1) * 128], cqn[:, c * 128:(c + 1) * 128], k.ident[:], [cqnb, k.cb], [psb[b2]])
        k.tr(pbf[:, 640:768], krb, k.ident[:], [krbb, k.cb], [psb[b2]])
        g = k.pf[:, gl:gl + 5]
        k.tt("dve", cT[:, :, i * 128:(i + 1) * 128], pbf[:, 0:640].rearrange("p (c t) -> p c t", t=128),
             g.unsqueeze(2).to_broadcast([128, 5, 128]), ALU.mult, [psb[b2], k.pfb], [cTb[i]])
        if k.stop == "A4":
            return
        k.cp("dve", krT[:, i * 128:(i + 1) * 128], pbf[:, 640:768], [psb[b2]], [krTb[i]])

    emit_norm_T(k, range(NT), lay["norm_mix"] + 8 * l, lambda i: (hTt[i % 2], hTtb[i % 2]), tmp, after=passA, lag=1)
    k.dump("cT", cT, [128, 5, S], BF16, cTb)
    k.dump("krT", krT, [128, S], BF16, krTb)

    if k.stop in ("passA", "A1", "A2", "A3", "A4"):
        return
    k.arena_mark_reset(mark)
    wuq = [k.carve([3, 768], BF16)]
    wukv = [k.carve([2, 1024], BF16)]
    wo = k.carve([4, D], BF16)
    wuqb = [k.buf("wuq")]
    wukvb = [k.buf("wukv")]
    wob = k.buf("wo")
    kT = k.carve([4, S], BF16); kTb = [k.buf("kT") for _ in range(NT)]
    v = k.carve([NT, 512], BF16); vb = [k.buf("v") for _ in range(NT)]
    qTn = [k.carve([4, 512], BF16) for _ in range(2)]; qTnb = [k.buf("qTn") for _ in range(2)]
    qTr = [k.carve([2, 512], BF16) for _ in range(2)]; qTrb = [k.buf("qTr") for _ in range(2)]
    oT = k.carve([4, 512], BF16); oTb = k.buf("oT")
    pT = [k.carve([512], BF16) for _ in range(3)]; pTb = [k.buf("pT") for _ in range(3)]
    rden = k.carve([512], F32); rdenb = k.buf("rden")
    two = lambda shape, dt_: ([k.carve(shape, dt_) for _ in range(2)], [k.buf("tp") for _ in range(2)])
    sq1_, sq1b_ = two([512], F32)
    sq2_, sq2b_ = two([256], F32)
    sq3_, sq3b_ = two([512], F32)
    ssq_, ssqb_ = two([48], F32)
    tq_, tqb_ = two([512], F32)
    tr__, trb_ = two([256], F32)
    trt_, trtb_ = two([256], F32)
    tro_, _ = two([256], F32)
    t3_, t3b_ = two([512], F32)
    qnb__, qnbb_ = two([512], BF16)
    qrb__, qrbb_ = two([256], BF16)
    knb__, knbb_ = two([512], BF16)
    h4 = lambda ap, d: ap.rearrange("p (h d) -> p h d", d=d)
    scale = 192.0 ** -0.5

    def load_group(G):
        sl = 0
        k.dma("pool", wuq[sl], k.dram["mla_wuq"][j, G].rearrange("(kc p) f -> p kc f", p=128), f"wuq{sl}", writes=[wuqb[sl]])
        k.dma("pool", wukv[sl], k.dram["mla_wukv"][j, G].rearrange("(kc p) f -> p kc f", p=128), f"wukv{sl}", writes=[wukvb[sl]])

    def load_wo(G):
        k.dma("pool", wo, k.dram["mla_wo"][j, G * 512:(G + 1) * 512, :].rearrange("(h p) d -> p h d", p=128), "mla_wo", writes=[wob])

    def tile_proj_gen(G, qb):
        sl = 0
        qs = qb % 2
        Bqn, Bqr, Bk = 5, 6, 7
        for i in range(4 * qb, 4 * qb + 4):
            tt = i - 4 * qb
            csl = slice(i * 128, (i + 1) * 128)
            pr = i % 2
            sq1, sq1b, sq2, sq2b, sq3, sq3b, ssq, ssqb = sq1_[pr], sq1b_[pr], sq2_[pr], sq2b_[pr], sq3_[pr], sq3b_[pr], ssq_[pr], ssqb_[pr]
            tq, tqb, tr_, trb, trt, trtb, tro, t3, t3b = tq_[pr], tqb_[pr], tr__[pr], trb_[pr], trt_[pr], trtb_[pr], tro_[pr], t3_[pr], t3b_[pr]
            qnb_, qnbb, qrb_, qrbb, knb_, knbb = qnb__[pr], qnbb_[pr], qrb__[pr], qrbb_[pr], knb__[pr], knbb_[pr]
            for kc in range(3):
                k.mm(ps[Bqn][:], cT[:, kc, csl], wuq[sl][:, kc, 0:512], kc == 0, kc == 2, [cTb[i], wuqb[sl]], [psb[Bqn]])
            for kc in range(3):
                k.mm(ps[Bqr][:, 0:256], cT[:, kc, csl], wuq[sl][:, kc, 512:768], kc == 0, kc == 2, [cTb[i], wuqb[sl]], [psb[Bqr]])
            for kc in range(2):
                k.mm(ps[Bk][:], cT[:, 3 + kc, csl], wukv[sl][:, kc, 0:512], kc == 0, kc == 1, [cTb[i], wukvb[sl]], [psb[Bk]])
            yield
            k.act(sq1, ps[Bqn][:], AF.Square, [psb[Bqn]], [sq1b])
            k.act(sq2, ps[Bqr][:, 0:256], AF.Square, [psb[Bqr]], [sq2b])
            k.act(sq3, ps[Bk][:], AF.Square, [psb[Bk]], [sq3b])
            yield
            k.red(ssq[:, 0:4], h4(sq1, 128), [sq1b], [ssqb])
            k.red(ssq[:, 4:8], h4(sq2, 64), [sq2b], [ssqb])
            k.red(ssq[:, 8:12], h4(sq3, 128), [sq3b], [ssqb])
            yield
            k.tt("dve", ssq[:, 12:24], ssq[:, 0:12], k.invn12[:], ALU.mult, [ssqb, k.cb], [ssqb])
            k.act(ssq[:, 24:36], ssq[:, 12:24], AF.Sqrt, [ssqb], [ssqb], bias=EPS, scale=1.0)
            k.recip(ssq[:, 36:48], ssq[:, 24:36], [ssqb], [ssqb])
            rs = ssq[:, 36:48]
            yield
            k.tt("dve", h4(tq, 128), h4(ps[Bqn][:], 128), rs[:, 0:4].unsqueeze(2).to_broadcast([128, 4, 128]), ALU.mult,
                 [psb[Bqn], ssqb], [tqb])
            yield
            for kc in range(2):
                k.mm(ps[Bqn][:], cT[:, 3 + kc, csl], wukv[sl][:, kc, 512:1024], kc == 0, kc == 1, [cTb[i], wukvb[sl]], [psb[Bqn]])
            k.act(v[:, i, :], ps[Bqn][:], AF.Copy, [psb[Bqn]], [vb[i]])
            k.tt("dve", h4(qnb_, 128), h4(tq, 128), k.pt[:, gt:gt + 128].unsqueeze(1).to_broadcast([128, 4, 128]), ALU.mult,
                 [tqb, k.ptb], [qnbb])
            yield
            k.tt("dve", h4(tr_, 64), h4(ps[Bqr][:, 0:256], 64), rs[:, 4:8].unsqueeze(2).to_broadcast([128, 4, 64]), ALU.mult,
                 [psb[Bqr], ssqb], [trb])
            k.tt("dve", h4(tr_, 64), h4(tr_, 64), k.pt[:, gt + 128:gt + 192].unsqueeze(1).to_broadcast([128, 4, 64]), ALU.mult,
                 [trb, k.ptb], [trb])
            yield
            emit_rope(k, qrb_, tr_, trt, tro, i, 4, [trb], [qrbb], trtb)
            yield
            k.tt("dve", h4(t3, 128), h4(ps[Bk][:], 128), rs[:, 8:12].unsqueeze(2).to_broadcast([128, 4, 128]), ALU.mult,
                 [psb[Bk], ssqb], [t3b])
            k.tt("dve", h4(knb_, 128), h4(t3, 128), k.pt[:, gt + 192:gt + 320].unsqueeze(1).to_broadcast([128, 4, 128]), ALU.mult,
                 [t3b, k.ptb], [knbb])
            yield
            p6 = ps[Bqn][:].bitcast(BF16)
            for c in range(4):
                k.tr(p6[:, c * 128:(c + 1) * 128], qnb_[:, c * 128:(c + 1) * 128], k.ident[:], [qnbb, k.cb], [psb[Bqn]])
            for c in range(2):
                k.tr(p6[:, 512 + c * 128:512 + (c + 1) * 128], qrb_[:, c * 128:(c + 1) * 128], k.ident[:], [qrbb, k.cb], [psb[Bqn]])
            yield
            k.cp("dve", qTn[qs][:, :, tt * 128:(tt + 1) * 128], h4(p6[:, 0:512], 128), [psb[Bqn]], [qTnb[qs]])
            k.cp("dve", qTr[qs][:, :, tt * 128:(tt + 1) * 128], h4(p6[:, 512:768], 128), [psb[Bqn]], [qTrb[qs]])
            p7 = ps[Bk][:].bitcast(BF16)
            for c in range(4):
                k.tr(p7[:, c * 128:(c + 1) * 128], knb_[:, c * 128:(c + 1) * 128], k.ident[:], [knbb, k.cb], [psb[Bk]])
            yield
            k.cp("dve", kT[:, :, csl], h4(p7[:, 0:512], 128), [psb[Bk]], [kTb[i]])
            yield

    def attention(G, qb):
        qs = qb % 2
        nkt = 4 * qb + 4
        steps = [(hh, kt) for hh in range(4) for kt in range(nkt)]
        sbank = [2, 3, 4]
        acc = [(0, 1), (0, 1)]

        def qk(si):
            hh, kt = steps[si]
            blk, half = hh % 2, hh // 2
            jd = kt - 4 * qb
            c0 = max(0, jd) * 128
            b = sbank[si % 3]
            ks = slice(kt * 128, (kt + 1) * 128)
            k.mm(ps[b][:, 0:512 - c0], kT[:, hh, ks], qTn[qs][:, hh, c0:512], True, False, [kTb[kt], qTnb[qs]], [psb[b]])
            k.mm(ps[b][:, 0:512 - c0], krT[half * 64:(half + 1) * 64, ks], qTr[qs][half * 64:(half + 1) * 64, blk, c0:512],
                 False, True, [krTb[kt], qTrb[qs]], [psb[b]])
            pb_ = si % 3
            k.act(pT[pb_][:, 0:512 - c0], ps[b][:, 0:512 - c0], AF.Exp, [psb[b]], [pTb[pb_]], scale=scale)
            if jd >= 0:
                blkap = pT[pb_][:, 0:128]
                P.add("pool", lambda e: e.affine_select(out=blkap, in_=blkap, pattern=[[1, 128]], compare_op=ALU.is_ge,
                                                        fill=0.0, base=0, channel_multiplier=-1), [pTb[pb_]], [pTb[pb_]])

        def pv(si):
            hh, kt = steps[si]
            jd = kt - 4 * qb
            c0 = max(0, jd) * 128
            bo, bd = acc[hh % 2]
            pb_ = si % 3
            k.mm(ps[bo][:, c0:512], v[:, kt, hh * 128:(hh + 1) * 128], pT[pb_][:, 0:512 - c0], kt == 0, kt == nkt - 1,
                 [vb[kt], pTb[pb_]], [psb[bo]])
            k.mm(ps[bd][:, c0:512], k.ones_bf[:], pT[pb_][:, 0:512 - c0], kt == 0, kt == nkt - 1,
                 [k.cb, pTb[pb_]], [psb[bd]])
            if kt == nkt - 1:
                k.recip(rden, ps[bd][:], [psb[bd]], [rdenb])
                k.tt("dve", oT[:, hh, :], ps[bo][:], rden, ALU.mult, [psb[bo], rdenb], [oTb])

        for si in range(len(steps) + 2):
            if si < len(steps):
                qk(si)
            if si >= 2:
                pv(si - 2)
            yield

    def out_proj(G, qb):
        for tt in range(4):
            i = 4 * qb + tt
            for half in range(2):
                b = (3, 4)[half]
                for hh in range(4):
                    k.mm(ps[b][:], oT[:, hh, tt * 128:(tt + 1) * 128], wo[:, hh, half * 512:(half + 1) * 512],
                         hh == 0, hh == 3, [oTb, wob], [psb[b]])
                xs = k.x[:, i, half * 512:(half + 1) * 512]
                k.tt("dve", xs, xs, ps[b][:], ALU.add, [k.xb[i], psb[b]], [k.xb[i]])

    for G in range(2):
        load_group(G)
        load_wo(G)
        for _ in tile_proj_gen(G, 0):
            pass
        for qb in range(4):
            ga = attention(G, qb)
            gb = tile_proj_gen(G, qb + 1) if qb + 1 < 4 else None
            rate = (4, 2, 1, 1)[qb]
            alive_b = gb is not None
            for _ in ga:
                if alive_b:
                    for _r in range(rate):
                        try:
                            next(gb)
                        except StopIteration:
                            alive_b = False
                            break
            if alive_b:
                for _ in gb:
                    pass
            if k.stop == "attn":
                continue
            out_proj(G, qb)


def emit_conv(k, l):
    P = k.P
    lay = k.lay
    ps, psb = k.ps, k.psb
    k.arena_reset()
    w2 = k.carve([8, D], BF16); w2b = k.buf("w2")
    k.dma("pool", w2, k.dram["cv_w2"].rearrange("(cc p) d -> p cc d", p=128), "cv_w2", writes=[w2b])
    tmp = norm_tmp(k, [0, 1])
    hTt = [k.carve([8, 512], BF16) for _ in range(2)]; hTtb = [k.buf("hTt") for _ in range(2)]
    w1 = [k.carve([8, 2, 128], BF16) for _ in range(2)]; w1b = [k.buf("w1") for _ in range(2)]
    Dm = [k.carve([31, 128], BF16) for _ in range(2)]; Dmb = [k.buf("Dm") for _ in range(2)]
    uT = k.carve([8, 542], BF16); uTb = [k.buf("uT") for _ in range(8)]
    ysb = k.carve([8, 512], F32); ysbb = [k.buf("ysb") for _ in range(8)]
    ysq = [k.carve([512], F32) for _ in range(2)]; ysqb = [k.buf("ysq") for _ in range(2)]
    sig = [k.carve([512], F32) for _ in range(2)]; sigb = [k.buf("sig") for _ in range(2)]
    zT = k.carve([8, 512], BF16); zTb = [k.buf("zT") for _ in range(8)]
    mean = k.carve([512], F32); msq = k.carve([512], F32); rstd = k.carve([512], F32); stb = k.buf("cvst")
    tn = [k.carve([512], F32) for _ in range(2)]; tnb = [k.buf("tn") for _ in range(2)]
    cb1 = lay["cv_b1"]; cwd = lay["cv_wdw"]; cbd = lay["cv_bdw"]; cg = lay["cv_lng"]; cbn = lay["cv_lnb"]
    w1_d = k.dram["cv_w1"]
    b2t = k.carve([D], F32); b2b = k.buf("b2t")
    k.dma("sp", b2t, k.dram["pt"][:, lay["cv_b2"]:lay["cv_b2"] + D], "cv_b2", writes=[b2b])

    def load_w1(n):
        cc = n % 8
        sl = n % 2
        k.dma("pool", w1[sl][:, :, 0, :], w1_d[:, cc * 128:(cc + 1) * 128].rearrange("(kc p) f -> p kc f", p=128),
              f"cvw1{sl}", writes=[w1b[sl]])
        k.dma("pool", w1[sl][:, :, 1, :], w1_d[:, D + cc * 128:D + (cc + 1) * 128].rearrange("(kc p) f -> p kc f", p=128),
              f"cvw1{sl}", writes=[w1b[sl]])

    load_w1(0)
    n = 0
    for tb in range(4):
        hs = tb % 2
        emit_norm_T(k, range(4 * tb, 4 * tb + 4), lay["norm_mix"] + 8 * l,
                    lambda i: (hTt[hs][:, :, (i % 4) * 128:(i % 4 + 1) * 128], hTtb[hs]), tmp)
        for i in range(4 * tb, 4 * tb + 4):
            k.tt("pool", k.x[:, i, :], k.x[:, i, :], b2t, ALU.add, [k.xb[i], b2b], [k.xb[i]])
        for cc in range(8):
            sl = n % 2
            if n + 1 < 32:
                load_w1(n + 1)
            n += 1
            wv = k.pf[:, cwd + cc * 31:cwd + (cc + 1) * 31]
            k.tt("pool", Dm[sl], k.identf[:].unsqueeze(1).to_broadcast([128, 31, 128]),
                 wv.unsqueeze(2).to_broadcast([128, 31, 128]), ALU.mult, [k.cb, k.pfb], [Dmb[sl]])
            ba, bg, bc = cc % 2, 2 + cc % 2, 4 + cc % 2
            for kc in range(8):
                k.mm(ps[ba][:], w1[sl][:, kc, 0, :], hTt[hs][:, kc, :], kc == 0, kc == 7, [w1b[sl], hTtb[hs]], [psb[ba]])
            for kc in range(8):
                k.mm(ps[bg][:], w1[sl][:, kc, 1, :], hTt[hs][:, kc, :], kc == 0, kc == 7, [w1b[sl], hTtb[hs]], [psb[bg]])
            ss_ = cc % 2
            k.act(sig[ss_], ps[bg][:], AF.Sigmoid, [psb[bg], k.pfb], [sigb[ss_]], bias=k.pf[:, cb1 + 8 + cc:cb1 + 9 + cc], scale=1.0)
            if tb == 0:
                k.memset("pool", uT[:, cc, 0:30], 0.0, [uTb[cc]])
            else:
                k.cp("pool", uT[:, cc, 0:30], uT[:, cc, 512:542], [uTb[cc]], [uTb[cc]])
            k.stt("dve", uT[:, cc, 30:542], ps[ba][:], k.pf[:, cb1 + cc:cb1 + cc + 1], sig[ss_], ALU.add, ALU.mult,
                  [psb[ba], sigb[ss_], k.pfb], [uTb[cc]])
            for jj in range(31):
                k.mm(ps[bc][:], Dm[sl][:, jj, :], uT[:, cc, jj:jj + 512], jj == 0, jj == 30, [Dmb[sl], uTb[cc]], [psb[bc]])
            bdw = k.pf[:, cbd + cc:cbd + cc + 1]
            k.act(ysb[:, cc, :], ps[bc][:], AF.Identity, [psb[bc], k.pfb], [ysbb[cc]], bias=bdw, scale=1.0)
            k.act(ysq[ss_], ps[bc][:], AF.Square, [psb[bc], k.pfb], [ysqb[ss_]], bias=bdw, scale=1.0)
            k.mm(ps[6][:], k.ones_f[:], ysb[:, cc, :], cc == 0, cc == 7, [k.cb, ysbb[cc]], [psb[6]])
            k.mm(ps[7][:], k.ones_f[:], ysq[ss_], cc == 0, cc == 7, [k.cb, ysqb[ss_]], [psb[7]])
        k.act(mean, ps[6][:], AF.Copy, [psb[6]], [stb], scale=1.0 / D)
        k.act(msq, ps[6][:], AF.Square, [psb[6]], [stb], scale=1.0 / D)
        k.stt("dve", rstd, ps[7][:], 1.0 / D, msq, ALU.mult, ALU.subtract, [psb[7], stb], [stb])
        k.act(rstd, rstd, AF.Sqrt, [stb], [stb], bias=EPS, scale=1.0)
        k.recip(rstd, rstd, [stb], [stb])
        for cc in range(8):
            ts_ = cc % 2
            k.tt("pool", tn[ts_], ysb[:, cc, :], mean, ALU.subtract, [ysbb[cc], stb], [tnb[ts_]])
            k.tt("dve", tn[ts_], tn[ts_], rstd, ALU.mult, [tnb[ts_], stb], [tnb[ts_]])
            k.act(zT[:, cc, :], tn[ts_], AF.Silu, [tnb[ts_], k.pfb], [zTb[cc]],
                  bias=k.pf[:, cbn + cc:cbn + cc + 1], scale=k.pf[:, cg + cc:cg + cc + 1])
        for tt in range(4):
            i = 4 * tb + tt
            for half in range(2):
                b = (2, 3)[half]
                for cc in range(8):
                    k.mm(ps[b][:], zT[:, cc, tt * 128:(tt + 1) * 128], w2[:, cc, half * 512:(half + 1) * 512],
                         cc == 0, cc == 7, [zTb[cc], w2b], [psb[b]])
                xs = k.x[:, i, half * 512:(half + 1) * 512]
                k.tt("dve", xs, xs, ps[b][:], ALU.add, [k.xb[i], psb[b]], [k.xb[i]])


def emit_hgrn_consts(k, l):
    lay = k.lay
    hgc = k.hgc
    cbs = [k.cb]
    lg = k.pf[:, lay["hg_lb"]:lay["hg_lb"] + 32]
    e = hgc[:, 0:32]
    k.act(e, lg, AF.Exp, [k.pfb], cbs)
    den = hgc[:, 32:40]
    k.tt("dve", den, e[:, 0:8], e[:, 8:16], ALU.add, cbs, cbs)
    k.tt("dve", den, den, e[:, 16:24], ALU.add, cbs, cbs)
    k.tt("dve", den, den, e[:, 24:32], ALU.add, cbs, cbs)
    k.recip(den, den, cbs, cbs)
    num = hgc[:, 40:48]
    k.memset("dve", num, 0.0, cbs)
    for i in range(1, l + 1):
        k.tt("dve", num, num, e[:, 8 * i:8 * i + 8], ALU.add, cbs, cbs)
    k.tt("dve", hgc[:, 48:56], num, den, ALU.mult, cbs, cbs)
    k.ts("dve", hgc[:, 56:64], hgc[:, 48:56], -1.0, 1.0, ALU.mult, ALU.add, cbs, cbs)
    k.ts("dve", hgc[:, 64:72], hgc[:, 56:64], -1.0, None, ALU.mult, None, cbs, cbs)
    k.memset("pool", k.mask2[:], 1.0, cbs)
    k.P.add("pool", lambda e_: e_.affine_select(out=k.mask2[:], in_=k.mask2[:], pattern=[[1, 128]], compare_op=ALU.is_ge,
                                                 fill=0.0, base=0, channel_multiplier=-1), cbs, cbs)
    k.memset("pool", k.mask2[0:64, 64:128], 0.0, cbs)


def emit_hgrn(k, l):
    P = k.P
    lay = k.lay
    ps, psb = k.ps, k.psb
    k.arena_reset()
    hT = k.carve([8, S], BF16); hTb = [k.buf("hT") for _ in range(4)]
    tmp = norm_tmp(k, [6, 7])
    Wp = [k.carve([8, 4, 256], BF16) for _ in range(2)]; Wpb = [k.buf("Wp") for _ in range(2)]
    wop = [k.carve([2, D], BF16) for _ in range(2)]; wopb = [k.buf("wop") for _ in range(2)]
    F = lambda: k.carve([512], F32)
    sg, f_, b_, bp, Em, t1, t1e, t2, on = [F() for _ in range(9)]
    sgb, fb, bb, bpb, Emb, t1b, t1eb, t2b, onb = [k.buf("hg") for _ in range(9)]
    two = lambda shape, dt_: ([k.carve(shape, dt_) for _ in range(2)], [k.buf("hg2") for _ in range(2)])
    Ep_, Epb_ = two([512], F32)
    gate_, gateb_ = two([512], F32)
    kT__, kTb__ = two([512], BF16)
    qT__, qTb__ = two([512], BF16)
    vT__, vTb__ = two([512], BF16)
    ktm_, ktmb_ = two([4, 128], BF16)
    vtm_, vtmb_ = two([4, 128], BF16)
    em_, emb_ = two([8], F32)
    el_, _ = two([8], F32)
    onT = [k.carve([512], BF16) for _ in range(2)]; onTb = [k.buf("onT") for _ in range(2)]
    Am = [k.carve([128], BF16) for _ in range(2)]; Amb = [k.buf("Am") for _ in range(2)]
    Sst = [k.carve([128], F32) for _ in range(2)]; Sb = [k.buf("S") for _ in range(2)]
    Stil = k.carve([128], BF16); Stilb = k.buf("Stil")
    tmpS = k.carve([128], F32); tmpSb = k.buf("tmpS")
    scanmask = k.carve([512], F32); smb = k.buf("scanmask")
    k.memset("dve", scanmask, 1.0, [smb])
    k.memset("dve", scanmask.rearrange("p (c t) -> p c t", t=64)[:, :, 0:1], 0.0, [smb])
    hc = k.hgc
    w_d = k.dram["hg_wi"]
    wo_d = k.dram["hg_wo"]
    c8 = lambda ap: ap.rearrange("p (c t) -> p c t", t=64)

    emit_norm_T(k, range(NT), lay["norm_mix"] + 8 * l, lambda i: (hT[:, :, i * 128:(i + 1) * 128], hTb[i // 4]), tmp)

    def load(hp):
        sl = hp % 2
        for kind in range(4):
            k.dma("pool", Wp[sl][:, :, kind, :],
                  w_d[:, kind * D + hp * 256:kind * D + (hp + 1) * 256].rearrange("(kc p) f -> p kc f", p=128),
                  f"hgw{sl}", writes=[Wpb[sl]])
        k.dma("pool", wop[sl], wo_d[hp * 256:(hp + 1) * 256, :].rearrange("(hh p) d -> p hh d", p=128),
              f"hgwo{sl}", writes=[wopb[sl]])

    rot = [0]

    def proj(sl, kind, hh, tb, bank):
        for kc in range(8):
            k.mm(ps[bank][:], Wp[sl][:, kc, kind, hh * 128:(hh + 1) * 128], hT[:, kc, tb * 512:(tb + 1) * 512],
                 kc == 0, kc == 7, [Wpb[sl], hTb[tb]], [psb[bank]])

    go = k.pf[:, lay["hg_on"]:lay["hg_on"] + 1]

    def pro(sl, hp, tb, hh):
        hcol = 2 * hp + hh
        lb = hc[:, 48 + hcol:49 + hcol]
        oml = hc[:, 56 + hcol:57 + hcol]
        noml = hc[:, 64 + hcol:65 + hcol]
        Ep, Epb, gate, gateb = Ep_[hh], Epb_[hh], gate_[hh], gateb_[hh]
        kT_, kTb_, qT_, qTb_, vT_, vTb_ = kT__[hh], kTb__[hh], qT__[hh], qTb__[hh], vT__[hh], vTb__[hh]
        ktm, ktmb, vtm, vtmb, em, emb, el = ktm_[hh], ktmb_[hh], vtm_[hh], vtmb_[hh], em_[hh], emb_[hh], el_[hh]
        bq = hh
        proj(sl, 0, hh, tb, bq)
        yield
        proj(sl, 1, hh, tb, 2)
        yield
        proj(sl, 2, hh, tb, 3)
        yield
        k.act(sg, ps[2][:], AF.Sigmoid, [psb[2]], [sgb])
        yield
        proj(sl, 3, hh, tb, 2)
        yield
        k.ts("dve", f_, sg, oml, lb, ALU.mult, ALU.add, [sgb, k.cb], [fb])
        yield
        k.act(f_, f_, AF.Ln, [fb], [fb])
        yield
        P.add("dve", lambda e: e.tensor_tensor_scan(out=b_, data0=scanmask, data1=f_, initial=0.0,
                                                    op0=ALU.mult, op1=ALU.add), [fb, smb], [bb])
        k.tt("dve", c8(bp), c8(b_), c8(b_)[:, :, 31:32].to_broadcast([128, 8, 64]), ALU.subtract, [bb], [bpb])
        yield
        k.act(Ep, bp, AF.Exp, [bpb], [Epb])
        yield
        k.act(Em, bp, AF.Exp, [bpb], [Emb], scale=-1.0)
        yield
        k.act(em, c8(b_)[:, :, 31], AF.Exp, [bb], [emb])
        yield
        k.act(el, c8(b_)[:, :, 63], AF.Exp, [bb], [emb])
        yield
        k.ts("dve", t1, sg, noml, oml, ALU.mult, ALU.add, [sgb, k.cb], [t1b])
        yield
        k.tt("pool", kT_, t1, Em, ALU.mult, [t1b, Emb], [kTb_])
        yield
        k.tt("dve", qT_, ps[bq][:], Ep, ALU.mult, [psb[bq], Epb], [qTb_])
        yield
        k.act(vT_, ps[3][:], AF.Copy, [psb[3]], [vTb_])
        yield
        k.act(gate, ps[2][:], AF.Silu, [psb[2]], [gateb])
        yield
        p6 = ps[6][:].bitcast(BF16)
        for tt in range(4):
            k.tr(p6[:, tt * 128:(tt + 1) * 128], kT_[:, tt * 128:(tt + 1) * 128], k.ident[:], [kTb_, k.cb], [psb[6]])
            yield
        for tt in range(4):
            k.tr(p6[:, 512 + tt * 128:512 + (tt + 1) * 128], vT_[:, tt * 128:(tt + 1) * 128], k.ident[:], [vTb_, k.cb], [psb[6]])
            yield
        k.cp("dve", ktm, p6[:, 0:512].rearrange("p (a b) -> p a b", b=128), [psb[6]], [ktmb])
        yield
        k.cp("dve", vtm, p6[:, 512:1024].rearrange("p (a b) -> p a b", b=128), [psb[6]], [vtmb])
        yield

    def loop_epi(hh):
        Ep, Epb, gate, gateb = Ep_[hh], Epb_[hh], gate_[hh], gateb_[hh]
        kT_, kTb_, qT_, qTb_ = kT__[hh], kTb__[hh], qT__[hh], qTb__[hh]
        ktm, ktmb, vtm, vtmb, em, emb, el = ktm_[hh], ktmb_[hh], vtm_[hh], vtmb_[hh], em_[hh], emb_[hh], el_[hh]
        e2 = c8(Ep)[:, :, 63]
        for tt in range(4):
            tsl = slice(tt * 128, (tt + 1) * 128)
            a = tt % 2
            k.mm(ps[4][:, 0:128], kT_[:, tsl], qT_[:, tsl], True, True, [kTb_, qTb_], [psb[4]])
            k.tt("dve", Am[a], ps[4][:, 0:128], k.mask2[:], ALU.mult, [psb[4], k.cb], [Amb[a]])
            k.mm(ps[7][:, tsl], vtm[:, tt, :], Am[a], True, False, [vtmb, Amb[a]], [psb[7]])
            for half in range(2):
                c = 2 * tt + half
                hs = slice(half * 64, (half + 1) * 64)
                csl = slice(tt * 128 + half * 64, tt * 128 + (half + 1) * 64)
                k.act(Stil, Sst[hh], AF.Copy, [Sb[hh], emb], [Stilb], scale=em[:, c:c + 1])
                k.mm(ps[7][:, csl], Stil, qT_[:, csl], False, half == 1, [Stilb, qTb_], [psb[7]])
                k.mm(ps[5][:, 0:128], ktm[hs, tt, :], vtm[hs, tt, :], True, True, [ktmb, vtmb], [psb[5]])
                k.ts("dve", tmpS, ps[5][:, 0:128], e2[:, c:c + 1], None, ALU.mult, None, [psb[5], Epb], [tmpSb])
                k.stt("dve", Sst[hh], Sst[hh], el[:, c:c + 1], tmpS, ALU.mult, ALU.add, [Sb[hh], emb, tmpSb], [Sb[hh]])
                yield
        k.act(t1e, ps[7][:], AF.Square, [psb[7]], [t1eb])
        yield
        k.mm(ps[4][:], k.ones_f[:], t1e, True, True, [k.cb, t1eb], [psb[4]])
        yield
        k.act(t2, ps[4][:], AF.Sqrt, [psb[4]], [t2b], bias=EPS, scale=1.0 / 128)
        yield
        k.recip(t2, t2, [t2b], [t2b])
        yield
        k.stt("dve", on, ps[7][:], go, t2, ALU.mult, ALU.mult, [psb[7], t2b, k.pfb], [onb])
        yield
        k.tt("pool", onT[hh], on, gate, ALU.mult, [onb, gateb], [onTb[hh]])
        yield

    def outp(sl, tb):
        for tt in range(4):
            i = 4 * tb + tt
            for half in range(2):
                bnk = (2, 3)[half]
                for hh in range(2):
                    k.mm(ps[bnk][:], onT[hh][:, tt * 128:(tt + 1) * 128], wop[sl][:, hh, half * 512:(half + 1) * 512],
                         hh == 0, hh == 1, [onTb[hh], wopb[sl]], [psb[bnk]])
                xs = k.x[:, i, half * 512:(half + 1) * 512]
                k.tt("dve", xs, xs, ps[bnk][:], ALU.add, [k.xb[i], psb[bnk]], [k.xb[i]])

    load(0)
    for hp in range(4):
        sl = hp % 2
        if hp + 1 < 4:
            load(hp + 1)
        for hh in range(2):
            k.memset("pool", Sst[hh], 0.0, [Sb[hh]])
        units = [(tb, hh) for tb in range(4) for hh in range(2)]
        for _ in pro(sl, hp, *units[0]):
            pass
        for n, (tb, hh) in enumerate(units):
            ga = loop_epi(hh)
            gb = pro(sl, hp, *units[n + 1]) if n + 1 < len(units) else None
            alive_a, alive_b = True, gb is not None
            while alive_a or alive_b:
                if alive_a:
                    try:
                        next(ga)
                    except StopIteration:
                        alive_a = False
                if alive_b:
                    for _ in range(2):
                        try:
                            next(gb)
                        except StopIteration:
                            alive_b = False
                            break
            if hh == 1:
                outp(sl, tb)

def fm(v):
    v = np.asarray(v, np.float32)
    return np.ascontiguousarray(v.reshape(-1, 128).T)


def pack_inputs(inp):
    cols = []
    lay = {}

    def put(name, arr):
        lay[name] = sum(c.shape[1] for c in cols)
        cols.append(np.asarray(arr, np.float32))

    put("norm_mix", np.concatenate([fm(inp["norm_mix"][l]) for l in range(4)], axis=1))
    put("norm_mlp", np.concatenate([fm(inp["norm_mlp"][l]) for l in range(4)], axis=1))
    put("mla_lat", np.concatenate([np.concatenate([fm(inp["mla_q_lat_norm"][j]), fm(inp["mla_kv_lat_norm"][j])], axis=1)
                                   for j in range(2)], axis=1))
    put("hg_lb", np.concatenate([fm(inp["hg_lb_logits"][i]) for i in range(4)], axis=1))
    put("hg_on", fm(inp["hg_out_norm"][0]))
    put("cv_b1", fm(inp["cv_b_pw1"][0]))
    put("cv_wdw", np.asarray(inp["cv_w_dw"][0], np.float32).T.reshape(8, 128, 31).transpose(1, 0, 2).reshape(128, 8 * 31))
    put("cv_bdw", fm(inp["cv_b_dw"][0]))
    put("cv_lng", fm(inp["cv_ln_g"][0]))
    put("cv_lnb", fm(inp["cv_ln_b"][0]))
    pf = np.ascontiguousarray(np.concatenate(cols, axis=1))
    lay["npf"] = pf.shape[1]
    tcols = []

    def putt(name, vec):
        lay[name] = sum(c.shape[0] for c in tcols)
        tcols.append(np.asarray(vec, np.float32).reshape(-1))

    putt("mla_head", np.concatenate([np.concatenate([inp["mla_q_head_norm"][j], inp["mla_k_head_norm"][j]]) for j in range(2)]))
    lay["npt_res"] = sum(c.shape[0] for c in tcols)
    putt("cv_b2", inp["cv_b_pw2"][0])
    ptv = np.concatenate(tcols)
    pt = np.ascontiguousarray(np.broadcast_to(ptv[None, :], (128, ptv.shape[0])))
    lay["npt"] = pt.shape[1]
    return pf, pt, lay


def pack_weights(inp):
    w = {}
    w["mlp_wi"] = np.asarray(inp["mlp_w_in"], np.float32)
    w["mlp_wo"] = np.asarray(inp["mlp_w_out"], np.float32)
    w["mla_wd"] = np.asarray(inp["mla_w_down"], np.float32)
    wuq = np.asarray(inp["mla_w_uq"], np.float32).reshape(2, 384, 8, 192)
    wukv = np.asarray(inp["mla_w_ukv"], np.float32).reshape(2, 256, 8, 256)
    uq = np.empty((2, 2, 384, 768), np.float32)
    ukv = np.empty((2, 2, 256, 1024), np.float32)
    for G in range(2):
        hs = [4 * G + i for i in range(4)]
        uq[:, G, :, 0:512] = wuq[:, :, hs, 0:128].reshape(2, 384, 512)
        rope_order = [4 * G + 0, 4 * G + 2, 4 * G + 1, 4 * G + 3]
        uq[:, G, :, 512:768] = wuq[:, :, rope_order, 128:192].reshape(2, 384, 256)
        ukv[:, G, :, 0:512] = wukv[:, :, hs, 0:128].reshape(2, 256, 512)
        ukv[:, G, :, 512:1024] = wukv[:, :, hs, 128:256].reshape(2, 256, 512)
    w["mla_wuq"] = uq
    w["mla_wukv"] = ukv
    w["mla_wo"] = np.asarray(inp["mla_w_o"], np.float32)
    w["cv_w1"] = np.asarray(inp["cv_w_pw1"][0], np.float32)
    w["cv_w2"] = np.asarray(inp["cv_w_pw2"][0], np.float32)
    w["hg_wi"] = np.asarray(inp["hg_w_in"][0], np.float32)
    w["hg_wo"] = np.asarray(inp["hg_w_o"][0], np.float32)
    return w


ALL_LAYERS = [("mla", 0, 0), ("mlp", 0, 0), ("hgrn", 1, 0), ("mlp", 1, 0),
              ("conv", 2, 0), ("mlp", 2, 0), ("mla", 3, 1), ("mlp", 3, 0)]


def run(inp, layers, n_cores=N_CORES, n_seq=2, trace=False, debug=False, stop=None):
    pf, pt, lay = pack_inputs(inp)
    nc, stats = build_program(n_seq, layers, lay, debug, stop)
    x = np.asarray(inp["x"], np.float32)
    pos = np.asarray(inp["positions"], np.int32)
    wts = pack_weights(inp)
    in_maps = []
    for c in range(n_cores):
        sl = slice(c * n_seq, (c + 1) * n_seq)
        m = dict(
            x=np.ascontiguousarray(x[sl]),
            pos=np.ascontiguousarray(pos[sl].reshape(n_seq, NT, 128).transpose(0, 2, 1)),
            pf=pf, pt=pt, **wts,
        )
        in_maps.append(m)
    res = run_bass_kernel_spmd(nc, in_maps, core_ids=list(range(n_cores)), **({"trace": True} if trace else {}))
    out = np.concatenate([r["out"] for r in res.results], axis=0)
    return out, res, stats


def kernel(**inputs):
    out, _, _ = run(inputs, ALL_LAYERS)
    return out.astype(np.float32)
```

```python
import math
import numpy as np
from contextlib import ExitStack
from functools import partial

import concourse.bass as bass
import concourse.mybir as mybir
from concourse.bass_utils import run_bass_kernel_spmd

F32 = mybir.dt.float32
BF16 = mybir.dt.bfloat16
I32 = mybir.dt.int32
AF = mybir.ActivationFunctionType
ALU = mybir.AluOpType
AX = mybir.AxisListType

S = 2048
D = 1024
NT = 16
DFF = 4096
EPS = 1e-6
N_CORES = 8
ENGS = ("pe", "act", "dve", "pool", "sp")


class Buf:
    __slots__ = ("name", "last_w", "readers")

    def __init__(self, name):
        self.name = name
        self.last_w = None
        self.readers = []


class Prog:
    def __init__(self, nc):
        self.nc = nc
        self.ins = []
        self.last_on_eng = {e: None for e in ENGS}
        self.last_dma = {}
        self.pending_fence = {e: None for e in ENGS}

    def add(self, eng, fn, reads=(), writes=(), dma=None):
        i = len(self.ins)
        deps = set()
        for b in reads:
            if b.last_w is not None:
                deps.add(b.last_w)
        for b in writes:
            if b.last_w is not None:
                deps.add(b.last_w)
            deps.update(b.readers)
        if self.pending_fence[eng] is not None:
            deps |= self.pending_fence[eng]
            self.pending_fence[eng] = None
        self.ins.append(dict(eng=eng, fn=fn, deps=deps, dma=dma, sig=False))
        for b in reads:
            b.readers.append(i)
        for b in writes:
            b.last_w = i
            b.readers = []
        self.last_on_eng[eng] = i
        if dma is not None:
            self.last_dma[dma] = i
        return i

    def fence(self):
        s = set(v for v in self.last_on_eng.values() if v is not None)
        s |= set(self.last_dma.values())
        for e in ENGS:
            self.pending_fence[e] = set(s) | (self.pending_fence[e] or set())

    def emit(self, es, final_wait_groups=()):
        nc = self.nc
        ins = self.ins
        for r in ins:
            nd = set()
            for d in r["deps"]:
                p = ins[d]
                if p["dma"] is None and r["dma"] is None and p["eng"] == r["eng"] and r["eng"] == "pe":
                    continue
                nd.add(d)
            r["deps"] = nd
            for d in nd:
                ins[d]["sig"] = True
        eng_sem = {e: es.enter_context(nc.semaphore("s_" + e)) for e in ("pe", "act", "dve", "pool")}
        grp_sem = {}
        for r in ins:
            if r["dma"] is not None and r["dma"] not in grp_sem:
                grp_sem[r["dma"]] = es.enter_context(nc.semaphore("g_" + r["dma"]))
        cnt = {e: 0 for e in eng_sem}
        gcnt = {g: 0 for g in grp_sem}
        for r in ins:
            if r["dma"] is not None:
                gcnt[r["dma"]] += 16
                r["tok"] = ("g", r["dma"], gcnt[r["dma"]])
            elif r["sig"]:
                cnt[r["eng"]] += 1
                r["tok"] = ("e", r["eng"], cnt[r["eng"]])
        gtot = {g: 0 for g in grp_sem}
        per_eng = {e: [] for e in ENGS}
        known = {e: {} for e in ENGS}
        for r in ins:
            waits = {}
            for d in r["deps"]:
                kind, key, val = ins[d]["tok"]
                if kind == "g":
                    val = max(val, gtot[key])
                k = (kind, key)
                waits[k] = max(waits.get(k, 0), val)
            if r["dma"] is not None:
                gtot[r["dma"]] += 16
            kn = known[r["eng"]]
            wl = []
            for k, v in waits.items():
                if kn.get(k, 0) >= v:
                    continue
                kn[k] = v
                wl.append((k, v))
            per_eng[r["eng"]].append((r, wl))
        self.stats = dict(n={e: len(per_eng[e]) for e in ENGS}, sem=dict(cnt), nsem=len(grp_sem) + 4)

        def semof(k):
            return eng_sem[k[1]] if k[0] == "e" else grp_sem[k[1]]

        def run(engname, eobj):
            for r, wl in per_eng[engname]:
                for k, v in wl:
                    eobj.wait_ge(semof(k), v)
                bi = r["fn"](eobj)
                if r["dma"] is not None:
                    bi.then_inc(grp_sem[r["dma"]], 16)
                elif r["sig"]:
                    bi.then_inc(eng_sem[r["eng"]], 1)
            if engname == "sp":
                for g in final_wait_groups:
                    eobj.wait_ge(grp_sem[g], gcnt[g])

        with nc.Block() as block:
            @block.tensor
            def _(e):
                run("pe", e)

            @block.scalar
            def _(e):
                run("act", e)

            @block.vector
            def _(e):
                run("dve", e)

            @block.gpsimd
            def _(e):
                run("pool", e)

            @block.sync
            def _(e):
                run("sp", e)


class K:
    def __init__(self, nc, es, n_seq):
        self.nc = nc
        self.es = es
        self.P = Prog(nc)
        self.n_seq = n_seq
        self.uid = 0
        self.debug = False
        self.stop = None
        self.dbg_names = []

    def sb(self, name, shape, dt):
        return self.es.enter_context(self.nc.sbuf_tensor("sb_" + name, shape, dt))

    def mm(self, out, lhsT, rhs, start, stop, reads, writes):
        self.P.add("pe", lambda e: e.matmul(out, lhsT=lhsT, rhs=rhs, start=start, stop=stop), reads, writes)

    def tr(self, out, in_, ident, reads, writes):
        self.P.add("pe", lambda e: e.transpose(out=out, in_=in_, identity=ident), reads, writes)

    def act(self, out, in_, func, reads, writes, **kw):
        self.P.add("act", lambda e: e.activation(out=out, in_=in_, func=func, **kw), reads, writes)

    def tt(self, eng, out, in0, in1, op, reads, writes):
        self.P.add(eng, lambda e: e.tensor_tensor(out=out, in0=in0, in1=in1, op=op), reads, writes)

    def ts(self, eng, out, in0, s1, s2, op0, op1, reads, writes):
        if s2 is None:
            self.P.add(eng, lambda e: e.tensor_scalar(out=out, in0=in0, scalar1=s1, scalar2=None, op0=op0), reads, writes)
        else:
            self.P.add(eng, lambda e: e.tensor_scalar(out=out, in0=in0, scalar1=s1, scalar2=s2, op0=op0, op1=op1), reads, writes)

    def stt(self, eng, out, in0, scalar, in1, op0, op1, reads, writes):
        self.P.add(eng, lambda e: e.scalar_tensor_tensor(out=out, in0=in0, scalar=scalar, in1=in1, op0=op0, op1=op1), reads, writes)

    def cp(self, eng, out, in_, reads, writes):
        self.P.add(eng, lambda e: e.tensor_copy(out=out, in_=in_), reads, writes)

    def recip(self, out, in_, reads, writes):
        self.P.add("dve", lambda e: e.reciprocal(out=out, in_=in_), reads, writes)

    def memset(self, eng, ap, val, writes):
        self.P.add(eng, lambda e: e.memset(ap, val), (), writes)

    def dma(self, eng, out, in_, grp, reads=(), writes=()):
        self.P.add(eng, lambda e: e.dma_start(out=out, in_=in_), reads, writes, dma=grp)

    def arena_reset(self):
        self.P.fence()
        self.aoff = 0

    def carve(self, free_shape, dt):
        n = int(np.prod(free_shape))
        nbytes = n * (4 if dt in (F32, I32) else 2)
        nbytes = (nbytes + 63) // 64 * 64
        assert self.aoff + nbytes <= self.arena_bytes, (self.aoff, nbytes, self.arena_bytes)
        ap = self.arena[:, self.aoff // 2:(self.aoff + nbytes) // 2]
        self.aoff += nbytes
        if dt != BF16:
            ap = ap.bitcast(dt)
        ap = ap[:, 0:n]
        if len(free_shape) == 2:
            ap = ap.rearrange("p (a b) -> p a b", b=free_shape[1])
        elif len(free_shape) == 3:
            ap = ap.rearrange("p (a b c) -> p a b c", b=free_shape[1], c=free_shape[2])
        return ap

    def buf(self, name):
        self.uid += 1
        return Buf(f"{name}_{self.uid}")

    def arena_mark_reset(self, mark):
        self.P.fence()
        self.aoff = mark

    def red(self, out, in_, reads, writes):
        self.P.add("dve", lambda e: e.tensor_reduce(out=out, in_=in_, axis=AX.X, op=ALU.add), reads, writes)

    def dump(self, name, ap, shape, dt, reads):
        if not getattr(self, "debug", False) or ("dbg_" + name) in self.dbg_names:
            return
        d = self.nc.dram_tensor("dbg_" + name, list(shape), dt, kind="ExternalOutput").ap()
        self.dma("sp", d, ap, "dbg", reads=reads)
        self.dbg_names.append("dbg_" + name)


def build_program(n_seq, layers, lay, debug=False, stop=None):
    nc = bass.Bass("TRN2", target_bir_lowering=False)
    es = ExitStack()
    k = K(nc, es, n_seq)
    k.debug = debug
    k.stop = stop
    P = k.P
    dt = lambda name, shape, d, kind="ExternalInput": nc.dram_tensor(name, shape, d, kind=kind).ap()
    x_d = dt("x", [n_seq, S, D], F32)
    out_d = dt("out", [n_seq, S, D], F32, "ExternalOutput")
    pos_d = dt("pos", [n_seq, 128, NT], I32)
    pf_d = dt("pf", [128, lay["npf"]], F32)
    pt_d = dt("pt", [128, lay["npt"]], F32)
    mlp_wi_d = dt("mlp_wi", [4, D, DFF], F32)
    mlp_wo_d = dt("mlp_wo", [4, DFF, D], F32)
    k.dram = dict(x=x_d, out=out_d, pos=pos_d, pf=pf_d, pt=pt_d, mlp_wi=mlp_wi_d, mlp_wo=mlp_wo_d)
    k.dram["mla_wd"] = dt("mla_wd", [2, D, 704], F32)
    k.dram["mla_wuq"] = dt("mla_wuq", [2, 2, 384, 768], F32)
    k.dram["mla_wukv"] = dt("mla_wukv", [2, 2, 256, 1024], F32)
    k.dram["mla_wo"] = dt("mla_wo", [2, D, D], F32)
    k.dram["cv_w1"] = dt("cv_w1", [D, 2 * D], F32)
    k.dram["cv_w2"] = dt("cv_w2", [D, D], F32)
    k.dram["hg_wi"] = dt("hg_wi", [D, 4 * D], F32)
    k.dram["hg_wo"] = dt("hg_wo", [D, D], F32)
    k.lay = lay

    k.x = k.sb("x", [128, NT, D], F32)
    k.xb = [Buf(f"x{i}") for i in range(NT)]
    k.pf = k.sb("pf", [128, lay["npf"]], F32)
    k.pfb = Buf("pf")
    k.ident = k.sb("ident", [128, 128], BF16)
    k.identf = k.sb("identf", [128, 128], F32)
    k.cb = Buf("consts")
    k.ss = k.sb("ss", [128, 64], F32)
    k.ps = [es.enter_context(nc.psum_tensor(f"ps{i}", [128, 512], F32)) for i in range(8)]
    k.psb = [Buf(f"ps{i}") for i in range(8)]
    k.pt = k.sb("pt", [128, lay["npt_res"]], F32)
    k.ptb = Buf("pt")
    k.ones_bf = k.sb("ones_bf", [128, 128], BF16)
    k.invn12 = k.sb("invn12", [128, 12], F32)
    k.ones_f = k.sb("ones_f", [128, 128], F32)
    k.hgc = k.sb("hgc", [128, 72], F32)
    k.mask2 = k.sb("mask2", [128, 128], F32)
    k.negpi = k.sb("negpi", [128, 1], F32)
    k.rope_invf = k.sb("rope_invf", [128, 32], F32)
    k.rope_posi = k.sb("rope_posi", [128, NT], I32)
    k.rope_posf = k.sb("rope_posf", [128, NT], F32)
    k.cos2 = k.sb("cos2", [128, NT, 64], F32)
    k.sinA = k.sb("sinA", [128, NT, 64], F32)
    k.ropeb = Buf("rope")
    k.arena_bytes = 128 * 1024
    k.arena = k.sb("arena", [128, k.arena_bytes // 2], BF16)
    k.aoff = 0

    k.dma("sp", k.pf[:], pf_d, "pf", writes=[k.pfb])
    k.memset("pool", k.identf[:], 0.0, [k.cb])
    P.add("pool", lambda e: e.affine_select(out=k.identf[:], in_=k.identf[:], pattern=[[-1, 128]],
                                            compare_op=ALU.not_equal, fill=1.0, base=0, channel_multiplier=1),
          [k.cb], [k.cb])
    k.cp("dve", k.ident[:], k.identf[:], [k.cb], [k.cb])
    k.dma("sp", k.pt[:], pt_d[:, 0:lay["npt_res"]], "pt", writes=[k.ptb])
    k.memset("dve", k.ones_bf[:], 1.0, [k.cb])
    k.memset("dve", k.ones_f[:], 1.0, [k.cb])
    k.memset("dve", k.invn12[:, 0:4], 1.0 / 128, [k.cb])
    k.memset("dve", k.invn12[:, 4:8], 1.0 / 64, [k.cb])
    k.memset("dve", k.invn12[:, 8:12], 1.0 / 128, [k.cb])
    k.memset("dve", k.negpi[:], -math.pi * (1.0 - 1e-6), [k.cb])
    for f in range(32):
        k.memset("pool", k.rope_invf[:, f:f + 1], float(np.float32(10000.0) ** np.float32(-2.0 * f / 64)), [k.cb])

    for (kind, l, j) in layers:
        if kind == "hgrn":
            emit_hgrn_consts(k, l)
    for s in range(n_seq):
        for i in range(NT):
            k.dma("sp", k.x[:, i, :], x_d[s, i * 128:(i + 1) * 128, :], "xio", writes=[k.xb[i]])
        if any(kd == "mla" for kd, _, _ in layers):
            emit_rope_tables(k, s)
        for (kind, l, j) in layers:
            if kind == "mlp":
                emit_mlp(k, l)
            elif kind == "mla":
                emit_mla(k, l, j)
            elif kind == "conv":
                emit_conv(k, l)
            elif kind == "hgrn":
                emit_hgrn(k, l)
            else:
                raise ValueError(kind)
        for i in range(NT):
            k.dma("sp", out_d[s, i * 128:(i + 1) * 128, :], k.x[:, i, :], "xio", reads=[k.xb[i]])
    P.emit(es, final_wait_groups=["xio"] + (["dbg"] if k.dbg_names else []))
    es.close()
    return nc, P.stats


def emit_norm_T(k, tiles, gcol, dst, tmp, after=None, lag=0):
    tiles = list(tiles)
    for n, i in enumerate(tiles):
        slot = n % 2
        junk, jb = tmp["junk"][slot], tmp["junkb"][slot]
        xn, xnb = tmp["xn"][slot], tmp["xnb"][slot]
        st, stb = tmp["st"][slot], tmp["stb"][slot]
        pb = tmp["psum"][slot]
        k.act(junk, k.x[:, i, :], AF.Square, [k.xb[i]], [jb, stb], accum_out=st[:, 0:1])
        k.act(st[:, 1:2], st[:, 0:1], AF.Sqrt, [stb], [stb], bias=EPS, scale=1.0 / D)
        k.recip(st[:, 2:3], st[:, 1:2], [stb], [stb])
        k.act(xn, k.x[:, i, :], AF.Copy, [k.xb[i], stb], [xnb], scale=st[:, 2:3])
        pbf = k.ps[pb][:].bitcast(BF16)
        for c in range(8):
            k.tr(pbf[:, c * 128:(c + 1) * 128], xn[:, c * 128:(c + 1) * 128], k.ident[:], [xnb, k.cb], [k.psb[pb]])
        d_ap, d_buf = dst(i)
        g = k.pf[:, gcol:gcol + 8]
        k.tt("dve", d_ap, pbf.rearrange("p (c t) -> p c t", t=128),
             g.unsqueeze(2).to_broadcast([128, 8, 128]), ALU.mult, [k.psb[pb], k.pfb], [d_buf])
        if after is not None:
            if lag == 0:
                after(i)
            elif n >= lag:
                after(tiles[n - lag])
    if after is not None and lag > 0:
        for i in tiles[len(tiles) - lag:]:
            after(i)


def norm_tmp(k, psum_banks):
    t = dict(junk=[], junkb=[], xn=[], xnb=[], st=[], stb=[], psum=psum_banks)
    for s in range(2):
        t["junk"].append(k.carve([D], BF16))
        t["junkb"].append(k.buf("junk"))
        t["xn"].append(k.carve([D], BF16))
        t["xnb"].append(k.buf("xn"))
        t["st"].append(k.carve([8], F32))
        t["stb"].append(k.buf("st"))
    return t


def emit_mlp(k, l):
    P = k.P
    k.arena_reset()
    hT = k.carve([8, S], BF16)
    hTb = [k.buf("hT") for _ in range(4)]
    tmp = norm_tmp(k, [6, 7])
    wi = [k.carve([8, 512], BF16) for _ in range(2)]
    wo = [k.carve([4, D], BF16) for _ in range(2)]
    wib = [k.buf("wi") for _ in range(2)]
    wob = [k.buf("wo") for _ in range(2)]
    aT = [k.carve([4, 512], BF16) for _ in range(2)]
    aTb = [k.buf("aT") for _ in range(2)]
    r = [k.carve([512], F32) for _ in range(2)]
    rb = [k.buf("r") for _ in range(2)]

    def norm_block(tb):
        emit_norm_T(k, range(4 * tb, 4 * tb + 4), k.lay["norm_mlp"] + 8 * l,
                    lambda i: (hT[:, :, i * 128:(i + 1) * 128], hTb[i // 4]), tmp)

    wi_d = k.dram["mlp_wi"]
    wo_d = k.dram["mlp_wo"]

    def load(g):
        sl = g % 2
        k.dma("pool", wi[sl], wi_d[l, :, g * 512:(g + 1) * 512].rearrange("(kc p) f -> p kc f", p=128),
              f"mwi{sl}", writes=[wib[sl]])
        k.dma("pool", wo[sl], wo_d[l, g * 512:(g + 1) * 512, :].rearrange("(fc p) d -> p fc d", p=128),
              f"mwo{sl}", writes=[wob[sl]])

    steps = [(g, tb) for g in range(8) for tb in range(4)]
    abank = [0, 1, 2, 3]
    ybank = [4, 5]
    state = dict(na=0, nr=0)

    def stage1(si):
        g, tb = steps[si]
        sl = g % 2
        a = si % 2
        for m in range(4):
            b = abank[state["na"] % 4]
            state["na"] += 1
            for kc in range(8):
                k.mm(k.ps[b][:], wi[sl][:, kc, m * 128:(m + 1) * 128], hT[:, kc, tb * 512:(tb + 1) * 512],
                     kc == 0, kc == 7, [wib[sl], hTb[tb]], [k.psb[b]])
            rr = state["nr"] % 2
            state["nr"] += 1
            k.act(r[rr], k.ps[b][:], AF.Relu, [k.psb[b]], [rb[rr]])
            k.act(aT[a][:, m, :], r[rr], AF.Square, [rb[rr]], [aTb[a]])

    def stage2(si):
        g, tb = steps[si]
        sl = g % 2
        a = si % 2
        for tt in range(4):
            i = tb * 4 + tt
            for half in range(2):
                b = ybank[half]
                for m in range(4):
                    k.mm(k.ps[b][:], aT[a][:, m, tt * 128:(tt + 1) * 128], wo[sl][:, m, half * 512:(half + 1) * 512],
                         m == 0, m == 3, [aTb[a], wob[sl]], [k.psb[b]])
                xs = k.x[:, i, half * 512:(half + 1) * 512]
                k.tt("dve", xs, xs, k.ps[b][:], ALU.add, [k.xb[i], k.psb[b]], [k.xb[i]])

    load(0)
    norm_block(0)
    for si in range(len(steps) + 1):
        if si < len(steps):
            stage1(si)
            if si + 1 < 4:
                norm_block(si + 1)
        if si >= 1:
            stage2(si - 1)
        if si < len(steps):
            g, tb = steps[si]
            if tb == 0 and g + 1 < 8:
                load(g + 1)


def emit_rope_tables(k, s):
    posi = k.rope_posi[:]
    k.dma("sp", posi, k.dram["pos"][s], "pos", writes=[k.ropeb])
    k.cp("dve", k.rope_posf[:], posi, [k.ropeb], [k.ropeb])
    k.arena_reset()
    ang = k.carve([NT, 32], F32)
    k.tt("dve", ang, k.rope_posf[:].unsqueeze(2).to_broadcast([128, NT, 32]),
         k.rope_invf[:].unsqueeze(1).to_broadcast([128, NT, 32]), ALU.mult, [k.ropeb, k.cb], [k.ropeb])
    r1 = k.carve([NT, 32], F32)
    ki = k.carve([NT, 32], I32)
    kf = k.carve([NT, 32], F32)
    sc = 1.0 - 1e-6
    rb = [k.ropeb]
    two_pi = 2 * math.pi

    def reduce_to_pi(shift):
        k.ts("dve", kf, ang, shift, 1.0 / two_pi, ALU.add, ALU.mult, rb, rb)
        k.cp("dve", ki, kf, rb, rb)
        k.cp("dve", kf, ki, rb, rb)
        k.stt("dve", r1, kf, -two_pi, ang, ALU.mult, ALU.add, rb, rb)
        if shift != 0.0:
            k.ts("dve", r1, r1, shift, None, ALU.add, None, rb, rb)
        k.ts("dve", kf, r1, math.pi, two_pi, ALU.is_gt, ALU.mult, rb, rb)
        k.tt("dve", r1, r1, kf, ALU.subtract, rb, rb)
        k.ts("dve", kf, r1, -math.pi, two_pi, ALU.is_lt, ALU.mult, rb, rb)
        k.tt("dve", r1, r1, kf, ALU.add, rb, rb)

    reduce_to_pi(0.0)
    k.act(k.sinA[:, :, 32:64], r1, AF.Sin, rb, rb, scale=sc)
    k.ts("dve", k.sinA[:, :, 0:32], k.sinA[:, :, 32:64], -1.0, None, ALU.mult, None, rb, rb)
    reduce_to_pi(math.pi / 2)
    k.act(k.cos2[:, :, 0:32], r1, AF.Sin, rb, rb, scale=sc)
    k.cp("dve", k.cos2[:, :, 32:64], k.cos2[:, :, 0:32], rb, rb)


def emit_rope(k, out_bf, t, tmp, o, i, nh, reads, writes, tb):
    v3 = lambda ap: ap.rearrange("p (h d) -> p h d", d=64)
    cosb = k.cos2[:, i, :].unsqueeze(1).to_broadcast([128, nh, 64])
    sa = k.sinA[:, i, :]
    s_lo = sa[:, 0:32].unsqueeze(1).to_broadcast([128, nh, 32])
    s_hi = sa[:, 32:64].unsqueeze(1).to_broadcast([128, nh, 32])
    k.tt("dve", v3(tmp)[:, :, 0:32], v3(t)[:, :, 32:64], s_lo, ALU.mult, reads + [k.ropeb], [tb])
    k.tt("dve", v3(tmp)[:, :, 32:64], v3(t)[:, :, 0:32], s_hi, ALU.mult, reads + [k.ropeb], [tb])
    k.tt("dve", v3(o), v3(t), cosb, ALU.mult, reads + [k.ropeb], [tb])
    k.tt("dve", out_bf, o, tmp, ALU.add, [tb], writes)


def emit_mla(k, l, j):
    P = k.P
    lay = k.lay
    if k.stop == "rope":
        return
    k.arena_reset()
    ps, psb = k.ps, k.psb
    cT = k.carve([5, S], BF16)
    cTb = [k.buf("cT") for _ in range(NT)]
    krT = k.carve([S], BF16)
    krTb = [k.buf("krT") for _ in range(NT)]
    mark = k.aoff
    wd = k.carve([8, 704], BF16)
    wdb = k.buf("wd")
    k.dma("pool", wd, k.dram["mla_wd"][j].rearrange("(kc p) f -> p kc f", p=128), "mla_wd", writes=[wdb])
    tmp = norm_tmp(k, [6, 7])
    hTt = [k.carve([8, 128], BF16) for _ in range(2)]
    hTtb = [k.buf("hTt") for _ in range(2)]
    two = lambda shape, dt_: ([k.carve(shape, dt_) for _ in range(2)], [k.buf("pa") for _ in range(2)])
    cqn_, cqnb_ = two([640], BF16)
    junkA_, junkAb_ = two([384], BF16)
    st2_, st2b_ = two([16], F32)
    krf_, krfb_ = two([64], F32)
    rt_, rtb_ = two([64], F32)
    ro_, _ = two([64], F32)
    krb_, krbb_ = two([128], BF16)
    gl = lay["mla_lat"] + 5 * j
    gt = lay["mla_head"] + 384 * j
    cnt = [0]

    def passA(i):
        sl = i % 2
        h, hb = hTt[sl], hTtb[sl]
        cqn, cqnb, junkA, junkAb, st2, st2b = cqn_[sl], cqnb_[sl], junkA_[sl], junkAb_[sl], st2_[sl], st2b_[sl]
        krf, krfb, rt, rtb, ro, krb, krbb = krf_[sl], krfb_[sl], rt_[sl], rtb_[sl], ro_[sl], krb_[sl], krbb_[sl]
        b0, b1, b2 = (0, 1, 2) if sl == 0 else (3, 4, 5)
        for kc in range(8):
            k.mm(ps[b0][:, 0:384], h[:, kc, :], wd[:, kc, 0:384], kc == 0, kc == 7, [hb, wdb], [psb[b0]])
        for kc in range(8):
            k.mm(ps[b1][:, 0:320], h[:, kc, :], wd[:, kc, 384:704], kc == 0, kc == 7, [hb, wdb], [psb[b1]])
        k.act(junkA[:, 0:384], ps[b0][:, 0:384], AF.Square, [psb[b0]], [junkAb, st2b], accum_out=st2[:, 0:1])
        k.act(junkA[:, 0:256], ps[b1][:, 0:256], AF.Square, [psb[b1]], [junkAb, st2b], accum_out=st2[:, 1:2])
        k.act(junkA[:, 0:64], ps[b1][:, 256:320], AF.Square, [psb[b1]], [junkAb, st2b], accum_out=st2[:, 2:3])
        for c, n in enumerate((384, 256, 64)):
            k.act(st2[:, 3 + c:4 + c], st2[:, c:c + 1], AF.Sqrt, [st2b], [st2b], bias=EPS, scale=1.0 / n)
        k.recip(st2[:, 6:9], st2[:, 3:6], [st2b], [st2b])
        if k.stop == "A1":
            return
        k.act(cqn[:, 0:384], ps[b0][:, 0:384], AF.Copy, [psb[b0], st2b], [cqnb], scale=st2[:, 6:7])
        k.act(cqn[:, 384:640], ps[b1][:, 0:256], AF.Copy, [psb[b1], st2b], [cqnb], scale=st2[:, 7:8])
        if k.stop == "A2":
            return
        k.stt("dve", krf, ps[b1][:, 256:320], st2[:, 8:9], k.pt[:, gt + 320:gt + 384], ALU.mult, ALU.mult,
              [psb[b1], st2b, k.ptb], [krfb])
        emit_rope(k, krb[:, 0:64], krf, rt, ro, i, 1, [krfb], [krbb], rtb)
        k.cp("dve", krb[:, 64:128], krb[:, 0:64], [krbb], [krbb])
        if k.stop == "A3":
            return
        pbf = ps[b2][:].bitcast(BF16)
        for c in range(5):
            k.tr(pbf[:, c * 128:(c + 1) * 128], cqn[:, c * 128:(c + 1) * 128], k.ident[:], [cqnb, k.cb], [psb[b2]])
        k.tr(pbf[:, 640:768], krb, k.ident[:], [krbb, k.cb], [psb[b2]])
        g = k.pf[:, gl:gl + 5]
        k.tt("dve", cT[:, :, i * 128:(i + 1) * 128], pbf[:, 0:640].rearrange("p (c t) -> p c t", t=128),
             g.unsqueeze(2).to_broadcast([128, 5, 128]), ALU.mult, [psb[b2], k.pfb], [cTb[i]])
        if k.stop == "A4":
            return
        k.cp("dve", krT[:, i * 128:(i + 1) * 128], pbf[:, 640:768], [psb[b2]], [krTb[i]])

    emit_norm_T(k, range(NT), lay["norm_mix"] + 8 * l, lambda i: (hTt[i % 2], hTtb[i % 2]), tmp, after=passA, lag=1)
    k.dump("cT", cT, [128, 5, S], BF16, cTb)
    k.dump("krT", krT, [128, S], BF16, krTb)

    if k.stop in ("passA", "A1", "A2", "A3", "A4"):
        return
    k.arena_mark_reset(mark)
    wuq = [k.carve([3, 768], BF16)]
    wukv = [k.carve([2, 1024], BF16)]
    wo = k.carve([4, D], BF16)
    wuqb = [k.buf("wuq")]
    wukvb = [k.buf("wukv")]
    wob = k.buf("wo")
    kT = k.carve([4, S], BF16); kTb = [k.buf("kT") for _ in range(NT)]
    v = k.carve([NT, 512], BF16); vb = [k.buf("v") for _ in range(NT)]
    qTn = [k.carve([4, 512], BF16) for _ in range(2)]; qTnb = [k.buf("qTn") for _ in range(2)]
    qTr = [k.carve([2, 512], BF16) for _ in range(2)]; qTrb = [k.buf("qTr") for _ in range(2)]
    oT = k.carve([4, 512], BF16); oTb = k.buf("oT")
    pT = [k.carve([512], BF16) for _ in range(3)]; pTb = [k.buf("pT") for _ in range(3)]
    rden = k.carve([512], F32); rdenb = k.buf("rden")
    two = lambda shape, dt_: ([k.carve(shape, dt_) for _ in range(2)], [k.buf("tp") for _ in range(2)])
    sq1_, sq1b_ = two([512], F32)
    sq2_, sq2b_ = two([256], F32)
    sq3_, sq3b_ = two([512], F32)
    ssq_, ssqb_ = two([48], F32)
    tq_, tqb_ = two([512], F32)
    tr__, trb_ = two([256], F32)
    trt_, trtb_ = two([256], F32)
    tro_, _ = two([256], F32)
    t3_, t3b_ = two([512], F32)
    qnb__, qnbb_ = two([512], BF16)
    qrb__, qrbb_ = two([256], BF16)
    knb__, knbb_ = two([512], BF16)
    h4 = lambda ap, d: ap.rearrange("p (h d) -> p h d", d=d)
    scale = 192.0 ** -0.5

    def load_group(G):
        sl = 0
        k.dma("pool", wuq[sl], k.dram["mla_wuq"][j, G].rearrange("(kc p) f -> p kc f", p=128), f"wuq{sl}", writes=[wuqb[sl]])
        k.dma("pool", wukv[sl], k.dram["mla_wukv"][j, G].rearrange("(kc p) f -> p kc f", p=128), f"wukv{sl}", writes=[wukvb[sl]])

    def load_wo(G):
        k.dma("pool", wo, k.dram["mla_wo"][j, G * 512:(G + 1) * 512, :].rearrange("(h p) d -> p h d", p=128), "mla_wo", writes=[wob])

    def tile_proj_gen(G, qb):
        sl = 0
        qs = qb % 2
        Bqn, Bqr, Bk = 5, 6, 7
        for i in range(4 * qb, 4 * qb + 4):
            tt = i - 4 * qb
            csl = slice(i * 128, (i + 1) * 128)
            pr = i % 2
            sq1, sq1b, sq2, sq2b, sq3, sq3b, ssq, ssqb = sq1_[pr], sq1b_[pr], sq2_[pr], sq2b_[pr], sq3_[pr], sq3b_[pr], ssq_[pr], ssqb_[pr]
            tq, tqb, tr_, trb, trt, trtb, tro, t3, t3b = tq_[pr], tqb_[pr], tr__[pr], trb_[pr], trt_[pr], trtb_[pr], tro_[pr], t3_[pr], t3b_[pr]
            qnb_, qnbb, qrb_, qrbb, knb_, knbb = qnb__[pr], qnbb_[pr], qrb__[pr], qrbb_[pr], knb__[pr], knbb_[pr]
            for kc in range(3):
                k.mm(ps[Bqn][:], cT[:, kc, csl], wuq[sl][:, kc, 0:512], kc == 0, kc == 2, [cTb[i], wuqb[sl]], [psb[Bqn]])
            for kc in range(3):
                k.mm(ps[Bqr][:, 0:256], cT[:, kc, csl], wuq[sl][:, kc, 512:768], kc == 0, kc == 2, [cTb[i], wuqb[sl]], [psb[Bqr]])
            for kc in range(2):
                k.mm(ps[Bk][:], cT[:, 3 + kc, csl], wukv[sl][:, kc, 0:512], kc == 0, kc == 1, [cTb[i], wukvb[sl]], [psb[Bk]])
            yield
            k.act(sq1, ps[Bqn][:], AF.Square, [psb[Bqn]], [sq1b])
            k.act(sq2, ps[Bqr][:, 0:256], AF.Square, [psb[Bqr]], [sq2b])
            k.act(sq3, ps[Bk][:], AF.Square, [psb[Bk]], [sq3b])
            yield
            k.red(ssq[:, 0:4], h4(sq1, 128), [sq1b], [ssqb])
            k.red(ssq[:, 4:8], h4(sq2, 64), [sq2b], [ssqb])
            k.red(ssq[:, 8:12], h4(sq3, 128), [sq3b], [ssqb])
            yield
            k.tt("dve", ssq[:, 12:24], ssq[:, 0:12], k.invn12[:], ALU.mult, [ssqb, k.cb], [ssqb])
            k.act(ssq[:, 24:36], ssq[:, 12:24], AF.Sqrt, [ssqb], [ssqb], bias=EPS, scale=1.0)
            k.recip(ssq[:, 36:48], ssq[:, 24:36], [ssqb], [ssqb])
            rs = ssq[:, 36:48]
            yield
            k.tt("dve", h4(tq, 128), h4(ps[Bqn][:], 128), rs[:, 0:4].unsqueeze(2).to_broadcast([128, 4, 128]), ALU.mult,
                 [psb[Bqn], ssqb], [tqb])
            yield
            for kc in range(2):
                k.mm(ps[Bqn][:], cT[:, 3 + kc, csl], wukv[sl][:, kc, 512:1024], kc == 0, kc == 1, [cTb[i], wukvb[sl]], [psb[Bqn]])
            k.act(v[:, i, :], ps[Bqn][:], AF.Copy, [psb[Bqn]], [vb[i]])
            k.tt("dve", h4(qnb_, 128), h4(tq, 128), k.pt[:, gt:gt + 128].unsqueeze(1).to_broadcast([128, 4, 128]), ALU.mult,
                 [tqb, k.ptb], [qnbb])
            yield
            k.tt("dve", h4(tr_, 64), h4(ps[Bqr][:, 0:256], 64), rs[:, 4:8].unsqueeze(2).to_broadcast([128, 4, 64]), ALU.mult,
                 [psb[Bqr], ssqb], [trb])
            k.tt("dve", h4(tr_, 64), h4(tr_, 64), k.pt[:, gt + 128:gt + 192].unsqueeze(1).to_broadcast([128, 4, 64]), ALU.mult,
                 [trb, k.ptb], [trb])
            yield
            emit_rope(k, qrb_, tr_, trt, tro, i, 4, [trb], [qrbb], trtb)
            yield
            k.tt("dve", h4(t3, 128), h4(ps[Bk][:], 128), rs[:, 8:12].unsqueeze(2).to_broadcast([128, 4, 128]), ALU.mult,
                 [psb[Bk], ssqb], [t3b])
            k.tt("dve", h4(knb_, 128), h4(t3, 128), k.pt[:, gt + 192:gt + 320].unsqueeze(1).to_broadcast([128, 4, 128]), ALU.mult,
                 [t3b, k.ptb], [knbb])
            yield
            p6 = ps[Bqn][:].bitcast(BF16)
            for c in range(4):
                k.tr(p6[:, c * 128:(c + 1) * 128], qnb_[:, c * 128:(c + 1) * 128], k.ident[:], [qnbb, k.cb], [psb[Bqn]])
            for c in range(2):
                k.tr(p6[:, 512 + c * 128:512 + (c + 1) * 128], qrb_[:, c * 128:(c + 1) * 128], k.ident[:], [qrbb, k.cb], [psb[Bqn]])
            yield
            k.cp("dve", qTn[qs][:, :, tt * 128:(tt + 1) * 128], h4(p6[:, 0:512], 128), [psb[Bqn]], [qTnb[qs]])
            k.cp("dve", qTr[qs][:, :, tt * 128:(tt + 1) * 128], h4(p6[:, 512:768], 128), [psb[Bqn]], [qTrb[qs]])
            p7 = ps[Bk][:].bitcast(BF16)
            for c in range(4):
                k.tr(p7[:, c * 128:(c + 1) * 128], knb_[:, c * 128:(c + 1) * 128], k.ident[:], [knbb, k.cb], [psb[Bk]])
            yield
            k.cp("dve", kT[:, :, csl], h4(p7[:, 0:512], 128), [psb[Bk]], [kTb[i]])
            yield

    def attention(G, qb):
        qs = qb % 2
        nkt = 4 * qb + 4
        steps = [(hh, kt) for hh in range(4) for kt in range(nkt)]
        sbank = [2, 3, 4]
        acc = [(0, 1), (0, 1)]

        def qk(si):
            hh, kt = steps[si]
            blk, half = hh % 2, hh // 2
            jd = kt - 4 * qb
            c0 = max(0, jd) * 128
            b = sbank[si % 3]
            ks = slice(kt * 128, (kt + 1) * 128)
            k.mm(ps[b][:, 0:512 - c0], kT[:, hh, ks], qTn[qs][:, hh, c0:512], True, False, [kTb[kt], qTnb[qs]], [psb[b]])
            k.mm(ps[b][:, 0:512 - c0], krT[half * 64:(half + 1) * 64, ks], qTr[qs][half * 64:(half + 1) * 64, blk, c0:512],
                 False, True, [krTb[kt], qTrb[qs]], [psb[b]])
            pb_ = si % 3
            k.act(pT[pb_][:, 0:512 - c0], ps[b][:, 0:512 - c0], AF.Exp, [psb[b]], [pTb[pb_]], scale=scale)
            if jd >= 0:
                blkap = pT[pb_][:, 0:128]
                P.add("pool", lambda e: e.affine_select(out=blkap, in_=blkap, pattern=[[1, 128]], compare_op=ALU.is_ge,
                                                        fill=0.0, base=0, channel_multiplier=-1), [pTb[pb_]], [pTb[pb_]])

        def pv(si):
            hh, kt = steps[si]
            jd = kt - 4 * qb
            c0 = max(0, jd) * 128
            bo, bd = acc[hh % 2]
            pb_ = si % 3
            k.mm(ps[bo][:, c0:512], v[:, kt, hh * 128:(hh + 1) * 128], pT[pb_][:, 0:512 - c0], kt == 0, kt == nkt - 1,
                 [vb[kt], pTb[pb_]], [psb[bo]])
            k.mm(ps[bd][:, c0:512], k.ones_bf[:], pT[pb_][:, 0:512 - c0], kt == 0, kt == nkt - 1,
                 [k.cb, pTb[pb_]], [psb[bd]])
            if kt == nkt - 1:
                k.recip(rden, ps[bd][:], [psb[bd]], [rdenb])
                k.tt("dve", oT[:, hh, :], ps[bo][:], rden, ALU.mult, [psb[bo], rdenb], [oTb])

        for si in range(len(steps) + 2):
            if si < len(steps):
                qk(si)
            if si >= 2:
                pv(si - 2)
            yield

    def out_proj(G, qb):
        for tt in range(4):
            i = 4 * qb + tt
            for half in range(2):
                b = (3, 4)[half]
                for hh in range(4):
                    k.mm(ps[b][:], oT[:, hh, tt * 128:(tt + 1) * 128], wo[:, hh, half * 512:(half + 1) * 512],
                         hh == 0, hh == 3, [oTb, wob], [psb[b]])
                xs = k.x[:, i, half * 512:(half + 1) * 512]
                k.tt("dve", xs, xs, ps[b][:], ALU.add, [k.xb[i], psb[b]], [k.xb[i]])

    for G in range(2):
        load_group(G)
        load_wo(G)
        for _ in tile_proj_gen(G, 0):
            pass
        for qb in range(4):
            ga = attention(G, qb)
            gb = tile_proj_gen(G, qb + 1) if qb + 1 < 4 else None
            rate = (4, 2, 1, 1)[qb]
            alive_b = gb is not None
            for _ in ga:
                if alive_b:
                    for _r in range(rate):
                        try:
                            next(gb)
                        except StopIteration:
                            alive_b = False
                            break
            if alive_b:
                for _ in gb:
                    pass
            if k.stop == "attn":
                continue
            out_proj(G, qb)


def emit_conv(k, l):
    P = k.P
    lay = k.lay
    ps, psb = k.ps, k.psb
    k.arena_reset()
    w2 = k.carve([8, D], BF16); w2b = k.buf("w2")
    k.dma("pool", w2, k.dram["cv_w2"].rearrange("(cc p) d -> p cc d", p=128), "cv_w2", writes=[w2b])
    tmp = norm_tmp(k, [0, 1])
    hTt = [k.carve([8, 512], BF16) for _ in range(2)]; hTtb = [k.buf("hTt") for _ in range(2)]
    w1 = [k.carve([8, 2, 128], BF16) for _ in range(2)]; w1b = [k.buf("w1") for _ in range(2)]
    Dm = [k.carve([31, 128], BF16) for _ in range(2)]; Dmb = [k.buf("Dm") for _ in range(2)]
    uT = k.carve([8, 542], BF16); uTb = [k.buf("uT") for _ in range(8)]
    ysb = k.carve([8, 512], F32); ysbb = [k.buf("ysb") for _ in range(8)]
    ysq = [k.carve([512], F32) for _ in range(2)]; ysqb = [k.buf("ysq") for _ in range(2)]
    sig = [k.carve([512], F32) for _ in range(2)]; sigb = [k.buf("sig") for _ in range(2)]
    zT = k.carve([8, 512], BF16); zTb = [k.buf("zT") for _ in range(8)]
    mean = k.carve([512], F32); msq = k.carve([512], F32); rstd = k.carve([512], F32); stb = k.buf("cvst")
    tn = [k.carve([512], F32) for _ in range(2)]; tnb = [k.buf("tn") for _ in range(2)]
    cb1 = lay["cv_b1"]; cwd = lay["cv_wdw"]; cbd = lay["cv_bdw"]; cg = lay["cv_lng"]; cbn = lay["cv_lnb"]
    w1_d = k.dram["cv_w1"]
    b2t = k.carve([D], F32); b2b = k.buf("b2t")
    k.dma("sp", b2t, k.dram["pt"][:, lay["cv_b2"]:lay["cv_b2"] + D], "cv_b2", writes=[b2b])

    def load_w1(n):
        cc = n % 8
        sl = n % 2
        k.dma("pool", w1[sl][:, :, 0, :], w1_d[:, cc * 128:(cc + 1) * 128].rearrange("(kc p) f -> p kc f", p=128),
              f"cvw1{sl}", writes=[w1b[sl]])
        k.dma("pool", w1[sl][:, :, 1, :], w1_d[:, D + cc * 128:D + (cc + 1) * 128].rearrange("(kc p) f -> p kc f", p=128),
              f"cvw1{sl}", writes=[w1b[sl]])

    load_w1(0)
    n = 0
    for tb in range(4):
        hs = tb % 2
        emit_norm_T(k, range(4 * tb, 4 * tb + 4), lay["norm_mix"] + 8 * l,
                    lambda i: (hTt[hs][:, :, (i % 4) * 128:(i % 4 + 1) * 128], hTtb[hs]), tmp)
        for i in range(4 * tb, 4 * tb + 4):
            k.tt("pool", k.x[:, i, :], k.x[:, i, :], b2t, ALU.add, [k.xb[i], b2b], [k.xb[i]])
        def stA(cc, n):
            sl = n % 2
            if n + 1 < 32:
                load_w1(n + 1)
            wv = k.pf[:, cwd + cc * 31:cwd + (cc + 1) * 31]
            k.tt("pool", Dm[sl], k.identf[:].unsqueeze(1).to_broadcast([128, 31, 128]),
                 wv.unsqueeze(2).to_broadcast([128, 31, 128]), ALU.mult, [k.cb, k.pfb], [Dmb[sl]])
            ba, bg = cc % 2, 2 + cc % 2
            for kc in range(8):
                k.mm(ps[ba][:], w1[sl][:, kc, 0, :], hTt[hs][:, kc, :], kc == 0, kc == 7, [w1b[sl], hTtb[hs]], [psb[ba]])
            for kc in range(8):
                k.mm(ps[bg][:], w1[sl][:, kc, 1, :], hTt[hs][:, kc, :], kc == 0, kc == 7, [w1b[sl], hTtb[hs]], [psb[bg]])
            ss_ = cc % 2
            k.act(sig[ss_], ps[bg][:], AF.Sigmoid, [psb[bg], k.pfb], [sigb[ss_]], bias=k.pf[:, cb1 + 8 + cc:cb1 + 9 + cc], scale=1.0)
            if tb == 0:
                k.memset("pool", uT[:, cc, 0:30], 0.0, [uTb[cc]])
            else:
                k.cp("pool", uT[:, cc, 0:30], uT[:, cc, 512:542], [uTb[cc]], [uTb[cc]])
            k.stt("dve", uT[:, cc, 30:542], ps[ba][:], k.pf[:, cb1 + cc:cb1 + cc + 1], sig[ss_], ALU.add, ALU.mult,
                  [psb[ba], sigb[ss_], k.pfb], [uTb[cc]])

        def stB(cc, n):
            sl = n % 2
            bc = 4 + cc % 2
            ss_ = cc % 2
            for jj in range(31):
                k.mm(ps[bc][:], Dm[sl][:, jj, :], uT[:, cc, jj:jj + 512], jj == 0, jj == 30, [Dmb[sl], uTb[cc]], [psb[bc]])
            bdw = k.pf[:, cbd + cc:cbd + cc + 1]
            k.act(ysb[:, cc, :], ps[bc][:], AF.Identity, [psb[bc], k.pfb], [ysbb[cc]], bias=bdw, scale=1.0)
            k.act(ysq[ss_], ps[bc][:], AF.Square, [psb[bc], k.pfb], [ysqb[ss_]], bias=bdw, scale=1.0)

        def stC(cc):
            ss_ = cc % 2
            k.mm(ps[6][:], k.ones_f[:], ysb[:, cc, :], cc == 0, cc == 7, [k.cb, ysbb[cc]], [psb[6]])
            k.mm(ps[7][:], k.ones_f[:], ysq[ss_], cc == 0, cc == 7, [k.cb, ysqb[ss_]], [psb[7]])

        for cc in range(10):
            if cc < 8:
                stA(cc, n)
                n += 1
            if 1 <= cc <= 8:
                stB(cc - 1, n - 1 - (1 if cc < 8 else 0))
            if cc >= 2:
                stC(cc - 2)
        k.act(mean, ps[6][:], AF.Copy, [psb[6]], [stb], scale=1.0 / D)
        k.act(msq, ps[6][:], AF.Square, [psb[6]], [stb], scale=1.0 / D)
        k.stt("dve", rstd, ps[7][:], 1.0 / D, msq, ALU.mult, ALU.subtract, [psb[7], stb], [stb])
        k.act(rstd, rstd, AF.Sqrt, [stb], [stb], bias=EPS, scale=1.0)
        k.recip(rstd, rstd, [stb], [stb])
        for cc in range(8):
            ts_ = cc % 2
            k.tt("pool", tn[ts_], ysb[:, cc, :], mean, ALU.subtract, [ysbb[cc], stb], [tnb[ts_]])
            k.tt("dve", tn[ts_], tn[ts_], rstd, ALU.mult, [tnb[ts_], stb], [tnb[ts_]])
            k.act(zT[:, cc, :], tn[ts_], AF.Silu, [tnb[ts_], k.pfb], [zTb[cc]],
                  bias=k.pf[:, cbn + cc:cbn + cc + 1], scale=k.pf[:, cg + cc:cg + cc + 1])
        for tt in range(4):
            i = 4 * tb + tt
            for half in range(2):
                b = (2, 3)[half]
                for cc in range(8):
                    k.mm(ps[b][:], zT[:, cc, tt * 128:(tt + 1) * 128], w2[:, cc, half * 512:(half + 1) * 512],
                         cc == 0, cc == 7, [zTb[cc], w2b], [psb[b]])
                xs = k.x[:, i, half * 512:(half + 1) * 512]
                k.tt("dve", xs, xs, ps[b][:], ALU.add, [k.xb[i], psb[b]], [k.xb[i]])


def emit_hgrn_consts(k, l):
    lay = k.lay
    hgc = k.hgc
    cbs = [k.cb]
    lg = k.pf[:, lay["hg_lb"]:lay["hg_lb"] + 32]
    e = hgc[:, 0:32]
    k.act(e, lg, AF.Exp, [k.pfb], cbs)
    den = hgc[:, 32:40]
    k.tt("dve", den, e[:, 0:8], e[:, 8:16], ALU.add, cbs, cbs)
    k.tt("dve", den, den, e[:, 16:24], ALU.add, cbs, cbs)
    k.tt("dve", den, den, e[:, 24:32], ALU.add, cbs, cbs)
    k.recip(den, den, cbs, cbs)
    num = hgc[:, 40:48]
    k.memset("dve", num, 0.0, cbs)
    for i in range(1, l + 1):
        k.tt("dve", num, num, e[:, 8 * i:8 * i + 8], ALU.add, cbs, cbs)
    k.tt("dve", hgc[:, 48:56], num, den, ALU.mult, cbs, cbs)
    k.ts("dve", hgc[:, 56:64], hgc[:, 48:56], -1.0, 1.0, ALU.mult, ALU.add, cbs, cbs)
    k.ts("dve", hgc[:, 64:72], hgc[:, 56:64], -1.0, None, ALU.mult, None, cbs, cbs)
    k.memset("pool", k.mask2[:], 1.0, cbs)
    k.P.add("pool", lambda e_: e_.affine_select(out=k.mask2[:], in_=k.mask2[:], pattern=[[1, 128]], compare_op=ALU.is_ge,
                                                 fill=0.0, base=0, channel_multiplier=-1), cbs, cbs)
    k.memset("pool", k.mask2[0:64, 64:128], 0.0, cbs)


def emit_hgrn(k, l):
    P = k.P
    lay = k.lay
    ps, psb = k.ps, k.psb
    k.arena_reset()
    hT = k.carve([8, S], BF16); hTb = [k.buf("hT") for _ in range(4)]
    tmp = norm_tmp(k, [6, 7])
    Wp = [k.carve([8, 4, 256], BF16) for _ in range(2)]; Wpb = [k.buf("Wp") for _ in range(2)]
    wop = [k.carve([2, D], BF16) for _ in range(2)]; wopb = [k.buf("wop") for _ in range(2)]
    F = lambda: k.carve([512], F32)
    sg, f_, b_, bp, Em, t1, t1e, t2, on = [F() for _ in range(9)]
    sgb, fb, bb, bpb, Emb, t1b, t1eb, t2b, onb = [k.buf("hg") for _ in range(9)]
    two = lambda shape, dt_: ([k.carve(shape, dt_) for _ in range(2)], [k.buf("hg2") for _ in range(2)])
    Ep_, Epb_ = two([512], F32)
    gate_, gateb_ = two([512], F32)
    kT__, kTb__ = two([512], BF16)
    qT__, qTb__ = two([512], BF16)
    vT__, vTb__ = two([512], BF16)
    ktm_, ktmb_ = two([4, 128], BF16)
    vtm_, vtmb_ = two([4, 128], BF16)
    em_, emb_ = two([8], F32)
    el_, _ = two([8], F32)
    onT = [k.carve([512], BF16) for _ in range(2)]; onTb = [k.buf("onT") for _ in range(2)]
    Am = [k.carve([128], BF16) for _ in range(2)]; Amb = [k.buf("Am") for _ in range(2)]
    Sst = [k.carve([128], F32) for _ in range(2)]; Sb = [k.buf("S") for _ in range(2)]
    Stil = k.carve([128], BF16); Stilb = k.buf("Stil")
    tmpS = k.carve([128], F32); tmpSb = k.buf("tmpS")
    scanmask = k.carve([512], F32); smb = k.buf("scanmask")
    k.memset("dve", scanmask, 1.0, [smb])
    k.memset("dve", scanmask.rearrange("p (c t) -> p c t", t=64)[:, :, 0:1], 0.0, [smb])
    hc = k.hgc
    w_d = k.dram["hg_wi"]
    wo_d = k.dram["hg_wo"]
    c8 = lambda ap: ap.rearrange("p (c t) -> p c t", t=64)

    emit_norm_T(k, range(NT), lay["norm_mix"] + 8 * l, lambda i: (hT[:, :, i * 128:(i + 1) * 128], hTb[i // 4]), tmp)

    def load(hp):
        sl = hp % 2
        for kind in range(4):
            k.dma("pool", Wp[sl][:, :, kind, :],
                  w_d[:, kind * D + hp * 256:kind * D + (hp + 1) * 256].rearrange("(kc p) f -> p kc f", p=128),
                  f"hgw{sl}", writes=[Wpb[sl]])
        k.dma("pool", wop[sl], wo_d[hp * 256:(hp + 1) * 256, :].rearrange("(hh p) d -> p hh d", p=128),
              f"hgwo{sl}", writes=[wopb[sl]])

    rot = [0]

    def proj(sl, kind, hh, tb, bank):
        for kc in range(8):
            k.mm(ps[bank][:], Wp[sl][:, kc, kind, hh * 128:(hh + 1) * 128], hT[:, kc, tb * 512:(tb + 1) * 512],
                 kc == 0, kc == 7, [Wpb[sl], hTb[tb]], [psb[bank]])

    go = k.pf[:, lay["hg_on"]:lay["hg_on"] + 1]

    def pro(sl, hp, tb, hh):
        hcol = 2 * hp + hh
        lb = hc[:, 48 + hcol:49 + hcol]
        oml = hc[:, 56 + hcol:57 + hcol]
        noml = hc[:, 64 + hcol:65 + hcol]
        Ep, Epb, gate, gateb = Ep_[hh], Epb_[hh], gate_[hh], gateb_[hh]
        kT_, kTb_, qT_, qTb_, vT_, vTb_ = kT__[hh], kTb__[hh], qT__[hh], qTb__[hh], vT__[hh], vTb__[hh]
        ktm, ktmb, vtm, vtmb, em, emb, el = ktm_[hh], ktmb_[hh], vtm_[hh], vtmb_[hh], em_[hh], emb_[hh], el_[hh]
        bq = hh
        proj(sl, 0, hh, tb, bq)
        yield
        proj(sl, 1, hh, tb, 2)
        yield
        proj(sl, 2, hh, tb, 3)
        yield
        k.act(sg, ps[2][:], AF.Sigmoid, [psb[2]], [sgb])
        yield
        proj(sl, 3, hh, tb, 2)
        yield
        k.ts("dve", f_, sg, oml, lb, ALU.mult, ALU.add, [sgb, k.cb], [fb])
        yield
        k.act(f_, f_, AF.Ln, [fb], [fb])
        yield
        P.add("dve", lambda e: e.tensor_tensor_scan(out=b_, data0=scanmask, data1=f_, initial=0.0,
                                                    op0=ALU.mult, op1=ALU.add), [fb, smb], [bb])
        k.tt("dve", c8(bp), c8(b_), c8(b_)[:, :, 31:32].to_broadcast([128, 8, 64]), ALU.subtract, [bb], [bpb])
        yield
        k.act(Ep, bp, AF.Exp, [bpb], [Epb])
        yield
        k.act(Em, bp, AF.Exp, [bpb], [Emb], scale=-1.0)
        yield
        k.act(em, c8(b_)[:, :, 31], AF.Exp, [bb], [emb])
        yield
        k.act(el, c8(b_)[:, :, 63], AF.Exp, [bb], [emb])
        yield
        k.ts("dve", t1, sg, noml, oml, ALU.mult, ALU.add, [sgb, k.cb], [t1b])
        yield
        k.tt("pool", kT_, t1, Em, ALU.mult, [t1b, Emb], [kTb_])
        yield
        k.tt("dve", qT_, ps[bq][:], Ep, ALU.mult, [psb[bq], Epb], [qTb_])
        yield
        k.act(vT_, ps[3][:], AF.Copy, [psb[3]], [vTb_])
        yield
        k.act(gate, ps[2][:], AF.Silu, [psb[2]], [gateb])
        yield
        p6 = ps[6][:].bitcast(BF16)
        for tt in range(4):
            k.tr(p6[:, tt * 128:(tt + 1) * 128], kT_[:, tt * 128:(tt + 1) * 128], k.ident[:], [kTb_, k.cb], [psb[6]])
            yield
        for tt in range(4):
            k.tr(p6[:, 512 + tt * 128:512 + (tt + 1) * 128], vT_[:, tt * 128:(tt + 1) * 128], k.ident[:], [vTb_, k.cb], [psb[6]])
            yield
        k.cp("dve", ktm, p6[:, 0:512].rearrange("p (a b) -> p a b", b=128), [psb[6]], [ktmb])
        yield
        k.cp("dve", vtm, p6[:, 512:1024].rearrange("p (a b) -> p a b", b=128), [psb[6]], [vtmb])
        yield

    def loop_epi(hh):
        Ep, Epb, gate, gateb = Ep_[hh], Epb_[hh], gate_[hh], gateb_[hh]
        kT_, kTb_, qT_, qTb_ = kT__[hh], kTb__[hh], qT__[hh], qTb__[hh]
        ktm, ktmb, vtm, vtmb, em, emb, el = ktm_[hh], ktmb_[hh], vtm_[hh], vtmb_[hh], em_[hh], emb_[hh], el_[hh]
        e2 = c8(Ep)[:, :, 63]
        for tt in range(4):
            tsl = slice(tt * 128, (tt + 1) * 128)
            a = tt % 2
            k.mm(ps[4][:, 0:128], kT_[:, tsl], qT_[:, tsl], True, True, [kTb_, qTb_], [psb[4]])
            k.tt("dve", Am[a], ps[4][:, 0:128], k.mask2[:], ALU.mult, [psb[4], k.cb], [Amb[a]])
            k.mm(ps[7][:, tsl], vtm[:, tt, :], Am[a], True, False, [vtmb, Amb[a]], [psb[7]])
            for half in range(2):
                c = 2 * tt + half
                hs = slice(half * 64, (half + 1) * 64)
                csl = slice(tt * 128 + half * 64, tt * 128 + (half + 1) * 64)
                k.act(Stil, Sst[hh], AF.Copy, [Sb[hh], emb], [Stilb], scale=em[:, c:c + 1])
                k.mm(ps[7][:, csl], Stil, qT_[:, csl], False, half == 1, [Stilb, qTb_], [psb[7]])
                k.mm(ps[5][:, 0:128], ktm[hs, tt, :], vtm[hs, tt, :], True, True, [ktmb, vtmb], [psb[5]])
                k.ts("dve", tmpS, ps[5][:, 0:128], e2[:, c:c + 1], None, ALU.mult, None, [psb[5], Epb], [tmpSb])
                k.stt("dve", Sst[hh], Sst[hh], el[:, c:c + 1], tmpS, ALU.mult, ALU.add, [Sb[hh], emb, tmpSb], [Sb[hh]])
                yield
        k.act(t1e, ps[7][:], AF.Square, [psb[7]], [t1eb])
        yield
        k.mm(ps[4][:], k.ones_f[:], t1e, True, True, [k.cb, t1eb], [psb[4]])
        yield
        k.act(t2, ps[4][:], AF.Sqrt, [psb[4]], [t2b], bias=EPS, scale=1.0 / 128)
        yield
        k.recip(t2, t2, [t2b], [t2b])
        yield
        k.stt("dve", on, ps[7][:], go, t2, ALU.mult, ALU.mult, [psb[7], t2b, k.pfb], [onb])
        yield
        k.tt("pool", onT[hh], on, gate, ALU.mult, [onb, gateb], [onTb[hh]])
        yield

    def outp(sl, tb):
        for tt in range(4):
            i = 4 * tb + tt
            for half in range(2):
                bnk = (2, 3)[half]
                for hh in range(2):
                    k.mm(ps[bnk][:], onT[hh][:, tt * 128:(tt + 1) * 128], wop[sl][:, hh, half * 512:(half + 1) * 512],
                         hh == 0, hh == 1, [onTb[hh], wopb[sl]], [psb[bnk]])
                xs = k.x[:, i, half * 512:(half + 1) * 512]
                k.tt("dve", xs, xs, ps[bnk][:], ALU.add, [k.xb[i], psb[bnk]], [k.xb[i]])

    load(0)
    for hp in range(4):
        sl = hp % 2
        if hp + 1 < 4:
            load(hp + 1)
        for hh in range(2):
            k.memset("pool", Sst[hh], 0.0, [Sb[hh]])
        units = [(tb, hh) for tb in range(4) for hh in range(2)]
        for _ in pro(sl, hp, *units[0]):
            pass
        for n, (tb, hh) in enumerate(units):
            ga = loop_epi(hh)
            gb = pro(sl, hp, *units[n + 1]) if n + 1 < len(units) else None
            alive_a, alive_b = True, gb is not None
            while alive_a or alive_b:
                if alive_a:
                    try:
                        next(ga)
                    except StopIteration:
                        alive_a = False
                if alive_b:
                    for _ in range(2):
                        try:
                            next(gb)
                        except StopIteration:
                            alive_b = False
                            break
            if hh == 1:
                outp(sl, tb)

def fm(v):
    v = np.asarray(v, np.float32)
    return np.ascontiguousarray(v.reshape(-1, 128).T)


def pack_inputs(inp):
    cols = []
    lay = {}

    def put(name, arr):
        lay[name] = sum(c.shape[1] for c in cols)
        cols.append(np.asarray(arr, np.float32))

    put("norm_mix", np.concatenate([fm(inp["norm_mix"][l]) for l in range(4)], axis=1))
    put("norm_mlp", np.concatenate([fm(inp["norm_mlp"][l]) for l in range(4)], axis=1))
    put("mla_lat", np.concatenate([np.concatenate([fm(inp["mla_q_lat_norm"][j]), fm(inp["mla_kv_lat_norm"][j])], axis=1)
                                   for j in range(2)], axis=1))
    put("hg_lb", np.concatenate([fm(inp["hg_lb_logits"][i]) for i in range(4)], axis=1))
    put("hg_on", fm(inp["hg_out_norm"][0]))
    put("cv_b1", fm(inp["cv_b_pw1"][0]))
    put("cv_wdw", np.asarray(inp["cv_w_dw"][0], np.float32).T.reshape(8, 128, 31).transpose(1, 0, 2).reshape(128, 8 * 31))
    put("cv_bdw", fm(inp["cv_b_dw"][0]))
    put("cv_lng", fm(inp["cv_ln_g"][0]))
    put("cv_lnb", fm(inp["cv_ln_b"][0]))
    pf = np.ascontiguousarray(np.concatenate(cols, axis=1))
    lay["npf"] = pf.shape[1]
    tcols = []

    def putt(name, vec):
        lay[name] = sum(c.shape[0] for c in tcols)
        tcols.append(np.asarray(vec, np.float32).reshape(-1))

    putt("mla_head", np.concatenate([np.concatenate([inp["mla_q_head_norm"][j], inp["mla_k_head_norm"][j]]) for j in range(2)]))
    lay["npt_res"] = sum(c.shape[0] for c in tcols)
    putt("cv_b2", inp["cv_b_pw2"][0])
    ptv = np.concatenate(tcols)
    pt = np.ascontiguousarray(np.broadcast_to(ptv[None, :], (128, ptv.shape[0])))
    lay["npt"] = pt.shape[1]
    return pf, pt, lay


def pack_weights(inp):
    w = {}
    w["mlp_wi"] = np.asarray(inp["mlp_w_in"], np.float32)
    w["mlp_wo"] = np.asarray(inp["mlp_w_out"], np.float32)
    w["mla_wd"] = np.asarray(inp["mla_w_down"], np.float32)
    wuq = np.asarray(inp["mla_w_uq"], np.float32).reshape(2, 384, 8, 192)
    wukv = np.asarray(inp["mla_w_ukv"], np.float32).reshape(2, 256, 8, 256)
    uq = np.empty((2, 2, 384, 768), np.float32)
    ukv = np.empty((2, 2, 256, 1024), np.float32)
    for G in range(2):
        hs = [4 * G + i for i in range(4)]
        uq[:, G, :, 0:512] = wuq[:, :, hs, 0:128].reshape(2, 384, 512)
        rope_order = [4 * G + 0, 4 * G + 2, 4 * G + 1, 4 * G + 3]
        uq[:, G, :, 512:768] = wuq[:, :, rope_order, 128:192].reshape(2, 384, 256)
        ukv[:, G, :, 0:512] = wukv[:, :, hs, 0:128].reshape(2, 256, 512)
        ukv[:, G, :, 512:1024] = wukv[:, :, hs, 128:256].reshape(2, 256, 512)
    w["mla_wuq"] = uq
    w["mla_wukv"] = ukv
    w["mla_wo"] = np.asarray(inp["mla_w_o"], np.float32)
    w["cv_w1"] = np.asarray(inp["cv_w_pw1"][0], np.float32)
    w["cv_w2"] = np.asarray(inp["cv_w_pw2"][0], np.float32)
    w["hg_wi"] = np.asarray(inp["hg_w_in"][0], np.float32)
    w["hg_wo"] = np.asarray(inp["hg_w_o"][0], np.float32)
    return w


ALL_LAYERS = [("mla", 0, 0), ("mlp", 0, 0), ("hgrn", 1, 0), ("mlp", 1, 0),
              ("conv", 2, 0), ("mlp", 2, 0), ("mla", 3, 1), ("mlp", 3, 0)]


def run(inp, layers, n_cores=N_CORES, n_seq=2, trace=False, debug=False, stop=None):
    pf, pt, lay = pack_inputs(inp)
    nc, stats = build_program(n_seq, layers, lay, debug, stop)
    x = np.asarray(inp["x"], np.float32)
    pos = np.asarray(inp["positions"], np.int32)
    wts = pack_weights(inp)
    in_maps = []
    for c in range(n_cores):
        sl = slice(c * n_seq, (c + 1) * n_seq)
        m = dict(
            x=np.ascontiguousarray(x[sl]),
            pos=np.ascontiguousarray(pos[sl].reshape(n_seq, NT, 128).transpose(0, 2, 1)),
            pf=pf, pt=pt, **wts,
        )
        in_maps.append(m)
    res = run_bass_kernel_spmd(nc, in_maps, core_ids=list(range(n_cores)), **({"trace": True} if trace else {}))
    out = np.concatenate([r["out"] for r in res.results], axis=0)
    return out, res, stats


def kernel(**inputs):
    out, _, _ = run(inputs, ALL_LAYERS)
    return out.astype(np.float32)
```
